# Optimizing a Trainium2 kernel written in Bass

```python
import jax, jax.numpy as jnp
from jax import lax
import numpy as np

D_MODEL = 2048
BATCH = 8
SEQ = 2048
DEPTH = 2

MEM_TOKENS = 256
MEM_HEADS = 4
MEM_HEAD_DIM = 256
MEM_WIDTH = MEM_HEADS * MEM_HEAD_DIM
MLA_HEADS = 8
Q_LORA_RANK = 512
KV_LORA_RANK = 256
QK_NOPE_DIM = 128
QK_ROPE_DIM = 64
QK_HEAD_DIM = QK_NOPE_DIM + QK_ROPE_DIM
V_HEAD_DIM = 128
MLA_WIDTH = MLA_HEADS * V_HEAD_DIM
ROPE_THETA = 10000.0
Q_BLOCK = 128
POOL_WINDOWS = (2, 4, 8, 16)
POOL_GROUPS = 4
POOL_GROUP_DIM = 256
POOL_WIDTH = POOL_GROUPS * POOL_GROUP_DIM
N_BRANCHES = 3
IN_SPLITS = (Q_LORA_RANK,
             Q_LORA_RANK + KV_LORA_RANK + QK_ROPE_DIM,
             Q_LORA_RANK + KV_LORA_RANK + QK_ROPE_DIM + POOL_WIDTH,
             Q_LORA_RANK + KV_LORA_RANK + QK_ROPE_DIM + POOL_WIDTH + MEM_WIDTH)
IN_WIDTH = IN_SPLITS[-1] + N_BRANCHES * D_MODEL
D_FF_DENSE = 5632
N_EXPERTS = 8
TOP_K = 2
D_FF_EXPERT = 7168
MOE_BLOCK = 256
N_DENSE_LAYERS = (DEPTH + 1) // 2
N_MOE_LAYERS = DEPTH // 2
NORM_EPS = 1e-6

kernel_name = 'hybrid_mla_pool_memory_moe_block'


def rmsnorm(x, g):
    xf = x.astype(jnp.float32)
    y = xf * lax.rsqrt(jnp.mean(xf * xf, axis=-1, keepdims=True) + NORM_EPS)
    return (y * g.astype(jnp.float32)).astype(x.dtype)


def rope_tables(positions):
    inv_freq = ROPE_THETA ** (-jnp.arange(0, QK_ROPE_DIM, 2, dtype=jnp.float32) / QK_ROPE_DIM)
    ang = positions.astype(jnp.float32)[..., None] * inv_freq
    return jnp.cos(ang)[:, :, None, :], jnp.sin(ang)[:, :, None, :]


def apply_rope(x, cos, sin):
    half = x.shape[-1] // 2
    x1, x2 = x[..., :half], x[..., half:]
    c = cos.astype(x.dtype)
    s = sin.astype(x.dtype)
    return jnp.concatenate([x1 * c - x2 * s, x2 * c + x1 * s], axis=-1)


def rope_tail(x, cos, sin):
    return jnp.concatenate([x[..., :QK_NOPE_DIM], apply_rope(x[..., QK_NOPE_DIM:], cos, sin)], axis=-1)


def causal_block_attention(q, k, v):
    B, S, H, Dh = q.shape
    nq = S // Q_BLOCK
    scale = Dh ** -0.5
    qb = q.reshape(B, nq, Q_BLOCK, H, Dh).transpose(1, 0, 2, 3, 4)
    kpos = jnp.arange(S)

    def one_block(args):
        qi, i = args
        s = jnp.einsum('bqhd,bkhd->bhqk', qi, k, preferred_element_type=jnp.float32) * scale
        qpos = i * Q_BLOCK + jnp.arange(Q_BLOCK)
        s = jnp.where(qpos[:, None] >= kpos[None, :], s, -jnp.inf)
        p = jax.nn.softmax(s, axis=-1).astype(v.dtype)
        return jnp.einsum('bhqk,bkhd->bqhd', p, v)

    o = lax.map(one_block, (qb, jnp.arange(nq)))
    return o.transpose(1, 0, 2, 3, 4).reshape(B, S, H, v.shape[-1])


def causal_multiscale_pool(u):
    B, S, G, C = u.shape
    uf = u.astype(jnp.float32)
    cs = jnp.concatenate([jnp.zeros((B, 1, G, C), jnp.float32), jnp.cumsum(uf, axis=1)], axis=1)
    t1 = jnp.arange(1, S + 1, dtype=jnp.float32)
    outs = []
    for g, w in enumerate(POOL_WINDOWS):
        c = cs[:, :, g]
        lo = jnp.concatenate([jnp.zeros((B, w - 1, C), jnp.float32), c[:, :S + 1 - w]], axis=1)
        mean = (c[:, 1:] - lo) / jnp.minimum(t1, float(w))[None, :, None]
        outs.append(mean - uf[:, :, g])
    return jnp.stack(outs, axis=2).astype(u.dtype)


def memory_cross_attention(q, k, v):
    s = jnp.einsum('bshd,bmhd->bhsm', q, k, preferred_element_type=jnp.float32) * (q.shape[-1] ** -0.5)
    p = jax.nn.softmax(s, axis=-1).astype(v.dtype)
    return jnp.einsum('bhsm,bmhd->bshd', p, v)


def token_mixer(h, mem_n, cos, sin, w_in, mla_q_a_norm_g, mla_w_uq, mla_kv_a_norm_g, mla_w_ukv,
                mla_q_norm_g, mla_k_norm_g, mla_w_out, pool_w, pool_scale, pool_w_out,
                mem_w_kv, mem_q_norm_g, mem_k_norm_g, mem_w_out, w_o):
    B, S, _ = h.shape
    M = mem_n.shape[1]
    z = h @ w_in
    cq, ckv, u_pool, q_mem, gate_logits = jnp.split(z, IN_SPLITS, axis=-1)

    q = (rmsnorm(cq, mla_q_a_norm_g) @ mla_w_uq).reshape(B, S, MLA_HEADS, QK_HEAD_DIM)
    c_kv, k_pe = ckv[..., :KV_LORA_RANK], ckv[..., KV_LORA_RANK:]
    kv = (rmsnorm(c_kv, mla_kv_a_norm_g) @ mla_w_ukv).reshape(B, S, MLA_HEADS, QK_NOPE_DIM + V_HEAD_DIM)
    k_nope, v = kv[..., :QK_NOPE_DIM], kv[..., QK_NOPE_DIM:]
    k = jnp.concatenate([k_nope, jnp.broadcast_to(k_pe[:, :, None, :], (B, S, MLA_HEADS, QK_ROPE_DIM))], axis=-1)
    q = rope_tail(rmsnorm(q, mla_q_norm_g), cos, sin)
    k = rope_tail(rmsnorm(k, mla_k_norm_g), cos, sin)
    o_mla = causal_block_attention(q, k, v).reshape(B, S, MLA_WIDTH) @ mla_w_out

    pooled = causal_multiscale_pool(u_pool.reshape(B, S, POOL_GROUPS, POOL_GROUP_DIM))
    mixed = jnp.einsum('bsgc,gcd->bsgd', pooled, pool_w).reshape(B, S, POOL_WIDTH) * pool_scale
    o_pool = mixed @ pool_w_out

    kv_m = mem_n @ mem_w_kv
    k_m = rmsnorm(kv_m[..., :MEM_WIDTH].reshape(B, M, MEM_HEADS, MEM_HEAD_DIM), mem_k_norm_g)
    v_m = kv_m[..., MEM_WIDTH:].reshape(B, M, MEM_HEADS, MEM_HEAD_DIM)
    q_m = rmsnorm(q_mem.reshape(B, S, MEM_HEADS, MEM_HEAD_DIM), mem_q_norm_g)
    o_mem = memory_cross_attention(q_m, k_m, v_m).reshape(B, S, MEM_WIDTH) @ mem_w_out

    gates = jax.nn.sigmoid(gate_logits.reshape(B, S, N_BRANCHES, D_MODEL))
    merged = gates[:, :, 0] * o_mla + gates[:, :, 1] * o_pool + gates[:, :, 2] * o_mem
    return merged @ w_o


def dense_swiglu(h, w_gate, w_up, w_down):
    return (jax.nn.silu(h @ w_gate) * (h @ w_up)) @ w_down


def moe_swiglu(h, router_w, router_b, w_gate, w_up, w_down):
    B, S, D = h.shape
    T = B * S
    A = T * TOP_K
    xt = h.reshape(T, D)
    logits = xt.astype(jnp.float32) @ router_w.astype(jnp.float32) + router_b.astype(jnp.float32)
    top_logit, top_idx = lax.top_k(logits, TOP_K)
    top_gate = jax.nn.softmax(top_logit, axis=-1)
    flat_e = top_idx.reshape(A)
    flat_tok = jnp.repeat(jnp.arange(T, dtype=jnp.int32), TOP_K)
    flat_w = top_gate.reshape(A)
    order = jnp.argsort(flat_e, stable=True)
    se, stok, sw = flat_e[order], flat_tok[order], flat_w[order]
    counts = jnp.bincount(flat_e, length=N_EXPERTS)
    starts = jnp.cumsum(counts) - counts
    padded = (counts + MOE_BLOCK - 1) // MOE_BLOCK * MOE_BLOCK
    pends = jnp.cumsum(padded)
    pstarts = pends - padded
    dest = pstarts[se] + (jnp.arange(A) - starts[se])
    n_blocks = -(-A // MOE_BLOCK) + N_EXPERTS
    n_slots = n_blocks * MOE_BLOCK
    slot_tok = jnp.full((n_slots,), T, jnp.int32).at[dest].set(stok)
    slot_w = jnp.zeros((n_slots,), jnp.float32).at[dest].set(sw)
    block_e = jnp.clip(jnp.searchsorted(pends, jnp.arange(n_blocks) * MOE_BLOCK, side='right'), 0, N_EXPERTS - 1)
    x_pad = jnp.concatenate([xt, jnp.zeros((1, D), xt.dtype)], axis=0)
    xs = x_pad[slot_tok].reshape(n_blocks, MOE_BLOCK, D)

    def expert_block(args):
        xb, e = args
        return (jax.nn.silu(xb @ w_gate[e]) * (xb @ w_up[e])) @ w_down[e]

    ys = lax.map(expert_block, (xs, block_e)).reshape(n_slots, D)
    y = jax.ops.segment_sum(ys * slot_w[:, None].astype(ys.dtype), slot_tok, num_segments=T + 1)[:T]
    return y.reshape(B, S, D)


def setup_inputs(seed: int = 0) -> dict:
    key = jax.random.key(seed)
    ks = iter(jax.random.split(key, 32))
    L, ND, NM = DEPTH, N_DENSE_LAYERS, N_MOE_LAYERS

    def w(shape, fan_in):
        return jax.random.normal(next(ks), shape, jnp.float32) * (fan_in ** -0.5)

    def gain(shape, s=0.02):
        return 1.0 + s * jax.random.normal(next(ks), shape, jnp.float32)

    x = jax.random.normal(next(ks), (BATCH, SEQ, D_MODEL), jnp.float32)
    mem = jax.random.normal(next(ks), (BATCH, MEM_TOKENS, D_MODEL), jnp.float32)
    positions = (jnp.arange(SEQ, dtype=jnp.int32)[None, :]
                 + jax.random.randint(next(ks), (BATCH, 1), 0, 1024, dtype=jnp.int32))
    return {
        'x': x,
        'mem': mem,
        'positions': positions,
        'attn_norm_g': gain((L, D_MODEL)),
        'w_in': w((L, D_MODEL, IN_WIDTH), D_MODEL),
        'mla_q_a_norm_g': gain((L, Q_LORA_RANK)),
        'mla_w_uq': w((L, Q_LORA_RANK, MLA_HEADS * QK_HEAD_DIM), Q_LORA_RANK),
        'mla_kv_a_norm_g': gain((L, KV_LORA_RANK)),
        'mla_w_ukv': w((L, KV_LORA_RANK, MLA_HEADS * (QK_NOPE_DIM + V_HEAD_DIM)), KV_LORA_RANK),
        'mla_q_norm_g': gain((L, QK_HEAD_DIM)),
        'mla_k_norm_g': gain((L, QK_HEAD_DIM)),
        'mla_w_out': w((L, MLA_WIDTH, D_MODEL), MLA_WIDTH),
        'pool_w': w((L, POOL_GROUPS, POOL_GROUP_DIM, POOL_GROUP_DIM), POOL_GROUP_DIM),
        'pool_scale': gain((L, POOL_WIDTH), 0.1),
        'pool_w_out': w((L, POOL_WIDTH, D_MODEL), POOL_WIDTH),
        'mem_norm_g': gain((L, D_MODEL)),
        'mem_w_kv': w((L, D_MODEL, 2 * MEM_WIDTH), D_MODEL),
        'mem_q_norm_g': gain((L, MEM_HEAD_DIM)),
        'mem_k_norm_g': gain((L, MEM_HEAD_DIM)),
        'mem_w_out': w((L, MEM_WIDTH, D_MODEL), MEM_WIDTH),
        'w_o': w((L, D_MODEL, D_MODEL), D_MODEL),
        'ffn_norm_g': gain((L, D_MODEL)),
        'dense_w_gate': w((ND, D_MODEL, D_FF_DENSE), D_MODEL),
        'dense_w_up': w((ND, D_MODEL, D_FF_DENSE), D_MODEL),
        'dense_w_down': w((ND, D_FF_DENSE, D_MODEL), D_FF_DENSE),
        'router_w': w((NM, D_MODEL, N_EXPERTS), D_MODEL),
        'router_b': 0.01 * jax.random.normal(next(ks), (NM, N_EXPERTS), jnp.float32),
        'moe_w_gate': w((NM, N_EXPERTS, D_MODEL, D_FF_EXPERT), D_MODEL),
        'moe_w_up': w((NM, N_EXPERTS, D_MODEL, D_FF_EXPERT), D_MODEL),
        'moe_w_down': w((NM, N_EXPERTS, D_FF_EXPERT, D_MODEL), D_FF_EXPERT),
    }


def reference(x, mem, positions, attn_norm_g, w_in, mla_q_a_norm_g, mla_w_uq, mla_kv_a_norm_g,
              mla_w_ukv, mla_q_norm_g, mla_k_norm_g, mla_w_out, pool_w, pool_scale, pool_w_out,
              mem_norm_g, mem_w_kv, mem_q_norm_g, mem_k_norm_g, mem_w_out, w_o, ffn_norm_g,
              dense_w_gate, dense_w_up, dense_w_down, router_w, router_b, moe_w_gate, moe_w_up,
              moe_w_down):
    cos, sin = rope_tables(positions)
    y = x
    for l in range(DEPTH):
        h = rmsnorm(y, attn_norm_g[l])
        mem_n = rmsnorm(mem, mem_norm_g[l])
        y = y + token_mixer(h, mem_n, cos, sin, w_in[l], mla_q_a_norm_g[l], mla_w_uq[l],
                            mla_kv_a_norm_g[l], mla_w_ukv[l], mla_q_norm_g[l], mla_k_norm_g[l],
                            mla_w_out[l], pool_w[l], pool_scale[l], pool_w_out[l], mem_w_kv[l],
                            mem_q_norm_g[l], mem_k_norm_g[l], mem_w_out[l], w_o[l])
        h = rmsnorm(y, ffn_norm_g[l])
        i = l // 2
        if l % 2 == 0:
            y = y + dense_swiglu(h, dense_w_gate[i], dense_w_up[i], dense_w_down[i])
        else:
            y = y + moe_swiglu(h, router_w[i], router_b[i], moe_w_gate[i], moe_w_up[i], moe_w_down[i])
    return y
```

```python
import numpy as np
import concourse.bass as bass
import concourse.mybir as mybir

F32 = mybir.dt.float32
BF16 = mybir.dt.bfloat16
I32 = mybir.dt.int32
ALU = mybir.AluOpType
AF = mybir.ActivationFunctionType
AX = mybir.AxisListType

ENGS = ("pe", "act", "dve", "pool", "sp")
EPOCH = 500
DMA_POOLN = {'sp': 40, 'pool': 24, 'act': 8, 'pe': 1, 'dve': 1}


class Buf:
    __slots__ = ("name", "writers", "readers", "const", "sems", "cnt", "excl")

    def __init__(self, name, const=False, excl=False):
        self.name = name
        self.excl = excl
        self.writers = []
        self.readers = {}
        self.const = const
        self.sems = []
        self.cnt = 0


class Op:
    __slots__ = ("eng", "fn", "deps", "is_dma", "sem", "semval", "signals", "seq", "sigpos", "clock", "dclock")

    def __init__(self, eng, fn, is_dma):
        self.eng = eng
        self.fn = fn
        self.is_dma = is_dma
        self.deps = []
        self.sem = None
        self.semval = 0
        self.signals = False
        self.seq = -1
        self.sigpos = -1
        self.clock = None
        self.dclock = None


class Prog:
    def __init__(self, nc, stack):
        self.nc = nc
        self.stack = stack
        self.ops = {e: [] for e in ENGS}
        self.seen = {e: {f: -1 for f in ENGS} for e in ENGS}
        self.dseen = {e: {} for e in ENGS}
        self.nsem = 0
        self.n_ops = 0
        self.dma_pool = {}
        self.dma_rr = {}

    def sem(self, name):
        self.nsem += 1
        return self.stack.enter_context(self.nc.semaphore(name))

    def sbuf(self, name, shape, dt):
        return self.stack.enter_context(self.nc.sbuf_tensor(name, list(shape), dt))

    def psum(self, name, shape, dt=F32):
        return self.stack.enter_context(self.nc.psum_tensor(name, list(shape), dt))

    def _record(self, eng, fn, reads, writes, is_dma, join):
        op = Op(eng, fn, is_dma)
        deps = {}
        if is_dma:
            pool = self.dma_pool.setdefault(eng, [])
            if len(pool) < DMA_POOLN[eng]:
                pool.append([self.sem(f"d_{eng}{len(pool)}"), 0, None])
                slot = pool[-1]
            else:
                i = self.dma_rr.get(eng, 0)
                self.dma_rr[eng] = (i + 1) % len(pool)
                slot = pool[i]
            if slot[2] is not None:
                deps[id(slot[2])] = slot[2]
            slot[1] += 16
            slot[2] = op
            op.sem = slot[0]
            op.semval = slot[1]
        for b in reads:
            for w in b.writers:
                deps[id(w)] = w
            if b.excl:
                for k, r in b.readers.items():
                    if k != eng:
                        deps[id(r)] = r
        for b in writes:
            for r in b.readers.values():
                deps[id(r)] = r
            if not join:
                for w in b.writers:
                    deps[id(w)] = w
        my_seen = self.seen[eng]
        my_dseen = self.dseen[eng]
        best_c = {}
        best_d = {}
        for d in deps.values():
            if d.is_dma:
                if my_dseen.get(id(d.sem), 0) >= d.semval:
                    continue
                k = id(d.sem)
                if k not in best_d or best_d[k].semval < d.semval:
                    best_d[k] = d
            else:
                if eng == "pe" and d.eng == "pe":
                    continue
                if my_seen[d.eng] >= d.seq:
                    continue
                if d.eng not in best_c or best_c[d.eng].seq < d.seq:
                    best_c[d.eng] = d
        need = list(best_c.values()) + list(best_d.values())
        dropped = [d for d in deps.values() if d not in need]
        for d in need:
            if d.is_dma:
                my_dseen[id(d.sem)] = max(my_dseen.get(id(d.sem), 0), d.semval)
            else:
                d.signals = True
                if my_seen[d.eng] < d.seq:
                    my_seen[d.eng] = d.seq
            for f, s in d.clock.items():
                if my_seen[f] < s:
                    my_seen[f] = s
            for k, v in d.dclock.items():
                if my_dseen.get(k, 0) < v:
                    my_dseen[k] = v
        op.deps = need
        op.seq = len(self.ops[eng])
        op.clock = dict(my_seen)
        op.dclock = dict(my_dseen)
        self.ops[eng].append(op)
        self.n_ops += 1
        for b in reads:
            if not b.const:
                key = (eng, id(op)) if is_dma else eng
                b.readers[key] = op
        for b in writes:
            if join:
                b.writers.append(op)
            else:
                b.writers = [op]
                b.readers = {}
        return op

    def op(self, eng, fn, reads=(), writes=(), join=False):
        return self._record(eng, fn, list(reads), list(writes), False, join)

    def dma(self, queue, out, in_, reads=(), writes=(), join=False, **kw):
        assert len(writes) >= 1
        return self._record(queue, lambda e: e.dma_start(out=out, in_=in_, **kw), list(reads), list(writes), True, join)

    def emit(self, final_waits=()):
        nc = self.nc
        esems = {}
        for e in ENGS:
            pos = 0
            for o in self.ops[e]:
                if (not o.is_dma) and o.signals:
                    o.sigpos = pos
                    pos += 1
            n_ep = (pos + EPOCH - 1) // EPOCH
            esems[e] = [self.sem(f"s_{e}{i}") for i in range(max(n_ep, 1))]
        engobj = {"pe": "tensor", "act": "scalar", "dve": "vector", "pool": "gpsimd", "sp": "sync"}
        with nc.Block() as block:
            def body_for(e):
                def body(eng):
                    for o in self.ops[e]:
                        for d in o.deps:
                            if d.is_dma:
                                eng.wait_ge(d.sem, d.semval)
                            else:
                                ep, idx = divmod(d.sigpos, EPOCH)
                                eng.wait_ge(esems[d.eng][ep], idx + 1)
                        ins = o.fn(eng)
                        if o.is_dma:
                            ins.then_inc(o.sem, 16)
                        elif o.signals:
                            ep, idx = divmod(o.sigpos, EPOCH)
                            ins.then_inc(esems[e][ep], 1)
                    if e == "sp":
                        for d in final_waits:
                            eng.wait_ge(d.sem, d.semval)
                return body
            for e in ENGS:
                if self.ops[e] or e == "sp":
                    getattr(block, engobj[e])(body_for(e))


from contextlib import ExitStack
import math

S = 2048
D = 2048
NT = 16
CAP = 768
NST = CAP // 128
EPS = 1e-6

WEIGHT_SHAPES = {
    'attn_norm_g': (2, 2048), 'w_in': (2, 2048, 9024), 'mla_q_a_norm_g': (2, 512), 'mla_w_uq': (2, 512, 1536),
    'mla_kv_a_norm_g': (2, 256), 'mla_w_ukv': (2, 256, 2048), 'mla_q_norm_g': (2, 192), 'mla_k_norm_g': (2, 192),
    'mla_w_out': (2, 1024, 2048), 'pool_w': (2, 4, 256, 256), 'pool_scale': (2, 1024), 'pool_w_out': (2, 1024, 2048),
    'mem_norm_g': (2, 2048), 'mem_w_kv': (2, 2048, 2048), 'mem_q_norm_g': (2, 256), 'mem_k_norm_g': (2, 256),
    'mem_w_out': (2, 1024, 2048), 'w_o': (2, 2048, 2048), 'ffn_norm_g': (2, 2048),
    'dense_w_gate': (1, 2048, 5632), 'dense_w_up': (1, 2048, 5632), 'dense_w_down': (1, 5632, 2048),
    'router_w': (1, 2048, 8), 'router_b': (1, 8), 'moe_w_gate': (1, 8, 2048, 7168), 'moe_w_up': (1, 8, 2048, 7168),
    'moe_w_down': (1, 8, 7168, 2048),
}
CONST_SHAPES = {'c_ident': (128, 128), 'c_tri': (128, 128), 'c_ones': (128, 128), 'c_pool': (12, 128, 128),
                'c_mask': (4, 128, 512), 'c_iota': (128, CAP), 'c_invf': (128, 32)}


def host_constants():
    c = {}
    c['c_ident'] = np.eye(128, dtype=np.float32)
    tri = np.zeros((128, 128), np.float32)
    for a in range(128):
        tri[a, a + 1:] = 1.0
    c['c_tri'] = tri
    c['c_ones'] = np.ones((128, 128), np.float32)
    pm = np.zeros((12, 128, 128), np.float32)
    for g, w in enumerate((2, 4, 8, 16)):
        for t in range(128):
            for d in range(w):
                tp = t - d
                if tp >= 0:
                    pm[3 * g + 1, tp, t] += 1.0 / w
                else:
                    pm[3 * g + 2, 128 + tp, t] += 1.0 / w
            pm[3 * g + 1, t, t] -= 1.0
            cnt = min(t + 1, w)
            for d in range(cnt):
                pm[3 * g + 0, t - d, t] += 1.0 / cnt
            pm[3 * g + 0, t, t] -= 1.0
    c['c_pool'] = pm
    mk = np.zeros((4, 128, 512), np.float32)
    for m in range(4):
        for kp in range(128):
            mk[m, kp, :] = (np.arange(512) >= 128 * m + kp)
    c['c_mask'] = mk
    c['c_iota'] = np.broadcast_to(np.arange(CAP, dtype=np.float32)[None, :], (128, CAP)).copy()
    invf = (np.float32(10000.0) ** (-np.arange(0, 64, 2, dtype=np.float32) / np.float32(64))).astype(np.float32)
    c['c_invf'] = np.broadcast_to(invf[None, :], (128, 32)).copy()
    return c


class _Stop(Exception):
    pass


def build_program(layers=(0, 1), parts=('mixer', 'ffn'), dbg_names=(), stop_at=None):
    nc = bass.Bass("TRN2", target_bir_lowering=False)
    class LazyInputs(dict):
        def __missing__(self, k):
            shp = {'x': (S, D), 'mem': (256, D), 'pos': (128, 16)}.get(k) or WEIGHT_SHAPES.get(k) or CONST_SHAPES[k]
            v = nc.dram_tensor(k, list(shp), I32 if k == 'pos' else F32, kind="ExternalInput").ap()
            self[k] = v
            return v
    T = LazyInputs()
    nc.used_inputs = T
    OUT = nc.dram_tensor('out', [S, D], F32, kind="ExternalOutput").ap()

    def scratch(name, shape, dt):
        kind = "ExternalOutput" if name in dbg_names else "Internal"
        return nc.dram_tensor(name, list(shape), dt, kind=kind).ap()

    mixT_d = scratch('mixT_d', [1024, S], BF16)
    xattT_d = scratch('xattT_d', [1024, S], BF16)
    attT_d = scratch('attT_d', [1024, S], BF16)
    gT_d = scratch('gT_d', [6144, S], BF16)
    htm_d = scratch('htm_d', [S, D], BF16)
    qnT_d = scratch('qnT_d', [1024, S], BF16)
    qrT_d = scratch('qrT_d', [512, S], BF16)
    knT_d = scratch('knT_d', [1024, S], BF16)
    krT_d = scratch('krT_d', [64, S], BF16)
    V_d = scratch('V_d', [S, 1024], BF16)
    hT_dbg = scratch('hT_dbg', [D, S], BF16) if 'hT_dbg' in dbg_names else None
    mrg_dbg = scratch('mrg_dbg', [D, S], BF16) if 'mrg_dbg' in dbg_names else None
    rt_dbg = scratch('rt_dbg', [128, 3 * 128], F32) if 'rt_dbg' in dbg_names else None

    with ExitStack() as st:
        P = Prog(nc, st)
        finals = []

        def chk(name):
            if stop_at == name:
                raise _Stop()

        RA_E = 56 * CAP
        RA = P.sbuf('RA', [128, RA_E], BF16)
        RAq = [Buf(f'RA_q{i}') for i in range(4)] + [Buf('RA_tail')]
        RB_E = 32768
        RB = P.sbuf('RB', [128, RB_E], BF16)
        RBB = [Buf(f'RB_{i}') for i in range(RB_E // 2048)]

        def rb(off_kb, size_kb, dt=BF16):
            a, b = off_kb * 512, (off_kb + size_kb) * 512
            a, b = int(a), int(b)
            ap = RB[:, a:b]
            if dt is F32:
                ap = ap.bitcast(F32)
            return ap, RBB[a // 2048:(b + 2047) // 2048]

        SLAB_E = 4096
        slabs = [(P.sbuf(f'slab{i}', [128, SLAB_E], BF16), Buf(f'slab{i}')) for i in range(2)]
        GAIN = P.sbuf('GAIN', [128, 1664], F32)
        bgain = Buf('gain')
        PSC = P.sbuf('PSC', [128, 8], F32)
        STAT = [(P.sbuf(f'stat{i}', [128, 64], F32), Buf(f'stat{i}')) for i in range(4)]
        SC = P.sbuf('SC', [128, 16, 64], F32)
        SIN = SC[:, :, 0:32]
        COS = SC[:, :, 32:64]
        bcs = Buf('cossin')
        SCK = P.sbuf('SCK', [128, 16, 8], F32)
        bsck = Buf('scaleK')
        SCM = P.sbuf('SCM', [128, 2, 4], F32)
        bscm = Buf('scaleM')
        ident = P.sbuf('ident', [128, 128], BF16)
        identf = P.sbuf('identf', [128, 128], F32)
        onesb = P.sbuf('onesb', [128, 128], BF16)
        trib = P.sbuf('trib', [128, 128], BF16)
        poolm = P.sbuf('poolm', [128, 12, 128], BF16)
        maskb = P.sbuf('maskb', [128, 4, 512], BF16)
        iota = P.sbuf('iota', [128, CAP], F32)
        invf = P.sbuf('invf', [128, 32], F32)
        pib = P.sbuf('pib', [128, 1], F32)
        bconst = Buf('const', const=True)
        RSEL = P.sbuf('RSEL', [128, 16, 8], F32)
        RWT = P.sbuf('RWT', [128, 16, 8], F32)
        RPOS = P.sbuf('RPOS', [128, 16, 8], F32)
        RSELB = P.sbuf('RSELB', [128, 16, 8], BF16)
        brt = Buf('router')
        RW = P.sbuf('RW', [128, 16, 8], F32)
        RBI = P.sbuf('RBI', [128, 8], F32)
        brw = Buf('rw')

        MM = [(P.psum(f'mm{i}', [128, 512], F32), Buf(f'mm{i}', excl=True)) for i in range(5)]
        TP = [(P.psum(f'tp{i}', [128, 1024], BF16)[:, 0:512], Buf(f'tp{i}', excl=True)) for i in range(2)]
        AUXt = P.psum('aux', [128, 512], F32)
        AUX = (AUXt, Buf('aux', excl=True))
        rr = {}

        def nxt(kind, lst, n=None):
            n = len(lst) if n is None else n
            i = rr.get(kind, 0) % n
            rr[kind] = i + 1
            return lst[i]

        def ev_eng():
            rr['ev'] = rr.get('ev', 0) ^ 1
            return 'dve'

        def copy_op(eng, out, in_, reads, writes, join=False):
            if eng == 'act':
                return P.op('act', lambda e: e.activation(out=out, in_=in_, func=AF.Copy), reads=reads, writes=writes, join=join)
            return P.op(eng, lambda e: e.tensor_copy(out=out, in_=in_), reads=reads, writes=writes, join=join)

        ybuf = [Buf(f'y{t}') for t in range(NT)]

        def cload(dst, src):
            P.dma('pool', dst, src, writes=[bconst], join=True)
        cload(ident[:], T['c_ident'])
        cload(identf[:], T['c_ident'])
        cload(onesb[:], T['c_ones'])
        cload(trib[:], T['c_tri'])
        cload(poolm[:], T['c_pool'].rearrange("k p f -> p k f"))
        cload(maskb[:], T['c_mask'].rearrange("k p f -> p k f"))
        cload(iota[:], T['c_iota'])
        cload(invf[:], T['c_invf'])

        def load_slab(W2d, r0, nrows, c0, ncols, dst=None):
            if dst is None:
                tile, buf = nxt('slab', slabs)
                bufs = [buf]
                flat = tile[:, :]
            else:
                flat, bufs = dst
            kc = nrows // 128
            assert nrows % 128 == 0 and kc * ncols <= flat.shape[1], (nrows, ncols, flat.shape)
            view = flat[:, 0:kc * ncols].rearrange("p (j f) -> p j f", f=ncols)
            src = W2d[r0:r0 + nrows, c0:c0 + ncols].rearrange("(j p) f -> p j f", p=128)
            step = 8
            for j0 in range(0, kc, step):
                j1 = min(kc, j0 + step)
                P.dma('pool', view[:, j0:j1, :], src[:, j0:j1, :], writes=bufs, join=(j0 > 0))
            return view, bufs

        def bcast_row(dst, row_ap, bufs, first=True):
            P.dma('sp', dst, row_ap.partition_broadcast(128), writes=bufs, join=not first)

        def mm_acc(ps, psb, pairs, reads, first=True, last=True):
            n = len(pairs)
            for i, (l, r) in enumerate(pairs):
                P.op('pe', lambda e, l=l, r=r, i=i: e.matmul(ps, lhsT=l, rhs=r, start=(first and i == 0), stop=(last and i == n - 1)),
                     reads=reads, writes=[psb], join=not (first and i == 0))

        def transposes(srcs, src_reads, dst_fn, dst_writes, width=128):
            for q in range(0, len(srcs), 4):
                m = min(4, len(srcs) - q)
                tp, tpb = nxt('tp', TP)
                tpv = tp.rearrange("p (a b) -> p a b", b=128)
                for i in range(m):
                    P.op('pe', lambda e, i=i, s=srcs[q + i], tpv=tpv: e.transpose(out=tpv[0:width, i, :], in_=s, identity=ident[:]),
                         reads=src_reads + [bconst], writes=[tpb], join=(i > 0))
                copy_op(ev_eng(), dst_fn(q, m), tpv[0:width, 0:m, :], [tpb], dst_writes, join=True)

        def rstd_from_ss(ss_ap, out_ap, n, statb, scale_extra=1.0):
            P.op('dve', lambda e: e.tensor_scalar(out=out_ap, in0=ss_ap, scalar1=1.0 / n, scalar2=EPS, op0=ALU.mult, op1=ALU.add),
                 reads=statb, writes=statb, join=True)
            P.op('act', lambda e: e.activation(out=out_ap, in_=out_ap, func=AF.Sqrt), reads=statb, writes=statb, join=True)
            P.op('dve', lambda e: e.reciprocal(out=out_ap, in_=out_ap), reads=statb, writes=statb, join=True)
            if scale_extra != 1.0:
                P.op('dve', lambda e: e.tensor_scalar(out=out_ap, in0=out_ap, scalar1=float(scale_extra), scalar2=None, op0=ALU.mult),
                     reads=statb, writes=statb, join=True)

        def store(dram_ap, sb_ap, reads, dbuf, join=True, queue='sp'):
            return P.dma(queue, dram_ap, sb_ap, reads=reads, writes=[dbuf], join=join)

        def phase_rope_tables():
            posi = P.sbuf('posi', [128, 16], I32)
            posf = P.sbuf('posf', [128, 16], F32)
            argt = P.sbuf('argt', [128, 16, 64], F32)
            ni = P.sbuf('rope_ni', [128, 16, 64], I32)
            nf = P.sbuf('rope_nf', [128, 16, 64], F32)
            bp = Buf('posi')
            J = dict(reads=[bp, bconst], writes=[bp], join=True)
            P.dma('sp', posi[:], T['pos'], writes=[bp])
            P.op('dve', lambda e: e.tensor_copy(out=posf[:], in_=posi[:]), reads=[bp], writes=[bp])
            for t in range(NT):
                P.op('dve', lambda e, t=t: e.tensor_scalar(out=argt[:, t, 0:32], in0=invf[:], scalar1=posf[:, t:t + 1], scalar2=None, op0=ALU.mult), **J)
            P.op('dve', lambda e: e.tensor_scalar(out=argt[:, :, 32:64], in0=argt[:, :, 0:32], scalar1=math.pi / 2, scalar2=None, op0=ALU.add), **J)
            P.op('dve', lambda e: e.tensor_scalar(out=nf[:], in0=argt[:], scalar1=1.0 / (2 * math.pi), scalar2=None, op0=ALU.mult), **J)
            P.op('dve', lambda e: e.tensor_copy(out=ni[:], in_=nf[:]), **J)
            P.op('dve', lambda e: e.tensor_copy(out=nf[:], in_=ni[:]), **J)
            P.op('dve', lambda e: e.scalar_tensor_tensor(out=argt[:], in0=nf[:], scalar=-2 * math.pi, in1=argt[:], op0=ALU.mult, op1=ALU.add), **J)
            P.op('dve', lambda e: e.tensor_scalar(out=nf[:], in0=argt[:], scalar1=math.pi, scalar2=None, op0=ALU.is_gt), **J)
            P.op('dve', lambda e: e.scalar_tensor_tensor(out=argt[:], in0=nf[:], scalar=-2 * math.pi, in1=argt[:], op0=ALU.mult, op1=ALU.add), **J)
            P.op('dve', lambda e: e.tensor_scalar(out=nf[:], in0=argt[:], scalar1=-math.pi, scalar2=None, op0=ALU.is_lt), **J)
            P.op('dve', lambda e: e.scalar_tensor_tensor(out=argt[:], in0=nf[:], scalar=2 * math.pi, in1=argt[:], op0=ALU.mult, op1=ALU.add), **J)
            P.op('act', lambda e: e.activation(out=SC[:], in_=argt[:], func=AF.Sin), reads=[bp], writes=[bcs])

        bhtm = Buf('htm_d')

        def phase_norm(src2d, ntok, g_row, mode, dst_view=None, dst_bufs_fn=None, src_is_out=False, router=False):
            GBv, GBb = rb(24, 8, F32)
            bcast_row(GBv, g_row, GBb)
            for t in range(ntok // 128):
                xt, xb = rb(8 * (t % 2), 8, F32)
                hb, hbb = rb(16 + 4 * (t % 2), 4)
                stt, stb = nxt('stat', STAT)
                P.dma('sp', xt, src2d[t * 128:(t + 1) * 128, :], reads=[ybuf[t]] if src_is_out else [], writes=xb)
                chk('n0')
                P.op('act', lambda e, xt=xt, hb=hb, stt=stt: e.activation(out=hb, in_=xt, func=AF.Square, accum_out=stt[:, 0:1]),
                     reads=xb, writes=hbb + [stb])
                chk('n1')
                rstd_from_ss(stt[:, 0:1], stt[:, 1:2], 2048, [stb])
                chk('n2')
                if router:
                    hf, hfb = rb(32, 8, F32)
                    P.op('dve', lambda e, xt=xt, hf=hf, stt=stt: e.scalar_tensor_tensor(out=hf, in0=xt, scalar=stt[:, 1:2], in1=GBv,
                                                                                      op0=ALU.mult, op1=ALU.mult), reads=xb + [stb] + GBb, writes=hfb)
                    P.op('act', lambda e, hb=hb, hf=hf: e.activation(out=hb, in_=hf, func=AF.Copy), reads=hfb, writes=hbb)
                    router_tile(t, hf, hfb)
                else:
                    P.op('dve', lambda e, xt=xt, hb=hb, stt=stt: e.scalar_tensor_tensor(out=hb, in0=xt, scalar=stt[:, 1:2], in1=GBv,
                                                                                      op0=ALU.mult, op1=ALU.mult), reads=xb + [stb] + GBb, writes=hbb)
                chk('n3')
                if mode == 'fm':
                    transposes([hb[:, c * 128:(c + 1) * 128] for c in range(16)], hbb,
                               lambda q, m, t=t: dst_view[:, q:q + m, t * 128:(t + 1) * 128], dst_bufs_fn(t))
                else:
                    store(htm_d[t * 128:(t + 1) * 128, :], hb, hbb, bhtm, join=(t > 0))
                chk('n4')
                chk('n4x')

        def router_tile(t, hf, hfb):
            ht32, htb = rb(40, 8, F32)
            htv = ht32.rearrange("p (c t) -> p c t", t=128)
            for q in range(0, 16, 4):
                ps, psb = nxt('mm', MM)
                psv = ps.rearrange("p (a b) -> p a b", b=128)
                for i in range(4):
                    P.op('pe', lambda e, i=i, q=q, psv=psv: e.transpose(out=psv[:, i, :], in_=hf[:, (q + i) * 128:(q + i + 1) * 128], identity=identf[:]),
                         reads=hfb + [bconst], writes=[psb], join=(i > 0))
                copy_op(ev_eng(), htv[:, q:q + 4, :], psv[:, :, :], [psb], htb, join=(q > 0))
            ps, psb = AUX
            mm_acc(ps[:, 0:8], psb, [(htv[:, j, :], RW[:, j, :]) for j in range(16)], htb + [brw])
            stt, stb = nxt('stat', STAT)
            lg = stt[:, 0:8]
            P.op('dve', lambda e: e.tensor_tensor(out=lg, in0=ps[:, 0:8], in1=RBI[:], op=ALU.add), reads=[psb, brw], writes=[stb])
            m1, eq1, l2, m2, eq2, dd = stt[:, 8:9], stt[:, 16:24], stt[:, 24:32], stt[:, 9:10], stt[:, 32:40], stt[:, 10:13]
            J = dict(reads=[stb], writes=[stb], join=True)
            P.op('dve', lambda e: e.reduce_max(out=m1, in_=lg, axis=AX.X), **J)
            P.op('dve', lambda e: e.tensor_scalar(out=eq1, in0=lg, scalar1=m1, scalar2=None, op0=ALU.is_equal), **J)
            P.op('dve', lambda e: e.scalar_tensor_tensor(out=l2, in0=eq1, scalar=-1e30, in1=lg, op0=ALU.mult, op1=ALU.add), **J)
            P.op('dve', lambda e: e.reduce_max(out=m2, in_=l2, axis=AX.X), **J)
            P.op('dve', lambda e: e.tensor_scalar(out=eq2, in0=l2, scalar1=m2, scalar2=None, op0=ALU.is_equal), **J)
            P.op('dve', lambda e: e.tensor_tensor(out=dd[:, 0:1], in0=m2, in1=m1, op=ALU.subtract), **J)
            P.op('act', lambda e: e.activation(out=dd[:, 0:1], in_=dd[:, 0:1], func=AF.Exp), **J)
            P.op('dve', lambda e: e.tensor_scalar(out=dd[:, 1:2], in0=dd[:, 0:1], scalar1=1.0, scalar2=None, op0=ALU.add), **J)
            P.op('dve', lambda e: e.reciprocal(out=dd[:, 1:2], in_=dd[:, 1:2]), **J)
            P.op('dve', lambda e: e.tensor_tensor(out=dd[:, 2:3], in0=dd[:, 0:1], in1=dd[:, 1:2], op=ALU.mult), **J)
            P.op('dve', lambda e: e.tensor_tensor(out=RSEL[:, t, :], in0=eq1, in1=eq2, op=ALU.add), reads=[stb], writes=[brt], join=True)
            P.op('dve', lambda e: e.tensor_scalar(out=RWT[:, t, :], in0=eq1, scalar1=dd[:, 1:2], scalar2=None, op0=ALU.mult), reads=[stb], writes=[brt], join=True)
            P.op('dve', lambda e: e.scalar_tensor_tensor(out=RWT[:, t, :], in0=eq2, scalar=dd[:, 2:3], in1=RWT[:, t, :], op0=ALU.mult, op1=ALU.add),
                 reads=[stb, brt], writes=[brt], join=True)
            P.op('dve', lambda e: e.tensor_copy(out=RSELB[:, t, :], in_=RSEL[:, t, :]), reads=[brt], writes=[brt], join=True)

        bq_d, bkn_d, bkr_d, bv_d = Buf('qT_d'), Buf('knT_d'), Buf('krT_d'), Buf('V_d')
        bmix_d = [Buf(f'mix_d{i}') for i in range(4)]
        bxat_d = [Buf(f'xat_d{i}') for i in range(4)]
        batt_d = [Buf(f'att_d{i}') for i in range(4)]
        bg_d = [Buf(f'g_d{i}') for i in range(4)]

        def phase_mixer(l):
            y_src = T['x'] if l == layers[0] else OUT
            w_in = T['w_in'][l]
            hTv = RA[:, 0:16 * S].rearrange("p (c t) -> p c t", t=S)
            hbuf = lambda t: [RAq[t // 4]]

            phase_norm(y_src, S, T['attn_norm_g'][l], 'fm', hTv, hbuf, src_is_out=(l > layers[0]))
            if hT_dbg is not None and l == 0:
                bdb = Buf('hT_dbg')
                for c in range(16):
                    finals.append(store(hT_dbg[c * 128:(c + 1) * 128, :], hTv[:, c, :], RAq[0:4], bdb, join=(c > 0)))
            memnT, memb = rb(32, 8)
            memv = memnT.rearrange("p (c t) -> p c t", t=256)
            phase_norm(T['mem'], 256, T['mem_norm_g'][l], 'fm', memv, lambda t: memb)
            bcast_row(GAIN[:, 0:512], T['mla_q_a_norm_g'][l], [bgain], first=True)
            bcast_row(GAIN[:, 512:768], T['mla_kv_a_norm_g'][l], [bgain], first=False)
            bcast_row(GAIN[:, 768:960], T['mla_q_norm_g'][l], [bgain], first=False)
            bcast_row(GAIN[:, 960:1152], T['mla_k_norm_g'][l], [bgain], first=False)
            bcast_row(GAIN[:, 1152:1408], T['mem_q_norm_g'][l], [bgain], first=False)
            bcast_row(GAIN[:, 1408:1664], T['mem_k_norm_g'][l], [bgain], first=False)
            P.dma('sp', PSC[:], T['pool_scale'][l].rearrange("(c p) -> p c", p=128), writes=[bgain], join=True, allow_slow_non_contiguous=True)
            P.op('dve', lambda e: e.tensor_tensor(out=GAIN[:, 768:896], in0=GAIN[:, 768:896], in1=GAIN[:, 960:1088], op=ALU.mult),
                 reads=[bgain], writes=[bgain], join=True)
            P.op('dve', lambda e: e.tensor_tensor(out=GAIN[:, 1152:1408], in0=GAIN[:, 1152:1408], in1=GAIN[:, 1408:1664], op=ALU.mult),
                 reads=[bgain], writes=[bgain], join=True)
            G_QA, G_KVA, G_Q, G_KPE, G_QM = GAIN[:, 0:512], GAIN[:, 512:768], GAIN[:, 768:960], GAIN[:, 1088:1152], GAIN[:, 1152:1408]

            chk('A')
            kmT, kmb = rb(40, 4)
            kmv = kmT.rearrange("p (c t) -> p c t", t=256)
            vm, vmb = rb(44, 4)
            vmv = vm.rearrange("p (m f) -> p m f", f=1024)
            wkv = T['mem_w_kv'][l]
            for cb in range(8):
                sl, slb = load_slab(wkv, 0, 2048, cb * 256, 256)
                if cb < 4:
                    for c in range(2):
                        ps, psb = nxt('mm', MM)
                        mm_acc(ps[:, 0:256], psb, [(sl[:, j, c * 128:(c + 1) * 128], memv[:, j, :]) for j in range(16)], slb + memb)
                        copy_op(ev_eng(), kmv[:, cb * 2 + c, :], ps[:, 0:256], [psb], kmb, join=True)
                    for mt in range(2):
                        ps, psb = nxt('mm', MM)
                        mm_acc(ps[:, 0:256], psb, [(memv[:, j, mt * 128:(mt + 1) * 128], sl[:, j, :]) for j in range(16)], slb + memb)
                        jk, jkb = rb(16, 4)
                        P.op('act', lambda e, ps=ps, jk=jk, mt=mt, cb=cb: e.activation(out=jk[:, 0:256], in_=ps[:, 0:256], func=AF.Square,
                                                                                    accum_out=SCM[:, mt, cb:cb + 1]), reads=[psb], writes=jkb + [bscm])
                else:
                    for mt in range(2):
                        ps, psb = nxt('mm', MM)
                        mm_acc(ps[:, 0:256], psb, [(memv[:, j, mt * 128:(mt + 1) * 128], sl[:, j, :]) for j in range(16)], slb + memb)
                        copy_op(ev_eng(), vmv[:, mt, (cb - 4) * 256:(cb - 3) * 256], ps[:, 0:256], [psb], vmb, join=True)
            scm2 = SCM[:, :, :].rearrange("p a b -> p (a b)")
            rstd_from_ss(scm2, scm2, 256, [bscm], scale_extra=256 ** -0.5)

            chk('Bmem')
            wuq, wuqb = rb(48, 12)
            wuqv, _ = load_slab(T['mla_w_uq'][l], 0, 512, 0, 1536, dst=(wuq, wuqb))
            wkvA, wkvAb = rb(32, 4)
            wkvAv, _ = load_slab(T['mla_w_ukv'][l], 0, 256, 0, 1024, dst=(wkvA, wkvAb))
            wkvB, wkvBb = rb(36, 4)
            wkvBv, _ = load_slab(T['mla_w_ukv'][l], 0, 256, 1024, 1024, dst=(wkvB, wkvBb))

            def tm_block(t, sl2, w):
                ps, psb = nxt('mm', MM)
                for half, (sl, slb) in enumerate(sl2):
                    for j in range(16):
                        P.op('pe', lambda e, j=j, sl=sl, ps=ps, t=t, half=half: e.matmul(ps[:, half * w:(half + 1) * w], lhsT=hTv[:, j, t * 128:(t + 1) * 128],
                                                                                        rhs=sl[:, j, :], start=(j == 0), stop=(j == 15)),
                             reads=hbuf(t) + slb, writes=[psb], join=not (half == 0 and j == 0))
                return ps, psb

            sl2 = [load_slab(w_in, 0, 2048, 0, 256), load_slab(w_in, 0, 2048, 256, 256)]
            for t in range(NT):
                ps, psb = tm_block(t, sl2, 256)
                stt, stb = nxt('stat', STAT)
                jk, jkb = rb(16, 4)
                P.op('act', lambda e, ps=ps, jk=jk, stt=stt: e.activation(out=jk[:, 0:512], in_=ps[:, :], func=AF.Square, accum_out=stt[:, 0:1]),
                     reads=[psb], writes=jkb + [stb])
                rstd_from_ss(stt[:, 0:1], stt[:, 1:2], 512, [stb])
                cqn, cqnb = rb(20, 4)
                P.op('dve', lambda e, ps=ps, cqn=cqn, stt=stt: e.scalar_tensor_tensor(out=cqn[:, 0:512], in0=ps[:, :], scalar=stt[:, 1:2], in1=G_QA,
                                                                                    op0=ALU.mult, op1=ALU.mult), reads=[psb, stb, bgain], writes=cqnb)
                cqT, cqTb = rb(0, 1)
                cqTv = cqT.rearrange("p (c t) -> p c t", t=128)
                transposes([cqn[:, c * 128:(c + 1) * 128] for c in range(4)], cqnb, lambda q, m: cqTv[:, q:q + m, :], cqTb[0:1])
                qf, qfb = rb(8, 8, F32)
                for nb in range(3):
                    ps2, ps2b = nxt('mm', MM)
                    mm_acc(ps2[:, :], ps2b, [(cqTv[:, j, :], wuqv[:, j, nb * 512:(nb + 1) * 512]) for j in range(4)], cqTb[0:1] + wuqb)
                    copy_op(ev_eng(), qf[:, nb * 512:(nb + 1) * 512], ps2[:, :], [ps2b], qfb, join=True)
                q_epilogue(t, qf, qfb, G_Q)

            chk('Bcq')
            sl2 = [load_slab(w_in, 0, 2048, 512, 160), load_slab(w_in, 0, 2048, 672, 160)]
            for t in range(NT):
                ps, psb = tm_block(t, sl2, 160)
                chk('k0')
                kv_epilogue(t, ps, psb, G_KVA, G_KPE, wkvAv, wkvAb, wkvBv, wkvBb)

            chk('Bckv')
            pwt, pwtb = rb(48, 4)
            pwv = pwt.rearrange("p (g j f) -> p g j f", g=4, j=2)
            for g in range(4):
                P.dma('pool', pwv[:, g, :, :], T['pool_w'][l][g].rearrange("(j p) f -> p j f", p=128), writes=pwtb, join=(g > 0))
            for ub in range(2):
                sl2 = [load_slab(w_in, 0, 2048, 832 + ub * 512, 256), load_slab(w_in, 0, 2048, 832 + ub * 512 + 256, 256)]
                for t in range(NT):
                    ps, psb = tm_block(t, sl2, 256)
                    ucur, ucb = rb(0 + (t % 2), 1)
                    copy_op(ev_eng(), ucur[:, 0:512], ps[:, :], [psb], ucb[0:1])
                    uprev, upb = rb(0 + ((t + 1) % 2), 1)
                    pp, ppb = nxt('mm', MM)
                    for gg in range(2):
                        g = 2 * ub + gg
                        pairs = [(poolm[:, 3 * g + (1 if t > 0 else 0), :], ucur[:, gg * 256:(gg + 1) * 256])]
                        if t > 0:
                            pairs.append((poolm[:, 3 * g + 2, :], uprev[:, gg * 256:(gg + 1) * 256]))
                        n = len(pairs)
                        for i, (lh, rh) in enumerate(pairs):
                            P.op('pe', lambda e, lh=lh, rh=rh, i=i, n=n, pp=pp, gg=gg: e.matmul(pp[:, gg * 256:(gg + 1) * 256], lhsT=lh, rhs=rh, start=(i == 0), stop=(i == n - 1)),
                                 reads=ucb[0:1] + upb[0:1] + [bconst], writes=[ppb], join=not (gg == 0 and i == 0))
                    pl, plb = rb(2, 1)
                    copy_op(ev_eng(), pl[:, 0:512], pp[:, :], [ppb], plb[0:1])
                    plT, plTb = rb(3, 1)
                    plTv = plT.rearrange("p (c t) -> p c t", t=128)
                    transposes([pl[:, c * 128:(c + 1) * 128] for c in range(4)], plb[0:1], lambda q, m: plTv[:, q:q + m, :], plTb[0:1])
                    pm_, pmb = nxt('mm', MM)
                    for gg in range(2):
                        g = 2 * ub + gg
                        for hc in range(2):
                            for j in range(2):
                                P.op('pe', lambda e, g=g, gg=gg, hc=hc, j=j, pm_=pm_: e.matmul(pm_[:, (gg * 2 + hc) * 128:(gg * 2 + hc + 1) * 128], lhsT=pwv[:, g, j, hc * 128:(hc + 1) * 128],
                                                                                            rhs=plTv[:, gg * 2 + j, :], start=(j == 0), stop=(j == 1)),
                                     reads=plTb[0:1] + pwtb, writes=[pmb], join=not (gg == 0 and hc == 0 and j == 0))
                    mx, mxb = rb(4 + (t % 2), 1)
                    mxv = mx.rearrange("p (c t) -> p c t", t=128)
                    for cc in range(4):
                        P.op('dve', lambda e, cc=cc, mxv=mxv, pm_=pm_, ub=ub: e.tensor_scalar(out=mxv[:, cc, :], in0=pm_[:, cc * 128:(cc + 1) * 128],
                                                                                             scalar1=PSC[:, ub * 4 + cc:ub * 4 + cc + 1], scalar2=None, op0=ALU.mult),
                             reads=[pmb, bgain], writes=mxb[0:1], join=(cc > 0))
                    store(mixT_d[ub * 512:(ub + 1) * 512, t * 128:(t + 1) * 128].rearrange("(c p) t -> p c t", p=128), mxv, mxb[0:1], bmix_d[t // 4])

            chk('Bu')
            for qb in range(2):
                sl2 = [load_slab(w_in, 0, 2048, 1856 + qb * 512, 256), load_slab(w_in, 0, 2048, 1856 + qb * 512 + 256, 256)]
                for t in range(NT):
                    ps, psb = tm_block(t, sl2, 256)
                    stt, stb = nxt('stat', STAT)
                    jk, jkb = rb(16, 4)
                    for hh in range(2):
                        P.op('act', lambda e, ps=ps, jk=jk, stt=stt, hh=hh: e.activation(out=jk[:, hh * 256:(hh + 1) * 256], in_=ps[:, hh * 256:(hh + 1) * 256], func=AF.Square,
                                                                                        accum_out=stt[:, hh:hh + 1]), reads=[psb], writes=jkb + [stb], join=(hh > 0))
                    rstd_from_ss(stt[:, 0:2], stt[:, 2:4], 256, [stb])
                    qmn, qmnb = rb(20, 4)
                    for hh in range(2):
                        P.op('dve', lambda e, ps=ps, qmn=qmn, stt=stt, hh=hh: e.scalar_tensor_tensor(out=qmn[:, hh * 256:(hh + 1) * 256], in0=ps[:, hh * 256:(hh + 1) * 256],
                                                                                                 scalar=stt[:, 2 + hh:3 + hh], in1=G_QM, op0=ALU.mult, op1=ALU.mult),
                             reads=[psb, stb, bgain], writes=qmnb, join=(hh > 0))
                    qmT, qmTb = rb(0, 1)
                    qmTv = qmT.rearrange("p (c t) -> p c t", t=128)
                    transposes([qmn[:, c * 128:(c + 1) * 128] for c in range(4)], qmnb, lambda q, m: qmTv[:, q:q + m, :], qmTb[0:1])
                    xo, xob = rb(4 + (t % 2), 1)
                    xov = xo.rearrange("p (c t) -> p c t", t=128)
                    for hh in range(2):
                        h = 2 * qb + hh
                        pT, pTb = rb(2, 1)
                        pTv = pT[:, 0:256].rearrange("p (m t) -> p m t", t=128)
                        for mt in range(2):
                            ps2, ps2b = nxt('mm', MM)
                            mm_acc(ps2[:, 0:128], ps2b, [(kmv[:, 2 * h + dd, mt * 128:(mt + 1) * 128], qmTv[:, 2 * hh + dd, :]) for dd in range(2)], kmb + qmTb[0:1])
                            P.op('act', lambda e, ps2=ps2, pTv=pTv, mt=mt, h=h: e.activation(out=pTv[:, mt, :], in_=ps2[:, 0:128], func=AF.Exp, scale=SCM[:, mt, h:h + 1]),
                                 reads=[ps2b, bscm], writes=pTb[0:1], join=(mt > 0))
                        ps3, ps3b = nxt('mm', MM)
                        first = True
                        for dv in range(2):
                            for mt in range(2):
                                P.op('pe', lambda e, dv=dv, mt=mt, h=h, ps3=ps3, pTv=pTv: e.matmul(ps3[:, dv * 128:(dv + 1) * 128], lhsT=vmv[:, mt, h * 256 + dv * 128:h * 256 + (dv + 1) * 128],
                                                                                              rhs=pTv[:, mt, :], start=(mt == 0), stop=(mt == 1)),
                                     reads=vmb + pTb[0:1], writes=[ps3b], join=not first)
                                first = False
                        for mt in range(2):
                            P.op('pe', lambda e, mt=mt, ps3=ps3, pTv=pTv: e.matmul(ps3[:, 256:384], lhsT=onesb[:], rhs=pTv[:, mt, :], start=(mt == 0), stop=(mt == 1)),
                                 reads=pTb[0:1] + [bconst], writes=[ps3b], join=True)
                        rc, rcb = rb(3, 1, F32)
                        P.op('dve', lambda e, rc=rc, ps3=ps3: e.reciprocal(out=rc[:, 0:128], in_=ps3[:, 256:384]), reads=[ps3b], writes=rcb[0:1])
                        for dv in range(2):
                            P.op('dve', lambda e, dv=dv, hh=hh, xov=xov, ps3=ps3, rc=rc: e.tensor_tensor(out=xov[:, hh * 2 + dv, :], in0=ps3[:, dv * 128:(dv + 1) * 128], in1=rc[:, 0:128], op=ALU.mult),
                                 reads=[ps3b] + rcb[0:1], writes=xob[0:1], join=not (hh == 0 and dv == 0))
                    store(xattT_d[qb * 512:(qb + 1) * 512, t * 128:(t + 1) * 128].rearrange("(c p) t -> p c t", p=128), xov, xob[0:1], bxat_d[t // 4])

            chk('Bqm')
            for cb in range(24):
                sl, slb = load_slab(w_in, 0, 2048, 2880 + cb * 256, 256)
                for c in range(2):
                    for qt in range(4):
                        ps, psb = nxt('mm', MM)
                        mm_acc(ps[:, :], psb, [(sl[:, j, c * 128:(c + 1) * 128], hTv[:, j, qt * 512:(qt + 1) * 512]) for j in range(16)], slb + [RAq[qt]])
                        sg, sgb = rb(16 + (rr.get('sg', 0) % 4), 1)
                        rr['sg'] = rr.get('sg', 0) + 1
                        P.op('act', lambda e, sg=sg, ps=ps: e.activation(out=sg[:, 0:512], in_=ps[:, :], func=AF.Sigmoid), reads=[psb], writes=sgb[0:1])
                        r0 = cb * 256 + c * 128
                        store(gT_d[r0:r0 + 128, qt * 512:(qt + 1) * 512], sg[:, 0:512], sgb[0:1], bg_d[qt])

            chk('C')
            phase_attention()

            chk('D')
            mrgv = RA[:, 0:16 * S].rearrange("p (c t) -> p c t", t=S)
            wouts = (T['mla_w_out'][l], T['pool_w_out'][l], T['mem_w_out'][l])
            srcs = (attT_d, mixT_d, xattT_d)
            sbufs = (batt_d, bmix_d, bxat_d)
            for qt in range(4):
                xs = []
                for b in range(3):
                    xt_, xtb = rb(8 * b, 8)
                    xv = xt_.rearrange("p (c t) -> p c t", t=512)
                    P.dma('sp', xv, srcs[b][:, qt * 512:(qt + 1) * 512].rearrange("(c p) t -> p c t", p=128), reads=[sbufs[b][qt]], writes=xtb)
                    xs.append((xv, xtb))
                for cb in range(4):
                    acc, accb = rb(24, 8, F32)
                    accv = acc.rearrange("p (c t) -> p c t", t=512)
                    for b in range(3):
                        sl, slb = load_slab(wouts[b], 0, 1024, cb * 512, 512)
                        gt, gtb = rb(32 + 4 * (b % 2), 4)
                        gtv = gt.rearrange("p (c t) -> p c t", t=512)
                        r0 = b * 2048 + cb * 512
                        P.dma('sp', gtv, gT_d[r0:r0 + 512, qt * 512:(qt + 1) * 512].rearrange("(c p) t -> p c t", p=128), reads=[bg_d[qt]], writes=gtb)
                        for c in range(4):
                            ps, psb = nxt('mm', MM)
                            mm_acc(ps[:, :], psb, [(sl[:, j, c * 128:(c + 1) * 128], xs[b][0][:, j, :]) for j in range(8)], slb + xs[b][1])
                            if b == 0:
                                P.op('dve', lambda e, c=c, ps=ps, accv=accv, gtv=gtv: e.tensor_tensor(out=accv[:, c, :], in0=ps[:, :], in1=gtv[:, c, :], op=ALU.mult),
                                     reads=[psb] + gtb, writes=accb, join=(c > 0))
                            else:
                                tmp, tmpb = rb(40 + 2 * (c % 2), 2, F32)
                                P.op('dve', lambda e, c=c, ps=ps, tmp=tmp, gtv=gtv: e.tensor_tensor(out=tmp[:, 0:512], in0=ps[:, :], in1=gtv[:, c, :], op=ALU.mult),
                                     reads=[psb] + gtb, writes=tmpb)
                                if b == 1:
                                    P.op('pool', lambda e, c=c, accv=accv, tmp=tmp: e.tensor_tensor(out=accv[:, c, :], in0=accv[:, c, :], in1=tmp[:, 0:512], op=ALU.add),
                                         reads=accb + tmpb, writes=accb, join=True)
                                else:
                                    P.op('pool', lambda e, c=c, cb=cb, qt=qt, accv=accv, tmp=tmp: e.tensor_tensor(out=mrgv[:, cb * 4 + c, qt * 512:(qt + 1) * 512], in0=accv[:, c, :],
                                                                                                                 in1=tmp[:, 0:512], op=ALU.add),
                                         reads=accb + tmpb, writes=[RAq[qt]], join=True)
            if mrg_dbg is not None and l == 0:
                bdb = Buf('mrg_dbg')
                for c in range(16):
                    finals.append(store(mrg_dbg[c * 128:(c + 1) * 128, :], mrgv[:, c, :], RAq[0:4], bdb, join=(c > 0)))

            chk('E')
            wo = T['w_o'][l]
            for cb in range(8):
                sl, slb = load_slab(wo, 0, 2048, cb * 256, 256)
                for t in range(NT):
                    ps, psb = nxt('mm', MM)
                    mm_acc(ps[:, 0:256], psb, [(mrgv[:, j, t * 128:(t + 1) * 128], sl[:, j, :]) for j in range(16)], slb + [RAq[t // 4]])
                    yt, ytb = rb(44 + (rr.get('yt', 0) % 4), 1, F32)
                    rr['yt'] = rr.get('yt', 0) + 1
                    P.dma('sp', yt[:, 0:256], y_src[t * 128:(t + 1) * 128, cb * 256:(cb + 1) * 256], reads=[ybuf[t]] if l > layers[0] else [], writes=ytb[0:1])
                    P.op('dve', lambda e, yt=yt, ps=ps: e.tensor_tensor(out=yt[:, 0:256], in0=ps[:, 0:256], in1=yt[:, 0:256], op=ALU.add), reads=[psb] + ytb[0:1], writes=ytb[0:1])
                    o = store(OUT[t * 128:(t + 1) * 128, cb * 256:(cb + 1) * 256], yt[:, 0:256], ytb[0:1], ybuf[t], join=(l == layers[0] and cb > 0))
                    finals.append(o)

        def q_epilogue(t, qf, qfb, G_Q):
            stt, stb = nxt('stat', STAT)
            qv = qf[:, 0:1536].rearrange("p (h d) -> p h d", d=192)
            sq, sqb = rb(24, 8, F32)
            sqv = sq[:, 0:1536].rearrange("p (h d) -> p h d", d=192)
            P.op('act', lambda e: e.activation(out=sq[:, 0:1536], in_=qf[:, 0:1536], func=AF.Square), reads=qfb, writes=sqb)
            P.op('dve', lambda e: e.tensor_reduce(out=stt[:, 0:8], in_=sqv, axis=AX.X, op=ALU.add), reads=sqb, writes=[stb])
            rstd_from_ss(stt[:, 0:8], stt[:, 8:16], 192, [stb])
            P.op('dve', lambda e: e.tensor_tensor(out=sqv, in0=qv, in1=stt[:, 8:16].unsqueeze(2).broadcast_to([128, 8, 192]), op=ALU.mult),
                 reads=qfb + [stb], writes=sqb)
            P.op('dve', lambda e: e.tensor_tensor(out=sqv, in0=sqv, in1=G_Q.unsqueeze(1).broadcast_to([128, 8, 192]), op=ALU.mult),
                 reads=sqb + [bgain], writes=sqb)
            qb_, qbb = rb(20, 4)
            qbn = qb_[:, 0:1024].rearrange("p (h d) -> p h d", d=128)
            qbr = qb_[:, 1024:1536].rearrange("p (h d) -> p h d", d=64)
            P.op('act', lambda e: e.activation(out=qbn, in_=sqv[:, :, 0:128], func=AF.Copy), reads=sqb, writes=qbb)
            x1, x2 = sqv[:, :, 128:160], sqv[:, :, 160:192]
            cb_ = COS[:, t, :].unsqueeze(1).broadcast_to([128, 8, 32])
            sb_ = SIN[:, t, :].unsqueeze(1).broadcast_to([128, 8, 32])
            tm, tmb = rb(60, 4, F32)
            t1 = tm[:, 0:256].rearrange("p (h d) -> p h d", d=32)
            t2 = tm[:, 256:512].rearrange("p (h d) -> p h d", d=32)
            P.op('pool', lambda e: e.tensor_tensor(out=t1, in0=x1, in1=cb_, op=ALU.mult), reads=sqb + [bcs], writes=tmb)
            P.op('pool', lambda e: e.tensor_tensor(out=t2, in0=x2, in1=sb_, op=ALU.mult), reads=sqb + [bcs], writes=tmb, join=True)
            P.op('pool', lambda e: e.tensor_tensor(out=qbr[:, :, 0:32], in0=t1, in1=t2, op=ALU.subtract), reads=tmb, writes=qbb, join=True)
            P.op('dve', lambda e: e.tensor_tensor(out=t1, in0=x2, in1=cb_, op=ALU.mult), reads=sqb + [bcs] + qbb, writes=tmb)
            P.op('dve', lambda e: e.tensor_tensor(out=t2, in0=x1, in1=sb_, op=ALU.mult), reads=sqb + [bcs], writes=tmb, join=True)
            P.op('dve', lambda e: e.tensor_tensor(out=qbr[:, :, 32:64], in0=t1, in1=t2, op=ALU.add), reads=tmb, writes=qbb, join=True)
            qs, qsb = rb(1, 2)
            qsn = qs[:, 0:1024].rearrange("p (h t) -> p h t", t=128)
            qsr_, qsrb = rb(5, 2)
            qsr = qsr_[:, 0:1024].rearrange("p (h t) -> p h t", t=128)
            transposes([qbn[:, h, :] for h in range(8)], qbb, lambda q, m: qsn[:, q:q + m, :], qsb)
            transposes([qbr[:, h, :] for h in range(8)], qbb, lambda q, m: qsr[0:64, q:q + m, :], qsrb, width=64)
            store(qnT_d[:, t * 128:(t + 1) * 128].rearrange("(h p) t -> p h t", p=128), qsn, qsb, bq_d, join=True)
            store(qrT_d[:, t * 128:(t + 1) * 128].rearrange("(h p) t -> p h t", p=64), qsr[0:64, :, :], qsrb, bq_d, join=True)

        def kv_epilogue(t, ps, psb, G_KVA, G_KPE, wkvAv, wkvAb, wkvBv, wkvBb):
            stt, stb = nxt('stat', STAT)
            jk, jkb = rb(16, 4)
            P.op('act', lambda e: e.activation(out=jk[:, 0:256], in_=ps[:, 0:256], func=AF.Square, accum_out=stt[:, 0:1]), reads=[psb], writes=jkb + [stb])
            P.op('act', lambda e: e.activation(out=jk[:, 256:320], in_=ps[:, 256:320], func=AF.Square, accum_out=stt[:, 2:3]), reads=[psb], writes=jkb + [stb], join=True)
            rstd_from_ss(stt[:, 0:1], stt[:, 1:2], 256, [stb])
            ckn, cknb = rb(20, 4)
            P.op('dve', lambda e: e.scalar_tensor_tensor(out=ckn[:, 0:256], in0=ps[:, 0:256], scalar=stt[:, 1:2], in1=G_KVA, op0=ALU.mult, op1=ALU.mult),
                 reads=[psb, stb, bgain], writes=cknb)
            chk('k1')
            kp, kpb = rb(60, 4, F32)
            P.op('dve', lambda e: e.tensor_tensor(out=kp[:, 0:64], in0=ps[:, 256:320], in1=G_KPE, op=ALU.mult), reads=[psb, bgain], writes=kpb)
            x1, x2 = kp[:, 0:32], kp[:, 32:64]
            c_, s_ = COS[:, t, :], SIN[:, t, :]
            t1, t2, t3, t4 = kp[:, 64:96], kp[:, 96:128], kp[:, 128:160], kp[:, 160:192]
            kr = ckn[:, 256:320]
            P.op('dve', lambda e: e.tensor_tensor(out=t1, in0=x1, in1=c_, op=ALU.mult), reads=kpb + [bcs], writes=kpb, join=True)
            P.op('dve', lambda e: e.tensor_tensor(out=t2, in0=x2, in1=s_, op=ALU.mult), reads=kpb + [bcs], writes=kpb, join=True)
            P.op('dve', lambda e: e.tensor_tensor(out=t3, in0=x2, in1=c_, op=ALU.mult), reads=kpb + [bcs], writes=kpb, join=True)
            P.op('dve', lambda e: e.tensor_tensor(out=t4, in0=x1, in1=s_, op=ALU.mult), reads=kpb + [bcs], writes=kpb, join=True)
            P.op('dve', lambda e: e.tensor_tensor(out=kr[:, 0:32], in0=t1, in1=t2, op=ALU.subtract), reads=kpb, writes=cknb, join=True)
            P.op('dve', lambda e: e.tensor_tensor(out=kr[:, 32:64], in0=t3, in1=t4, op=ALU.add), reads=kpb, writes=cknb, join=True)
            chk('k2')
            ckT, ckTb = rb(0, 1)
            ckTv = ckT[:, 0:256].rearrange("p (c t) -> p c t", t=128)
            transposes([ckn[:, c * 128:(c + 1) * 128] for c in range(2)], cknb, lambda q, m: ckTv[:, q:q + m, :], ckTb[0:1])
            krs, krsb = rb(1 + (t % 2), 1)
            krv = krs[:, 0:128].rearrange("p (c t) -> p c t", t=128)
            transposes([kr], cknb, lambda q, m: krv[0:64, q:q + m, :], krsb[0:1], width=64)
            store(krT_d[:, t * 128:(t + 1) * 128], krs[0:64, 0:128], krsb[0:1], bkr_d, join=True)
            chk('k3')
            vst, vstb = rb(3 + (t % 2) * 2, 2)
            kns, knsb = rb(8 + (t % 2) * 2, 2)
            sq, sqb = rb(24, 8, F32)
            for nb in range(4):
                wv, wb = (wkvAv, wkvAb) if nb < 2 else (wkvBv, wkvBb)
                ps2, ps2b = nxt('mm', MM)
                mm_acc(ps2[:, :], ps2b, [(ckTv[:, j, :], wv[:, j, (nb % 2) * 512:(nb % 2 + 1) * 512]) for j in range(2)], ckTb[0:1] + wb)
                for hh in range(2):
                    hd = 2 * nb + hh
                    kcol = ps2[:, hh * 256:hh * 256 + 128]
                    vcol = ps2[:, hh * 256 + 128:hh * 256 + 256]
                    fj = not (nb == 0 and hh == 0)
                    P.op('act', lambda e, kcol=kcol, hd=hd: e.activation(out=kns[:, hd * 128:(hd + 1) * 128], in_=kcol, func=AF.Copy),
                         reads=[ps2b], writes=knsb, join=fj)
                    P.op('dve', lambda e, vcol=vcol, hd=hd: e.tensor_copy(out=vst[:, hd * 128:(hd + 1) * 128], in_=vcol),
                         reads=[ps2b], writes=vstb, join=fj)
                    P.op('act', lambda e, kcol=kcol, hd=hd: e.activation(out=sq[:, hd * 128:(hd + 1) * 128], in_=kcol, func=AF.Square),
                         reads=[ps2b], writes=sqb, join=fj)
            chk('k4')
            store(V_d[t * 128:(t + 1) * 128, :], vst[:, 0:1024], vstb, bv_d, join=True)
            P.op('dve', lambda e: e.tensor_reduce(out=stt[:, 8:16], in_=sq[:, 0:1024].rearrange("p (h d) -> p h d", d=128), axis=AX.X, op=ALU.add), reads=sqb, writes=[stb], join=True)
            P.op('dve', lambda e: e.tensor_scalar(out=stt[:, 8:16], in0=stt[:, 8:16], scalar1=stt[:, 2:3], scalar2=None, op0=ALU.add), reads=[stb], writes=[stb], join=True)
            rstd_from_ss(stt[:, 8:16], stt[:, 8:16], 192, [stb], scale_extra=192 ** -0.5)
            P.op('dve', lambda e: e.tensor_copy(out=SCK[:, t, :], in_=stt[:, 8:16]), reads=[stb], writes=[bsck], join=True)
            chk('k5')
            knT, knTb = rb(12 + (t % 2) * 2, 2)
            knTv = knT[:, 0:1024].rearrange("p (h t) -> p h t", t=128)
            transposes([kns[:, h * 128:(h + 1) * 128] for h in range(8)], knsb, lambda q, m: knTv[:, q:q + m, :], knTb)
            store(knT_d[:, t * 128:(t + 1) * 128].rearrange("(h p) t -> p h t", p=128), knTv, knTb, bkn_d, join=True)

        def phase_attention():
            krT, krTb = rb(0, 4)
            P.dma('sp', krT[0:64, :], krT_d[:, :], reads=[bkr_d], writes=krTb)
            for h in range(8):
                o = 4 + (h % 2) * 16
                qn, qnb = rb(o, 4)
                qr, qrb = rb(o + 4, 4)
                kn, knb = rb(o + 8, 4)
                vh, vhb = rb(o + 12, 4)
                vhv = vh.rearrange("p (t d) -> p t d", d=128)
                P.dma('sp', qn, qnT_d[h * 128:(h + 1) * 128, :], reads=[bq_d], writes=qnb)
                P.dma('sp', qr[0:64, :], qrT_d[h * 64:(h + 1) * 64, :], reads=[bq_d], writes=qrb)
                P.dma('sp', kn, knT_d[h * 128:(h + 1) * 128, :], reads=[bkn_d], writes=knb)
                for hf_ in range(2):
                    P.dma('sp', vhv[:, hf_ * 8:(hf_ + 1) * 8, :], V_d[hf_ * 1024:(hf_ + 1) * 1024, h * 128:(h + 1) * 128].rearrange("(t p) d -> p t d", p=128),
                          reads=[bv_d], writes=vhb, join=(hf_ > 0))
                for qt in range(4):
                    nk = 4 * (qt + 1)
                    num, numb = MM[4]
                    den, denb = AUX
                    qs = slice(qt * 512, (qt + 1) * 512)
                    for kt in range(nk):
                        ps, psb = nxt('mmA', MM, 4)
                        ks = slice(kt * 128, (kt + 1) * 128)
                        P.op('pe', lambda e, ps=ps, ks=ks, qs=qs, kn=kn, qn=qn: e.matmul(ps[:, :], lhsT=kn[:, ks], rhs=qn[:, qs], start=True, stop=False),
                             reads=knb + qnb, writes=[psb])
                        P.op('pe', lambda e, ps=ps, ks=ks, qs=qs, qr=qr: e.matmul(ps[:, :], lhsT=krT[0:64, ks], rhs=qr[0:64, qs], start=False, stop=True),
                             reads=krTb + qrb, writes=[psb], join=True)
                        pT, pTb = rb(36 + (rr.get('pT', 0) % 4), 1)
                        rr['pT'] = rr.get('pT', 0) + 1
                        P.op('act', lambda e, ps=ps, pT=pT, kt=kt, h=h: e.activation(out=pT[:, 0:512], in_=ps[:, :], func=AF.Exp, scale=SCK[:, kt, h:h + 1]),
                             reads=[psb, bsck], writes=pTb[0:1])
                        if kt >= 4 * qt:
                            P.op('dve', lambda e, pT=pT, kt=kt, qt=qt: e.tensor_tensor(out=pT[:, 0:512], in0=pT[:, 0:512], in1=maskb[:, kt - 4 * qt, :], op=ALU.mult),
                                 reads=pTb[0:1] + [bconst], writes=pTb[0:1])
                        P.op('pe', lambda e, pT=pT, kt=kt, nk=nk, vhv=vhv, num=num: e.matmul(num[:, :], lhsT=vhv[:, kt, :], rhs=pT[:, 0:512], start=(kt == 0), stop=(kt == nk - 1)),
                             reads=vhb + pTb[0:1], writes=[numb], join=(kt > 0))
                        P.op('pe', lambda e, pT=pT, kt=kt, nk=nk, den=den: e.matmul(den[:, :], lhsT=onesb[:], rhs=pT[:, 0:512], start=(kt == 0), stop=(kt == nk - 1)),
                             reads=pTb[0:1] + [bconst], writes=[denb], join=(kt > 0))
                    rc, rcb = rb(40, 2, F32)
                    P.op('dve', lambda e, rc=rc, den=den: e.reciprocal(out=rc[:, 0:512], in_=den[:, :]), reads=[denb], writes=rcb)
                    ao, aob = rb(42 + (qt % 2), 1)
                    P.op('dve', lambda e, ao=ao, num=num, rc=rc: e.tensor_tensor(out=ao[:, 0:512], in0=num[:, :], in1=rc[:, 0:512], op=ALU.mult), reads=[numb] + rcb, writes=aob[0:1])
                    store(attT_d[h * 128:(h + 1) * 128, qs], ao[:, 0:512], aob[0:1], batt_d[qt])

        def phase_dense_ffn(l):
            h2v = RA[:, 0:16 * S].rearrange("p (c t) -> p c t", t=S)
            phase_norm(OUT, S, T['ffn_norm_g'][l], 'fm', h2v, lambda t: [RAq[t // 4]], src_is_out=True)
            wg, wu, wd = T['dense_w_gate'][0], T['dense_w_up'][0], T['dense_w_down'][0]
            aT, aTb = rb(0, 44)
            aTv = aT.rearrange("p (f t) -> p f t", t=512)
            for qt in range(4):
                for fs in range(22):
                    sg_, sgb = load_slab(wg, 0, 2048, fs * 256, 256)
                    su_, sub = load_slab(wu, 0, 2048, fs * 256, 256)
                    for c in range(2):
                        pg, pgb = nxt('mm', MM)
                        pu, pub = nxt('mm', MM)
                        mm_acc(pg[:, :], pgb, [(sg_[:, j, c * 128:(c + 1) * 128], h2v[:, j, qt * 512:(qt + 1) * 512]) for j in range(16)], sgb + [RAq[qt]])
                        mm_acc(pu[:, :], pub, [(su_[:, j, c * 128:(c + 1) * 128], h2v[:, j, qt * 512:(qt + 1) * 512]) for j in range(16)], sub + [RAq[qt]])
                        sl_, slb = rb(44 + 2 * (rr.get('silu', 0) % 2), 2, F32)
                        rr['silu'] = rr.get('silu', 0) + 1
                        P.op('act', lambda e, sl_=sl_, pg=pg: e.activation(out=sl_[:, 0:512], in_=pg[:, :], func=AF.Silu), reads=[pgb], writes=slb)
                        P.op('dve', lambda e, sl_=sl_, pu=pu, fs=fs, c=c: e.tensor_tensor(out=aTv[:, fs * 2 + c, :], in0=sl_[:, 0:512], in1=pu[:, :], op=ALU.mult),
                             reads=slb + [pub], writes=aTb, join=True)
                for cb in range(4):
                    accs = [MM[i] for i in range(4)]
                    for fr in range(0, 44, 8):
                        nf = min(8, 44 - fr)
                        sd, sdb = load_slab(wd, fr * 128, nf * 128, cb * 512, 512)
                        for fc in range(nf):
                            f = fr + fc
                            for tt in range(4):
                                ps, psb = accs[tt]
                                P.op('pe', lambda e, ps=ps, f=f, tt=tt, sd=sd, fc=fc: e.matmul(ps[:, :], lhsT=aTv[:, f, tt * 128:(tt + 1) * 128], rhs=sd[:, fc, :], start=(f == 0), stop=(f == 43)),
                                     reads=aTb + sdb, writes=[psb], join=(f > 0))
                    for tt in range(4):
                        t = qt * 4 + tt
                        ps, psb = accs[tt]
                        yt, ytb = rb(48 + 2 * (rr.get('yt2', 0) % 4), 2, F32)
                        rr['yt2'] = rr.get('yt2', 0) + 1
                        P.dma('sp', yt[:, 0:512], OUT[t * 128:(t + 1) * 128, cb * 512:(cb + 1) * 512], reads=[ybuf[t]], writes=ytb)
                        P.op('dve', lambda e, yt=yt, ps=ps: e.tensor_tensor(out=yt[:, 0:512], in0=ps[:, :], in1=yt[:, 0:512], op=ALU.add), reads=[psb] + ytb, writes=ytb)
                        finals.append(store(OUT[t * 128:(t + 1) * 128, cb * 512:(cb + 1) * 512], yt[:, 0:512], ytb, ybuf[t], join=False))

        def phase_moe(l):
            P.dma('sp', RW[:], T['router_w'][0].rearrange("(j p) e -> p j e", p=128), writes=[brw])
            bcast_row(RBI[:], T['router_b'][0], [brw], first=False)
            phase_norm(OUT, S, T['ffn_norm_g'][l], 'tm', src_is_out=True, router=True)
            for m in range(NT):
                ps, psb = AUX
                pairs = [(onesb[:], RSELB[:, k, :]) for k in range(m)] + [(trib[:], RSELB[:, m, :])]
                mm_acc(ps[:, 0:8], psb, pairs, [brt, bconst])
                P.op('dve', lambda e, m=m, ps=ps: e.tensor_copy(out=RPOS[:, m, :], in_=ps[:, 0:8]), reads=[psb], writes=[brt], join=True)
            if rt_dbg is not None:
                for i, src in enumerate((RSEL, RWT, RPOS)):
                    finals.append(store(rt_dbg[:, i * 128:(i + 1) * 128], src[:, :, :].rearrange("p a b -> p (a b)"), [brt], Buf(f'rt_dbg{i}'), join=False))
            aT = RA[:, 0:56 * CAP]
            aTv = aT.rearrange("p (f s) -> p f s", s=CAP)
            aTb = RAq
            GT, GTb = rb(0, 24)
            GTv = GT.rearrange("p (j s) -> p j s", s=CAP)
            OEv = GT.rearrange("p (s f) -> p s f", f=2048)
            PE_, PEb = rb(24, 24)
            PEv = PE_.rearrange("p (k s) -> p k s", s=CAP)
            HALF = CAP // 2
            for e_ in range(8):
                wg, wu, wd = T['moe_w_gate'][0][e_], T['moe_w_up'][0][e_], T['moe_w_down'][0][e_]
                for k in range(NT):
                    eng = 'dve' if k % 2 == 0 else 'pool'
                    P.op(eng, lambda e, k=k, e_=e_: e.tensor_scalar(out=PEv[:, k, :], in0=iota[:], scalar1=RPOS[:, k, e_:e_ + 1], scalar2=RSEL[:, k, e_:e_ + 1],
                                                                    op0=ALU.is_equal, op1=ALU.mult), reads=[brt, bconst], writes=PEb, join=(k > 0))
                for j in range(16):
                    hs, hsb = rb(48 + 4 * (j % 2), 4)
                    hsv = hs.rearrange("p (k f) -> p k f", f=128)
                    P.dma('sp', hsv, htm_d[:, j * 128:(j + 1) * 128].rearrange("(k p) f -> p k f", p=128), reads=[bhtm], writes=hsb)
                    for half in range(2):
                        ps, psb = nxt('mm', MM)
                        mm_acc(ps[:, 0:HALF], psb, [(hsv[:, k, :], PEv[:, k, half * HALF:(half + 1) * HALF]) for k in range(NT)], hsb + PEb)
                        copy_op(ev_eng(), GTv[:, j, half * HALF:(half + 1) * HALF], ps[:, 0:HALF], [psb], GTb, join=True)
                for fs in range(28):
                    sg_, sgb = load_slab(wg, 0, 2048, fs * 256, 256)
                    su_, sub = load_slab(wu, 0, 2048, fs * 256, 256)
                    for c in range(2):
                        for half in range(2):
                            pg, pgb = nxt('mm', MM)
                            pu, pub = nxt('mm', MM)
                            hs_ = slice(half * HALF, (half + 1) * HALF)
                            mm_acc(pg[:, 0:HALF], pgb, [(sg_[:, j, c * 128:(c + 1) * 128], GTv[:, j, hs_]) for j in range(16)], sgb + GTb)
                            mm_acc(pu[:, 0:HALF], pub, [(su_[:, j, c * 128:(c + 1) * 128], GTv[:, j, hs_]) for j in range(16)], sub + GTb)
                            sl_, slb = rb(56 + 2 * (rr.get('silu', 0) % 2), 2, F32)
                            rr['silu'] = rr.get('silu', 0) + 1
                            P.op('act', lambda e, sl_=sl_, pg=pg: e.activation(out=sl_[:, 0:HALF], in_=pg[:, 0:HALF], func=AF.Silu), reads=[pgb], writes=slb)
                            P.op('dve', lambda e, sl_=sl_, pu=pu, fs=fs, c=c, hs_=hs_: e.tensor_tensor(out=aTv[:, fs * 2 + c, hs_], in0=sl_[:, 0:HALF], in1=pu[:, 0:HALF], op=ALU.mult),
                                 reads=slb + [pub], writes=aTb, join=True)
                for cb in range(4):
                    accs = [MM[i] for i in range(5)] + [AUX]
                    for fr in range(0, 56, 8):
                        sd, sdb = load_slab(wd, fr * 128, 8 * 128, cb * 512, 512)
                        for fc in range(8):
                            f = fr + fc
                            for s_ in range(NST):
                                ps, psb = accs[s_]
                                P.op('pe', lambda e, ps=ps, f=f, s_=s_, sd=sd, fc=fc: e.matmul(ps[:, :], lhsT=aTv[:, f, s_ * 128:(s_ + 1) * 128], rhs=sd[:, fc, :], start=(f == 0), stop=(f == 55)),
                                     reads=aTb + sdb, writes=[psb], join=(f > 0))
                    for s_ in range(NST):
                        ps, psb = accs[s_]
                        copy_op(ev_eng(), OEv[:, s_, cb * 512:(cb + 1) * 512], ps[:, :], [psb], GTb, join=True)
                for m in range(NT):
                    pwT, pwTb = rb(56 + 2 * (m % 2), 2)
                    pwTv = pwT[:, 0:NST * 128].rearrange("p (s t) -> p s t", t=128)
                    transposes([PEv[:, m, s_ * 128:(s_ + 1) * 128] for s_ in range(NST)], PEb, lambda q, mm_: pwTv[:, q:q + mm_, :], pwTb)
                    for cb in range(4):
                        ps, psb = nxt('mm', MM)
                        mm_acc(ps[:, :], psb, [(pwTv[:, s_, :], OEv[:, s_, cb * 512:(cb + 1) * 512]) for s_ in range(NST)], pwTb + GTb)
                        sc, scb = rb(60 + (rr.get('sc', 0) % 2) * 2, 2, F32)
                        rr['sc'] = rr.get('sc', 0) + 1
                        P.op('dve', lambda e, sc=sc, ps=ps, m=m, e_=e_: e.tensor_scalar(out=sc[:, 0:512], in0=ps[:, :], scalar1=RWT[:, m, e_:e_ + 1], scalar2=None, op0=ALU.mult),
                             reads=[psb, brt], writes=scb)
                        finals.append(P.dma('pool', OUT[m * 128:(m + 1) * 128, cb * 512:(cb + 1) * 512], sc[:, 0:512], reads=scb + [ybuf[m]], writes=[ybuf[m]],
                                            accum_op=ALU.add))

        try:
            if stop_at != 'n4x':
                phase_rope_tables()
            chk('rope')
            for l in layers:
                if 'mixer' in parts:
                    phase_mixer(l)
                if 'ffn' in parts:
                    if l % 2 == 0:
                        phase_dense_ffn(l)
                    else:
                        phase_moe(l)
        except _Stop:
            pass
        if not finals:
            finals.append(store(OUT[0:128, 0:32], RA[:, 0:64].bitcast(F32), RAq[0:1], Buf('dummy_out'), join=False))
        P.emit(final_waits=finals)
    return nc


from concourse.bass_utils import run_bass_kernel_spmd


def kernel(**inputs):
    nc = build_program()
    consts = host_constants()
    B = inputs['x'].shape[0]
    in_maps = []
    used = set(nc.used_inputs.keys())
    for b in range(B):
        m = {}
        for k in used:
            if k == 'x':
                m[k] = np.ascontiguousarray(inputs['x'][b])
            elif k == 'mem':
                m[k] = np.ascontiguousarray(inputs['mem'][b])
            elif k == 'pos':
                m[k] = np.ascontiguousarray(inputs['positions'][b].reshape(16, 128).T).astype(np.int32)
            elif k in consts:
                m[k] = consts[k]
            else:
                m[k] = np.ascontiguousarray(inputs[k])
        in_maps.append(m)
    res = run_bass_kernel_spmd(nc, in_maps, core_ids=list(range(B)))
    return np.stack([np.asarray(r['out']) for r in res.results], axis=0).astype(np.float32)
```

```python
import numpy as np
import concourse.bass as bass
import concourse.mybir as mybir

F32 = mybir.dt.float32
BF16 = mybir.dt.bfloat16
I32 = mybir.dt.int32
ALU = mybir.AluOpType
AF = mybir.ActivationFunctionType
AX = mybir.AxisListType

ENGS = ("pe", "act", "dve", "pool", "sp")
EPOCH = 500
DMA_POOLN = {'sp': 40, 'pool': 24, 'act': 8, 'pe': 1, 'dve': 1}


class Buf:
    __slots__ = ("name", "writers", "readers", "const", "sems", "cnt", "excl")

    def __init__(self, name, const=False, excl=False):
        self.name = name
        self.excl = excl
        self.writers = []
        self.readers = {}
        self.const = const
        self.sems = []
        self.cnt = 0


class Op:
    __slots__ = ("eng", "fn", "deps", "is_dma", "sem", "semval", "signals", "seq", "sigpos", "clock", "dclock")

    def __init__(self, eng, fn, is_dma):
        self.eng = eng
        self.fn = fn
        self.is_dma = is_dma
        self.deps = []
        self.sem = None
        self.semval = 0
        self.signals = False
        self.seq = -1
        self.sigpos = -1
        self.clock = None
        self.dclock = None


class Prog:
    def __init__(self, nc, stack):
        self.nc = nc
        self.stack = stack
        self.ops = {e: [] for e in ENGS}
        self.seen = {e: {f: -1 for f in ENGS} for e in ENGS}
        self.dseen = {e: {} for e in ENGS}
        self.nsem = 0
        self.n_ops = 0
        self.dma_pool = {}
        self.dma_rr = {}

    def sem(self, name):
        self.nsem += 1
        return self.stack.enter_context(self.nc.semaphore(name))

    def sbuf(self, name, shape, dt):
        return self.stack.enter_context(self.nc.sbuf_tensor(name, list(shape), dt))

    def psum(self, name, shape, dt=F32):
        return self.stack.enter_context(self.nc.psum_tensor(name, list(shape), dt))

    def _record(self, eng, fn, reads, writes, is_dma, join):
        op = Op(eng, fn, is_dma)
        deps = {}
        if is_dma:
            pool = self.dma_pool.setdefault(eng, [])
            if len(pool) < DMA_POOLN[eng]:
                pool.append([self.sem(f"d_{eng}{len(pool)}"), 0, None])
                slot = pool[-1]
            else:
                i = self.dma_rr.get(eng, 0)
                self.dma_rr[eng] = (i + 1) % len(pool)
                slot = pool[i]
            if slot[2] is not None:
                deps[id(slot[2])] = slot[2]
            slot[1] += 16
            slot[2] = op
            op.sem = slot[0]
            op.semval = slot[1]
        for b in reads:
            for w in b.writers:
                deps[id(w)] = w
            if b.excl:
                for k, r in b.readers.items():
                    if k != eng:
                        deps[id(r)] = r
        for b in writes:
            for r in b.readers.values():
                deps[id(r)] = r
            if not join:
                for w in b.writers:
                    deps[id(w)] = w
        my_seen = self.seen[eng]
        my_dseen = self.dseen[eng]
        best_c = {}
        best_d = {}
        for d in deps.values():
            if d.is_dma:
                if my_dseen.get(id(d.sem), 0) >= d.semval:
                    continue
                k = id(d.sem)
                if k not in best_d or best_d[k].semval < d.semval:
                    best_d[k] = d
            else:
                if eng == "pe" and d.eng == "pe":
                    continue
                if my_seen[d.eng] >= d.seq:
                    continue
                if d.eng not in best_c or best_c[d.eng].seq < d.seq:
                    best_c[d.eng] = d
        need = list(best_c.values()) + list(best_d.values())
        dropped = [d for d in deps.values() if d not in need]
        for d in need:
            if d.is_dma:
                my_dseen[id(d.sem)] = max(my_dseen.get(id(d.sem), 0), d.semval)
            else:
                d.signals = True
                if my_seen[d.eng] < d.seq:
                    my_seen[d.eng] = d.seq
            for f, s in d.clock.items():
                if my_seen[f] < s:
                    my_seen[f] = s
            for k, v in d.dclock.items():
                if my_dseen.get(k, 0) < v:
                    my_dseen[k] = v
        op.deps = need
        op.seq = len(self.ops[eng])
        op.clock = dict(my_seen)
        op.dclock = dict(my_dseen)
        self.ops[eng].append(op)
        self.n_ops += 1
        for b in reads:
            if not b.const:
                key = (eng, id(op)) if is_dma else eng
                b.readers[key] = op
        for b in writes:
            if join:
                b.writers.append(op)
            else:
                b.writers = [op]
                b.readers = {}
        return op

    def op(self, eng, fn, reads=(), writes=(), join=False):
        return self._record(eng, fn, list(reads), list(writes), False, join)

    def dma(self, queue, out, in_, reads=(), writes=(), join=False, **kw):
        assert len(writes) >= 1
        return self._record(queue, lambda e: e.dma_start(out=out, in_=in_, **kw), list(reads), list(writes), True, join)

    def emit(self, final_waits=()):
        nc = self.nc
        esems = {}
        for e in ENGS:
            pos = 0
            for o in self.ops[e]:
                if (not o.is_dma) and o.signals:
                    o.sigpos = pos
                    pos += 1
            n_ep = (pos + EPOCH - 1) // EPOCH
            esems[e] = [self.sem(f"s_{e}{i}") for i in range(max(n_ep, 1))]
        engobj = {"pe": "tensor", "act": "scalar", "dve": "vector", "pool": "gpsimd", "sp": "sync"}
        with nc.Block() as block:
            def body_for(e):
                def body(eng):
                    for o in self.ops[e]:
                        for d in o.deps:
                            if d.is_dma:
                                eng.wait_ge(d.sem, d.semval)
                            else:
                                ep, idx = divmod(d.sigpos, EPOCH)
                                eng.wait_ge(esems[d.eng][ep], idx + 1)
                        ins = o.fn(eng)
                        if o.is_dma:
                            ins.then_inc(o.sem, 16)
                        elif o.signals:
                            ep, idx = divmod(o.sigpos, EPOCH)
                            ins.then_inc(esems[e][ep], 1)
                    if e == "sp":
                        for d in final_waits:
                            eng.wait_ge(d.sem, d.semval)
                return body
            for e in ENGS:
                if self.ops[e] or e == "sp":
                    getattr(block, engobj[e])(body_for(e))


from contextlib import ExitStack
import math

S = 2048
D = 2048
NT = 16
CAP = 768
NST = CAP // 128
EPS = 1e-6

WEIGHT_SHAPES = {
    'attn_norm_g': (2, 2048), 'w_in': (2, 2048, 9024), 'mla_q_a_norm_g': (2, 512), 'mla_w_uq': (2, 512, 1536),
    'mla_kv_a_norm_g': (2, 256), 'mla_w_ukv': (2, 256, 2048), 'mla_q_norm_g': (2, 192), 'mla_k_norm_g': (2, 192),
    'mla_w_out': (2, 1024, 2048), 'pool_w': (2, 4, 256, 256), 'pool_scale': (2, 1024), 'pool_w_out': (2, 1024, 2048),
    'mem_norm_g': (2, 2048), 'mem_w_kv': (2, 2048, 2048), 'mem_q_norm_g': (2, 256), 'mem_k_norm_g': (2, 256),
    'mem_w_out': (2, 1024, 2048), 'w_o': (2, 2048, 2048), 'ffn_norm_g': (2, 2048),
    'dense_w_gate': (1, 2048, 5632), 'dense_w_up': (1, 2048, 5632), 'dense_w_down': (1, 5632, 2048),
    'router_w': (1, 2048, 8), 'router_b': (1, 8), 'moe_w_gate': (1, 8, 2048, 7168), 'moe_w_up': (1, 8, 2048, 7168),
    'moe_w_down': (1, 8, 7168, 2048),
}
CONST_SHAPES = {'c_ident': (128, 128), 'c_tri': (128, 128), 'c_ones': (128, 128), 'c_pool': (12, 128, 128),
                'c_mask': (4, 128, 512), 'c_iota': (128, CAP), 'c_invf': (128, 32)}


TILED = {
    'tw_in_a': ('w_in', 1, (0, 512), 2048, 256),
    'tw_in_b': ('w_in', 1, (512, 832), 2048, 160),
    'tw_in_c': ('w_in', 1, (832, 9024), 2048, 256),
    'tw_memkv': ('mem_w_kv', 1, None, 2048, 256),
    'tw_o': ('w_o', 1, None, 2048, 256),
    'tw_mlaout': ('mla_w_out', 1, None, 1024, 512),
    'tw_poolout': ('pool_w_out', 1, None, 1024, 512),
    'tw_memout': ('mem_w_out', 1, None, 1024, 512),
    'tw_dg': ('dense_w_gate', 1, None, 2048, 256),
    'tw_du': ('dense_w_up', 1, None, 2048, 256),
    'tw_dd': ('dense_w_down', 1, None, 512, 512),
    'tw_mg': ('moe_w_gate', 2, None, 2048, 256),
    'tw_mu': ('moe_w_up', 2, None, 2048, 256),
    'tw_md': ('moe_w_down', 2, None, 1024, 512),
}


def tiled_shape(name):
    key, nlead, cr, R, C = TILED[name]
    shp = WEIGHT_SHAPES[key]
    lead, (K, F) = shp[:nlead], shp[nlead:]
    if cr is not None:
        F = cr[1] - cr[0]
    return tuple(lead) + (K // R, F // C, 128, R // 128, C)


def host_tile(name, w):
    key, nlead, cr, R, C = TILED[name]
    if cr is not None:
        w = w[..., cr[0]:cr[1]]
    lead, (K, F) = w.shape[:nlead], w.shape[nlead:]
    w = w.reshape(lead + (K // R, R // 128, 128, F // C, C))
    n = len(lead)
    w = w.transpose(tuple(range(n)) + (n, n + 3, n + 2, n + 1, n + 4))
    return np.ascontiguousarray(w)


class TW:
    def __init__(self, ap, R, C, c_base=0):
        self.ap, self.R, self.C, self.c_base = ap, R, C, c_base

    def slab(self, r0, nrows, c0, ncols):
        assert nrows == self.R and ncols == self.C and r0 % self.R == 0 and (c0 - self.c_base) % self.C == 0, (r0, nrows, c0, ncols, self.R, self.C)
        return self.ap[r0 // self.R, (c0 - self.c_base) // self.C]


def host_constants():
    c = {}
    c['c_ident'] = np.eye(128, dtype=np.float32)
    tri = np.zeros((128, 128), np.float32)
    for a in range(128):
        tri[a, a + 1:] = 1.0
    c['c_tri'] = tri
    c['c_ones'] = np.ones((128, 128), np.float32)
    pm = np.zeros((12, 128, 128), np.float32)
    for g, w in enumerate((2, 4, 8, 16)):
        for t in range(128):
            for d in range(w):
                tp = t - d
                if tp >= 0:
                    pm[3 * g + 1, tp, t] += 1.0 / w
                else:
                    pm[3 * g + 2, 128 + tp, t] += 1.0 / w
            pm[3 * g + 1, t, t] -= 1.0
            cnt = min(t + 1, w)
            for d in range(cnt):
                pm[3 * g + 0, t - d, t] += 1.0 / cnt
            pm[3 * g + 0, t, t] -= 1.0
    c['c_pool'] = pm
    mk = np.zeros((4, 128, 512), np.float32)
    for m in range(4):
        for kp in range(128):
            mk[m, kp, :] = (np.arange(512) >= 128 * m + kp)
    c['c_mask'] = mk
    c['c_iota'] = np.broadcast_to(np.arange(CAP, dtype=np.float32)[None, :], (128, CAP)).copy()
    invf = (np.float32(10000.0) ** (-np.arange(0, 64, 2, dtype=np.float32) / np.float32(64))).astype(np.float32)
    c['c_invf'] = np.broadcast_to(invf[None, :], (128, 32)).copy()
    return c


class _Stop(Exception):
    pass


def build_program(layers=(0, 1), parts=('mixer', 'ffn'), dbg_names=(), stop_at=None):
    nc = bass.Bass("TRN2", target_bir_lowering=False)
    class LazyInputs(dict):
        def __missing__(self, k):
            shp = {'x': (S, D), 'mem': (256, D), 'pos': (128, 16)}.get(k) or (tiled_shape(k) if k in TILED else None) or WEIGHT_SHAPES.get(k) or CONST_SHAPES[k]
            v = nc.dram_tensor(k, list(shp), I32 if k == 'pos' else F32, kind="ExternalInput").ap()
            self[k] = v
            return v
    T = LazyInputs()
    nc.used_inputs = T
    OUT = nc.dram_tensor('out', [S, D], F32, kind="ExternalOutput").ap()

    def scratch(name, shape, dt):
        kind = "ExternalOutput" if name in dbg_names else "Internal"
        return nc.dram_tensor(name, list(shape), dt, kind=kind).ap()

    mixT_d = scratch('mixT_d', [1024, S], BF16)
    xattT_d = scratch('xattT_d', [1024, S], BF16)
    attT_d = scratch('attT_d', [1024, S], BF16)
    gT_d = scratch('gT_d', [6144, S], BF16)
    htm_d = scratch('htm_d', [S, D], BF16)
    qnT_d = scratch('qnT_d', [1024, S], BF16)
    qrT_d = scratch('qrT_d', [512, S], BF16)
    knT_d = scratch('knT_d', [1024, S], BF16)
    krT_d = scratch('krT_d', [64, S], BF16)
    V_d = scratch('V_d', [S, 1024], BF16)
    hT_dbg = scratch('hT_dbg', [D, S], BF16) if 'hT_dbg' in dbg_names else None
    mrg_dbg = scratch('mrg_dbg', [D, S], BF16) if 'mrg_dbg' in dbg_names else None
    rt_dbg = scratch('rt_dbg', [128, 3 * 128], F32) if 'rt_dbg' in dbg_names else None

    with ExitStack() as st:
        P = Prog(nc, st)
        finals = []

        def chk(name):
            if stop_at == name:
                raise _Stop()

        RA_E = 56 * CAP
        RA = P.sbuf('RA', [128, RA_E], BF16)
        RAq = [Buf(f'RA_q{i}') for i in range(4)] + [Buf('RA_tail')]
        RB_E = 32768
        RB = P.sbuf('RB', [128, RB_E], BF16)
        RBB = [Buf(f'RB_{i}') for i in range(RB_E // 2048)]

        def rb(off_kb, size_kb, dt=BF16):
            a, b = off_kb * 512, (off_kb + size_kb) * 512
            a, b = int(a), int(b)
            ap = RB[:, a:b]
            if dt is F32:
                ap = ap.bitcast(F32)
            return ap, RBB[a // 2048:(b + 2047) // 2048]

        SLAB_E = 4096
        slabs = [(P.sbuf(f'slab{i}', [128, SLAB_E], BF16), Buf(f'slab{i}')) for i in range(4)]
        GAIN = P.sbuf('GAIN', [128, 1664], F32)
        bgain = Buf('gain')
        PSC = P.sbuf('PSC', [128, 8], F32)
        STAT = [(P.sbuf(f'stat{i}', [128, 64], F32), Buf(f'stat{i}')) for i in range(4)]
        SC = P.sbuf('SC', [128, 16, 64], F32)
        SIN = SC[:, :, 0:32]
        COS = SC[:, :, 32:64]
        bcs = Buf('cossin')
        SCK = P.sbuf('SCK', [128, 16, 8], F32)
        bsck = Buf('scaleK')
        SCM = P.sbuf('SCM', [128, 2, 4], F32)
        bscm = Buf('scaleM')
        ident = P.sbuf('ident', [128, 128], BF16)
        identf = P.sbuf('identf', [128, 128], F32)
        onesb = P.sbuf('onesb', [128, 128], BF16)
        trib = P.sbuf('trib', [128, 128], BF16)
        poolm = P.sbuf('poolm', [128, 12, 128], BF16)
        maskb = P.sbuf('maskb', [128, 4, 512], BF16)
        iota = P.sbuf('iota', [128, CAP], F32)
        invf = P.sbuf('invf', [128, 32], F32)
        pib = P.sbuf('pib', [128, 1], F32)
        bconst = Buf('const', const=True)
        RSEL = P.sbuf('RSEL', [128, 16, 8], F32)
        RWT = P.sbuf('RWT', [128, 16, 8], F32)
        RPOS = P.sbuf('RPOS', [128, 16, 8], F32)
        RSELB = P.sbuf('RSELB', [128, 16, 8], BF16)
        brt = Buf('router')
        RW = P.sbuf('RW', [128, 16, 8], F32)
        RBI = P.sbuf('RBI', [128, 8], F32)
        brw = Buf('rw')

        MM = [(P.psum(f'mm{i}', [128, 512], F32), Buf(f'mm{i}', excl=True)) for i in range(5)]
        TP = [(P.psum(f'tp{i}', [128, 1024], BF16)[:, 0:512], Buf(f'tp{i}', excl=True)) for i in range(2)]
        AUXt = P.psum('aux', [128, 512], F32)
        AUX = (AUXt, Buf('aux', excl=True))
        MM6 = MM + [AUX]
        rr = {}

        def nxt(kind, lst, n=None):
            n = len(lst) if n is None else n
            i = rr.get(kind, 0) % n
            rr[kind] = i + 1
            return lst[i]

        def ev_eng():
            rr['ev'] = rr.get('ev', 0) ^ 1
            return 'act' if rr['ev'] else 'dve'

        def copy_op(eng, out, in_, reads, writes, join=False):
            if eng == 'act':
                return P.op('act', lambda e: e.activation(out=out, in_=in_, func=AF.Copy), reads=reads, writes=writes, join=join)
            return P.op(eng, lambda e: e.tensor_copy(out=out, in_=in_), reads=reads, writes=writes, join=join)

        ybuf = [Buf(f'y{t}') for t in range(NT)]

        def cload(dst, src):
            P.dma('pool', dst, src, writes=[bconst], join=True)
        cload(ident[:], T['c_ident'])
        cload(identf[:], T['c_ident'])
        cload(onesb[:], T['c_ones'])
        cload(trib[:], T['c_tri'])
        cload(poolm[:], T['c_pool'].rearrange("k p f -> p k f"))
        cload(maskb[:], T['c_mask'].rearrange("k p f -> p k f"))
        cload(iota[:], T['c_iota'])
        cload(invf[:], T['c_invf'])

        def load_slab(W2d, r0, nrows, c0, ncols, dst=None):
            if dst is None:
                tile, buf = nxt('slab', slabs)
                bufs = [buf]
                flat = tile[:, :]
            else:
                flat, bufs = dst
            kc = nrows // 128
            assert nrows % 128 == 0 and kc * ncols <= flat.shape[1], (nrows, ncols, flat.shape)
            view = flat[:, 0:kc * ncols].rearrange("p (j f) -> p j f", f=ncols)
            if hasattr(W2d, 'slab'):
                P.dma('pool', view, W2d.slab(r0, nrows, c0, ncols), writes=bufs)
                return view, bufs
            src = W2d[r0:r0 + nrows, c0:c0 + ncols].rearrange("(j p) f -> p j f", p=128)
            step = 8
            for j0 in range(0, kc, step):
                j1 = min(kc, j0 + step)
                P.dma('pool', view[:, j0:j1, :], src[:, j0:j1, :], writes=bufs, join=(j0 > 0))
            return view, bufs

        def bcast_row(dst, row_ap, bufs, first=True):
            P.dma('sp', dst, row_ap.partition_broadcast(128), writes=bufs, join=not first)

        def mm_acc(ps, psb, pairs, reads, first=True, last=True):
            n = len(pairs)
            for i, (l, r) in enumerate(pairs):
                P.op('pe', lambda e, l=l, r=r, i=i: e.matmul(ps, lhsT=l, rhs=r, start=(first and i == 0), stop=(last and i == n - 1)),
                     reads=reads, writes=[psb], join=not (first and i == 0))

        def transposes(srcs, src_reads, dst_fn, dst_writes, width=128):
            for q in range(0, len(srcs), 4):
                m = min(4, len(srcs) - q)
                tp, tpb = nxt('tp', TP)
                tpv = tp.rearrange("p (a b) -> p a b", b=128)
                for i in range(m):
                    P.op('pe', lambda e, i=i, s=srcs[q + i], tpv=tpv: e.transpose(out=tpv[0:width, i, :], in_=s, identity=ident[:]),
                         reads=src_reads + [bconst], writes=[tpb], join=(i > 0))
                copy_op(ev_eng(), dst_fn(q, m), tpv[0:width, 0:m, :], [tpb], dst_writes, join=True)

        def rstd_from_ss(ss_ap, out_ap, n, statb, scale_extra=1.0):
            P.op('dve', lambda e: e.tensor_scalar(out=out_ap, in0=ss_ap, scalar1=1.0 / n, scalar2=EPS, op0=ALU.mult, op1=ALU.add),
                 reads=statb, writes=statb, join=True)
            P.op('act', lambda e: e.activation(out=out_ap, in_=out_ap, func=AF.Sqrt), reads=statb, writes=statb, join=True)
            P.op('dve', lambda e: e.reciprocal(out=out_ap, in_=out_ap), reads=statb, writes=statb, join=True)
            if scale_extra != 1.0:
                P.op('dve', lambda e: e.tensor_scalar(out=out_ap, in0=out_ap, scalar1=float(scale_extra), scalar2=None, op0=ALU.mult),
                     reads=statb, writes=statb, join=True)

        def store(dram_ap, sb_ap, reads, dbuf, join=True, queue='sp'):
            return P.dma(queue, dram_ap, sb_ap, reads=reads, writes=[dbuf], join=join)

        def phase_rope_tables():
            posi = P.sbuf('posi', [128, 16], I32)
            posf = P.sbuf('posf', [128, 16], F32)
            argt_, b1 = rb(0, 4, F32)
            nf_, b2 = rb(4, 4, F32)
            ni_, b3 = rb(8, 4, F32)
            argt = argt_.rearrange("p (t f) -> p t f", f=64)
            nf = nf_.rearrange("p (t f) -> p t f", f=64)
            ni = ni_.bitcast(I32).rearrange("p (t f) -> p t f", f=64)
            bp = Buf('posi')
            J = dict(reads=[bp, bconst] + b1 + b2 + b3, writes=[bp] + b1 + b2 + b3, join=True)
            P.dma('sp', posi[:], T['pos'], writes=[bp])
            P.op('dve', lambda e: e.tensor_copy(out=posf[:], in_=posi[:]), reads=[bp], writes=[bp])
            for t in range(NT):
                P.op('dve', lambda e, t=t: e.tensor_scalar(out=argt[:, t, 0:32], in0=invf[:], scalar1=posf[:, t:t + 1], scalar2=None, op0=ALU.mult), **J)
            P.op('dve', lambda e: e.tensor_scalar(out=argt[:, :, 32:64], in0=argt[:, :, 0:32], scalar1=math.pi / 2, scalar2=None, op0=ALU.add), **J)
            P.op('dve', lambda e: e.tensor_scalar(out=nf[:], in0=argt[:], scalar1=1.0 / (2 * math.pi), scalar2=None, op0=ALU.mult), **J)
            P.op('dve', lambda e: e.tensor_copy(out=ni[:], in_=nf[:]), **J)
            P.op('dve', lambda e: e.tensor_copy(out=nf[:], in_=ni[:]), **J)
            P.op('dve', lambda e: e.scalar_tensor_tensor(out=argt[:], in0=nf[:], scalar=-2 * math.pi, in1=argt[:], op0=ALU.mult, op1=ALU.add), **J)
            P.op('dve', lambda e: e.tensor_scalar(out=nf[:], in0=argt[:], scalar1=math.pi, scalar2=None, op0=ALU.is_gt), **J)
            P.op('dve', lambda e: e.scalar_tensor_tensor(out=argt[:], in0=nf[:], scalar=-2 * math.pi, in1=argt[:], op0=ALU.mult, op1=ALU.add), **J)
            P.op('dve', lambda e: e.tensor_scalar(out=nf[:], in0=argt[:], scalar1=-math.pi, scalar2=None, op0=ALU.is_lt), **J)
            P.op('dve', lambda e: e.scalar_tensor_tensor(out=argt[:], in0=nf[:], scalar=2 * math.pi, in1=argt[:], op0=ALU.mult, op1=ALU.add), **J)
            P.op('act', lambda e: e.activation(out=SC[:], in_=argt[:], func=AF.Sin), reads=[bp] + b1, writes=[bcs])

        bhtm = Buf('htm_d')

        def phase_norm(src2d, ntok, g_row, mode, dst_view=None, dst_bufs_fn=None, src_is_out=False, router=False):
            GBv, GBb = rb(24, 8, F32)
            bcast_row(GBv, g_row, GBb)
            for t in range(ntok // 128):
                xt, xb = rb(8 * (t % 2), 8, F32)
                hb, hbb = rb(16 + 4 * (t % 2), 4)
                stt, stb = nxt('stat', STAT)
                P.dma('sp', xt, src2d[t * 128:(t + 1) * 128, :], reads=[ybuf[t]] if src_is_out else [], writes=xb)
                chk('n0')
                P.op('act', lambda e, xt=xt, hb=hb, stt=stt: e.activation(out=hb, in_=xt, func=AF.Square, accum_out=stt[:, 0:1]),
                     reads=xb, writes=hbb + [stb])
                chk('n1')
                rstd_from_ss(stt[:, 0:1], stt[:, 1:2], 2048, [stb])
                chk('n2')
                if router:
                    hf, hfb = rb(32, 8, F32)
                    P.op('dve', lambda e, xt=xt, hf=hf, stt=stt: e.scalar_tensor_tensor(out=hf, in0=xt, scalar=stt[:, 1:2], in1=GBv,
                                                                                      op0=ALU.mult, op1=ALU.mult), reads=xb + [stb] + GBb, writes=hfb)
                    P.op('act', lambda e, hb=hb, hf=hf: e.activation(out=hb, in_=hf, func=AF.Copy), reads=hfb, writes=hbb)
                    router_tile(t, hf, hfb)
                else:
                    P.op('dve', lambda e, xt=xt, hb=hb, stt=stt: e.scalar_tensor_tensor(out=hb, in0=xt, scalar=stt[:, 1:2], in1=GBv,
                                                                                      op0=ALU.mult, op1=ALU.mult), reads=xb + [stb] + GBb, writes=hbb)
                chk('n3')
                if mode == 'fm':
                    transposes([hb[:, c * 128:(c + 1) * 128] for c in range(16)], hbb,
                               lambda q, m, t=t: dst_view[:, q:q + m, t * 128:(t + 1) * 128], dst_bufs_fn(t))
                else:
                    store(htm_d[t * 128:(t + 1) * 128, :], hb, hbb, bhtm, join=(t > 0))
                chk('n4')
                chk('n4x')

        def router_tile(t, hf, hfb):
            ht32, htb = rb(40, 8, F32)
            htv = ht32.rearrange("p (c t) -> p c t", t=128)
            for q in range(0, 16, 4):
                ps, psb = nxt('mm', MM)
                psv = ps.rearrange("p (a b) -> p a b", b=128)
                for i in range(4):
                    P.op('pe', lambda e, i=i, q=q, psv=psv: e.transpose(out=psv[:, i, :], in_=hf[:, (q + i) * 128:(q + i + 1) * 128], identity=identf[:]),
                         reads=hfb + [bconst], writes=[psb], join=(i > 0))
                copy_op(ev_eng(), htv[:, q:q + 4, :], psv[:, :, :], [psb], htb, join=(q > 0))
            ps, psb = AUX
            mm_acc(ps[:, 0:8], psb, [(htv[:, j, :], RW[:, j, :]) for j in range(16)], htb + [brw])
            stt, stb = nxt('stat', STAT)
            lg = stt[:, 0:8]
            P.op('dve', lambda e: e.tensor_tensor(out=lg, in0=ps[:, 0:8], in1=RBI[:], op=ALU.add), reads=[psb, brw], writes=[stb])
            m1, eq1, l2, m2, eq2, dd = stt[:, 8:9], stt[:, 16:24], stt[:, 24:32], stt[:, 9:10], stt[:, 32:40], stt[:, 10:13]
            J = dict(reads=[stb], writes=[stb], join=True)
            P.op('dve', lambda e: e.reduce_max(out=m1, in_=lg, axis=AX.X), **J)
            P.op('dve', lambda e: e.tensor_scalar(out=eq1, in0=lg, scalar1=m1, scalar2=None, op0=ALU.is_equal), **J)
            P.op('dve', lambda e: e.scalar_tensor_tensor(out=l2, in0=eq1, scalar=-1e30, in1=lg, op0=ALU.mult, op1=ALU.add), **J)
            P.op('dve', lambda e: e.reduce_max(out=m2, in_=l2, axis=AX.X), **J)
            P.op('dve', lambda e: e.tensor_scalar(out=eq2, in0=l2, scalar1=m2, scalar2=None, op0=ALU.is_equal), **J)
            P.op('dve', lambda e: e.tensor_tensor(out=dd[:, 0:1], in0=m2, in1=m1, op=ALU.subtract), **J)
            P.op('act', lambda e: e.activation(out=dd[:, 0:1], in_=dd[:, 0:1], func=AF.Exp), **J)
            P.op('dve', lambda e: e.tensor_scalar(out=dd[:, 1:2], in0=dd[:, 0:1], scalar1=1.0, scalar2=None, op0=ALU.add), **J)
            P.op('dve', lambda e: e.reciprocal(out=dd[:, 1:2], in_=dd[:, 1:2]), **J)
            P.op('dve', lambda e: e.tensor_tensor(out=dd[:, 2:3], in0=dd[:, 0:1], in1=dd[:, 1:2], op=ALU.mult), **J)
            P.op('dve', lambda e: e.tensor_tensor(out=RSEL[:, t, :], in0=eq1, in1=eq2, op=ALU.add), reads=[stb], writes=[brt], join=True)
            P.op('dve', lambda e: e.tensor_scalar(out=RWT[:, t, :], in0=eq1, scalar1=dd[:, 1:2], scalar2=None, op0=ALU.mult), reads=[stb], writes=[brt], join=True)
            P.op('dve', lambda e: e.scalar_tensor_tensor(out=RWT[:, t, :], in0=eq2, scalar=dd[:, 2:3], in1=RWT[:, t, :], op0=ALU.mult, op1=ALU.add),
                 reads=[stb, brt], writes=[brt], join=True)
            P.op('dve', lambda e: e.tensor_copy(out=RSELB[:, t, :], in_=RSEL[:, t, :]), reads=[brt], writes=[brt], join=True)

        bq_d, bkn_d, bkr_d, bv_d = Buf('qT_d'), Buf('knT_d'), Buf('krT_d'), Buf('V_d')
        bmix_d = [Buf(f'mix_d{i}') for i in range(4)]
        bxat_d = [Buf(f'xat_d{i}') for i in range(4)]
        batt_d = [Buf(f'att_d{i}') for i in range(4)]
        bg_d = [Buf(f'g_d{i}') for i in range(4)]

        def phase_mixer(l):
            y_src = T['x'] if l == layers[0] else OUT
            tw_a, tw_b, tw_c = TW(T['tw_in_a'][l], 2048, 256, 0), TW(T['tw_in_b'][l], 2048, 160, 512), TW(T['tw_in_c'][l], 2048, 256, 832)

            class _WIn:
                def slab(self, r0, nrows, c0, ncols):
                    return (tw_a if c0 < 512 else tw_b if c0 < 832 else tw_c).slab(r0, nrows, c0, ncols)
            w_in = _WIn()
            hTv = RA[:, 0:16 * S].rearrange("p (c t) -> p c t", t=S)
            hbuf = lambda t: [RAq[t // 4]]

            phase_norm(y_src, S, T['attn_norm_g'][l], 'fm', hTv, hbuf, src_is_out=(l > layers[0]))
            if hT_dbg is not None and l == 0:
                bdb = Buf('hT_dbg')
                for c in range(16):
                    finals.append(store(hT_dbg[c * 128:(c + 1) * 128, :], hTv[:, c, :], RAq[0:4], bdb, join=(c > 0)))
            memnT, memb = rb(32, 8)
            memv = memnT.rearrange("p (c t) -> p c t", t=256)
            phase_norm(T['mem'], 256, T['mem_norm_g'][l], 'fm', memv, lambda t: memb)
            bcast_row(GAIN[:, 0:512], T['mla_q_a_norm_g'][l], [bgain], first=True)
            bcast_row(GAIN[:, 512:768], T['mla_kv_a_norm_g'][l], [bgain], first=False)
            bcast_row(GAIN[:, 768:960], T['mla_q_norm_g'][l], [bgain], first=False)
            bcast_row(GAIN[:, 960:1152], T['mla_k_norm_g'][l], [bgain], first=False)
            bcast_row(GAIN[:, 1152:1408], T['mem_q_norm_g'][l], [bgain], first=False)
            bcast_row(GAIN[:, 1408:1664], T['mem_k_norm_g'][l], [bgain], first=False)
            P.dma('sp', PSC[:], T['pool_scale'][l].rearrange("(c p) -> p c", p=128), writes=[bgain], join=True, allow_slow_non_contiguous=True)
            P.op('dve', lambda e: e.tensor_tensor(out=GAIN[:, 768:896], in0=GAIN[:, 768:896], in1=GAIN[:, 960:1088], op=ALU.mult),
                 reads=[bgain], writes=[bgain], join=True)
            P.op('dve', lambda e: e.tensor_tensor(out=GAIN[:, 1152:1408], in0=GAIN[:, 1152:1408], in1=GAIN[:, 1408:1664], op=ALU.mult),
                 reads=[bgain], writes=[bgain], join=True)
            G_QA, G_KVA, G_Q, G_KPE, G_QM = GAIN[:, 0:512], GAIN[:, 512:768], GAIN[:, 768:960], GAIN[:, 1088:1152], GAIN[:, 1152:1408]

            chk('A')
            kmT, kmb = rb(40, 4)
            kmv = kmT.rearrange("p (c t) -> p c t", t=256)
            vm, vmb = rb(44, 4)
            vmv = vm.rearrange("p (m f) -> p m f", f=1024)
            wkv = TW(T['tw_memkv'][l], 2048, 256)
            for cb in range(8):
                sl, slb = load_slab(wkv, 0, 2048, cb * 256, 256)
                if cb < 4:
                    for c in range(2):
                        ps, psb = nxt('mm', MM)
                        mm_acc(ps[:, 0:256], psb, [(sl[:, j, c * 128:(c + 1) * 128], memv[:, j, :]) for j in range(16)], slb + memb)
                        copy_op(ev_eng(), kmv[:, cb * 2 + c, :], ps[:, 0:256], [psb], kmb, join=True)
                    for mt in range(2):
                        ps, psb = nxt('mm', MM)
                        mm_acc(ps[:, 0:256], psb, [(memv[:, j, mt * 128:(mt + 1) * 128], sl[:, j, :]) for j in range(16)], slb + memb)
                        jk, jkb = rb(16, 4)
                        P.op('act', lambda e, ps=ps, jk=jk, mt=mt, cb=cb: e.activation(out=jk[:, 0:256], in_=ps[:, 0:256], func=AF.Square,
                                                                                    accum_out=SCM[:, mt, cb:cb + 1]), reads=[psb], writes=jkb + [bscm])
                else:
                    for mt in range(2):
                        ps, psb = nxt('mm', MM)
                        mm_acc(ps[:, 0:256], psb, [(memv[:, j, mt * 128:(mt + 1) * 128], sl[:, j, :]) for j in range(16)], slb + memb)
                        copy_op(ev_eng(), vmv[:, mt, (cb - 4) * 256:(cb - 3) * 256], ps[:, 0:256], [psb], vmb, join=True)
            scm2 = SCM[:, :, :].rearrange("p a b -> p (a b)")
            rstd_from_ss(scm2, scm2, 256, [bscm], scale_extra=256 ** -0.5)

            chk('Bmem')
            wuq, wuqb = rb(48, 12)
            wuqv, _ = load_slab(T['mla_w_uq'][l], 0, 512, 0, 1536, dst=(wuq, wuqb))
            wkvA, wkvAb = rb(32, 4)
            wkvAv, _ = load_slab(T['mla_w_ukv'][l], 0, 256, 0, 1024, dst=(wkvA, wkvAb))
            wkvB, wkvBb = rb(36, 4)
            wkvBv, _ = load_slab(T['mla_w_ukv'][l], 0, 256, 1024, 1024, dst=(wkvB, wkvBb))

            def tm_block(t, sl2, w):
                ps, psb = nxt('mm', MM)
                for half, (sl, slb) in enumerate(sl2):
                    for j in range(16):
                        P.op('pe', lambda e, j=j, sl=sl, ps=ps, t=t, half=half: e.matmul(ps[:, half * w:(half + 1) * w], lhsT=hTv[:, j, t * 128:(t + 1) * 128],
                                                                                        rhs=sl[:, j, :], start=(j == 0), stop=(j == 15)),
                             reads=hbuf(t) + slb, writes=[psb], join=not (half == 0 and j == 0))
                return ps, psb

            sl2 = [load_slab(w_in, 0, 2048, 0, 256), load_slab(w_in, 0, 2048, 256, 256)]
            for t in range(NT):
                ps, psb = tm_block(t, sl2, 256)
                stt, stb = nxt('stat', STAT)
                jk, jkb = rb(16, 4)
                P.op('act', lambda e, ps=ps, jk=jk, stt=stt: e.activation(out=jk[:, 0:512], in_=ps[:, :], func=AF.Square, accum_out=stt[:, 0:1]),
                     reads=[psb], writes=jkb + [stb])
                rstd_from_ss(stt[:, 0:1], stt[:, 1:2], 512, [stb])
                cqn, cqnb = rb(20, 4)
                P.op('dve', lambda e, ps=ps, cqn=cqn, stt=stt: e.scalar_tensor_tensor(out=cqn[:, 0:512], in0=ps[:, :], scalar=stt[:, 1:2], in1=G_QA,
                                                                                    op0=ALU.mult, op1=ALU.mult), reads=[psb, stb, bgain], writes=cqnb)
                cqT, cqTb = rb(0, 1)
                cqTv = cqT.rearrange("p (c t) -> p c t", t=128)
                transposes([cqn[:, c * 128:(c + 1) * 128] for c in range(4)], cqnb, lambda q, m: cqTv[:, q:q + m, :], cqTb[0:1])
                qf, qfb = rb(8, 8, F32)
                for nb in range(3):
                    ps2, ps2b = nxt('mm', MM)
                    mm_acc(ps2[:, :], ps2b, [(cqTv[:, j, :], wuqv[:, j, nb * 512:(nb + 1) * 512]) for j in range(4)], cqTb[0:1] + wuqb)
                    copy_op(ev_eng(), qf[:, nb * 512:(nb + 1) * 512], ps2[:, :], [ps2b], qfb, join=True)
                q_epilogue(t, qf, qfb, G_Q)

            chk('Bcq')
            sl2 = [load_slab(w_in, 0, 2048, 512, 160), load_slab(w_in, 0, 2048, 672, 160)]
            for t in range(NT):
                ps, psb = tm_block(t, sl2, 160)
                chk('k0')
                kv_epilogue(t, ps, psb, G_KVA, G_KPE, wkvAv, wkvAb, wkvBv, wkvBb)

            chk('Bckv')
            pwt, pwtb = rb(48, 4)
            pwv = pwt.rearrange("p (g j f) -> p g j f", g=4, j=2)
            for g in range(4):
                P.dma('pool', pwv[:, g, :, :], T['pool_w'][l][g].rearrange("(j p) f -> p j f", p=128), writes=pwtb, join=(g > 0))
            for ub in range(2):
                sl2 = [load_slab(w_in, 0, 2048, 832 + ub * 512, 256), load_slab(w_in, 0, 2048, 832 + ub * 512 + 256, 256)]
                for t in range(NT):
                    ps, psb = tm_block(t, sl2, 256)
                    ucur, ucb = rb(0 + (t % 2), 1)
                    copy_op(ev_eng(), ucur[:, 0:512], ps[:, :], [psb], ucb[0:1])
                    uprev, upb = rb(0 + ((t + 1) % 2), 1)
                    pp, ppb = nxt('mm', MM)
                    for gg in range(2):
                        g = 2 * ub + gg
                        pairs = [(poolm[:, 3 * g + (1 if t > 0 else 0), :], ucur[:, gg * 256:(gg + 1) * 256])]
                        if t > 0:
                            pairs.append((poolm[:, 3 * g + 2, :], uprev[:, gg * 256:(gg + 1) * 256]))
                        n = len(pairs)
                        for i, (lh, rh) in enumerate(pairs):
                            P.op('pe', lambda e, lh=lh, rh=rh, i=i, n=n, pp=pp, gg=gg: e.matmul(pp[:, gg * 256:(gg + 1) * 256], lhsT=lh, rhs=rh, start=(i == 0), stop=(i == n - 1)),
                                 reads=ucb[0:1] + upb[0:1] + [bconst], writes=[ppb], join=not (gg == 0 and i == 0))
                    pl, plb = rb(2, 1)
                    copy_op(ev_eng(), pl[:, 0:512], pp[:, :], [ppb], plb[0:1])
                    plT, plTb = rb(3, 1)
                    plTv = plT.rearrange("p (c t) -> p c t", t=128)
                    transposes([pl[:, c * 128:(c + 1) * 128] for c in range(4)], plb[0:1], lambda q, m: plTv[:, q:q + m, :], plTb[0:1])
                    pm_, pmb = nxt('mm', MM)
                    for gg in range(2):
                        g = 2 * ub + gg
                        for hc in range(2):
                            for j in range(2):
                                P.op('pe', lambda e, g=g, gg=gg, hc=hc, j=j, pm_=pm_: e.matmul(pm_[:, (gg * 2 + hc) * 128:(gg * 2 + hc + 1) * 128], lhsT=pwv[:, g, j, hc * 128:(hc + 1) * 128],
                                                                                            rhs=plTv[:, gg * 2 + j, :], start=(j == 0), stop=(j == 1)),
                                     reads=plTb[0:1] + pwtb, writes=[pmb], join=not (gg == 0 and hc == 0 and j == 0))
                    mx, mxb = rb(4 + (t % 2), 1)
                    mxv = mx.rearrange("p (c t) -> p c t", t=128)
                    for cc in range(4):
                        P.op('dve', lambda e, cc=cc, mxv=mxv, pm_=pm_, ub=ub: e.tensor_scalar(out=mxv[:, cc, :], in0=pm_[:, cc * 128:(cc + 1) * 128],
                                                                                             scalar1=PSC[:, ub * 4 + cc:ub * 4 + cc + 1], scalar2=None, op0=ALU.mult),
                             reads=[pmb, bgain], writes=mxb[0:1], join=(cc > 0))
                    store(mixT_d[ub * 512:(ub + 1) * 512, t * 128:(t + 1) * 128].rearrange("(c p) t -> p c t", p=128), mxv, mxb[0:1], bmix_d[t // 4])

            chk('Bu')
            for qb in range(2):
                sl2 = [load_slab(w_in, 0, 2048, 1856 + qb * 512, 256), load_slab(w_in, 0, 2048, 1856 + qb * 512 + 256, 256)]
                for t in range(NT):
                    ps, psb = tm_block(t, sl2, 256)
                    stt, stb = nxt('stat', STAT)
                    jk, jkb = rb(16, 4)
                    for hh in range(2):
                        P.op('act', lambda e, ps=ps, jk=jk, stt=stt, hh=hh: e.activation(out=jk[:, hh * 256:(hh + 1) * 256], in_=ps[:, hh * 256:(hh + 1) * 256], func=AF.Square,
                                                                                        accum_out=stt[:, hh:hh + 1]), reads=[psb], writes=jkb + [stb], join=(hh > 0))
                    rstd_from_ss(stt[:, 0:2], stt[:, 2:4], 256, [stb])
                    qmn, qmnb = rb(20, 4)
                    for hh in range(2):
                        P.op('dve', lambda e, ps=ps, qmn=qmn, stt=stt, hh=hh: e.scalar_tensor_tensor(out=qmn[:, hh * 256:(hh + 1) * 256], in0=ps[:, hh * 256:(hh + 1) * 256],
                                                                                                 scalar=stt[:, 2 + hh:3 + hh], in1=G_QM, op0=ALU.mult, op1=ALU.mult),
                             reads=[psb, stb, bgain], writes=qmnb, join=(hh > 0))
                    qmT, qmTb = rb(0, 1)
                    qmTv = qmT.rearrange("p (c t) -> p c t", t=128)
                    transposes([qmn[:, c * 128:(c + 1) * 128] for c in range(4)], qmnb, lambda q, m: qmTv[:, q:q + m, :], qmTb[0:1])
                    xo, xob = rb(4 + (t % 2), 1)
                    xov = xo.rearrange("p (c t) -> p c t", t=128)
                    for hh in range(2):
                        h = 2 * qb + hh
                        pT, pTb = rb(2, 1)
                        pTv = pT[:, 0:256].rearrange("p (m t) -> p m t", t=128)
                        for mt in range(2):
                            ps2, ps2b = nxt('mm', MM)
                            mm_acc(ps2[:, 0:128], ps2b, [(kmv[:, 2 * h + dd, mt * 128:(mt + 1) * 128], qmTv[:, 2 * hh + dd, :]) for dd in range(2)], kmb + qmTb[0:1])
                            P.op('act', lambda e, ps2=ps2, pTv=pTv, mt=mt, h=h: e.activation(out=pTv[:, mt, :], in_=ps2[:, 0:128], func=AF.Exp, scale=SCM[:, mt, h:h + 1]),
                                 reads=[ps2b, bscm], writes=pTb[0:1], join=(mt > 0))
                        ps3, ps3b = nxt('mm', MM)
                        first = True
                        for dv in range(2):
                            for mt in range(2):
                                P.op('pe', lambda e, dv=dv, mt=mt, h=h, ps3=ps3, pTv=pTv: e.matmul(ps3[:, dv * 128:(dv + 1) * 128], lhsT=vmv[:, mt, h * 256 + dv * 128:h * 256 + (dv + 1) * 128],
                                                                                              rhs=pTv[:, mt, :], start=(mt == 0), stop=(mt == 1)),
                                     reads=vmb + pTb[0:1], writes=[ps3b], join=not first)
                                first = False
                        for mt in range(2):
                            P.op('pe', lambda e, mt=mt, ps3=ps3, pTv=pTv: e.matmul(ps3[:, 256:384], lhsT=onesb[:], rhs=pTv[:, mt, :], start=(mt == 0), stop=(mt == 1)),
                                 reads=pTb[0:1] + [bconst], writes=[ps3b], join=True)
                        rc, rcb = rb(3, 1, F32)
                        P.op('dve', lambda e, rc=rc, ps3=ps3: e.reciprocal(out=rc[:, 0:128], in_=ps3[:, 256:384]), reads=[ps3b], writes=rcb[0:1])
                        for dv in range(2):
                            P.op('dve', lambda e, dv=dv, hh=hh, xov=xov, ps3=ps3, rc=rc: e.tensor_tensor(out=xov[:, hh * 2 + dv, :], in0=ps3[:, dv * 128:(dv + 1) * 128], in1=rc[:, 0:128], op=ALU.mult),
                                 reads=[ps3b] + rcb[0:1], writes=xob[0:1], join=not (hh == 0 and dv == 0))
                    store(xattT_d[qb * 512:(qb + 1) * 512, t * 128:(t + 1) * 128].rearrange("(c p) t -> p c t", p=128), xov, xob[0:1], bxat_d[t // 4])

            chk('Bqm')
            for cb in range(24):
                sl, slb = load_slab(w_in, 0, 2048, 2880 + cb * 256, 256)
                for c in range(2):
                    for qt in range(4):
                        ps, psb = nxt('mm', MM)
                        mm_acc(ps[:, :], psb, [(sl[:, j, c * 128:(c + 1) * 128], hTv[:, j, qt * 512:(qt + 1) * 512]) for j in range(16)], slb + [RAq[qt]])
                        sg, sgb = rb(16 + (rr.get('sg', 0) % 4), 1)
                        rr['sg'] = rr.get('sg', 0) + 1
                        P.op('act', lambda e, sg=sg, ps=ps: e.activation(out=sg[:, 0:512], in_=ps[:, :], func=AF.Sigmoid), reads=[psb], writes=sgb[0:1])
                        r0 = cb * 256 + c * 128
                        store(gT_d[r0:r0 + 128, qt * 512:(qt + 1) * 512], sg[:, 0:512], sgb[0:1], bg_d[qt])

            chk('C')
            phase_attention()

            chk('D')
            mrgv = RA[:, 0:16 * S].rearrange("p (c t) -> p c t", t=S)
            wouts = (TW(T['tw_mlaout'][l], 1024, 512), TW(T['tw_poolout'][l], 1024, 512), TW(T['tw_memout'][l], 1024, 512))
            srcs = (attT_d, mixT_d, xattT_d)
            sbufs = (batt_d, bmix_d, bxat_d)
            for qt in range(4):
                xs = []
                for b in range(3):
                    xt_, xtb = rb(8 * b, 8)
                    xv = xt_.rearrange("p (c t) -> p c t", t=512)
                    P.dma('sp', xv, srcs[b][:, qt * 512:(qt + 1) * 512].rearrange("(c p) t -> p c t", p=128), reads=[sbufs[b][qt]], writes=xtb)
                    xs.append((xv, xtb))
                for cb in range(4):
                    acc, accb = rb(24, 8, F32)
                    accv = acc.rearrange("p (c t) -> p c t", t=512)
                    for b in range(3):
                        sl, slb = load_slab(wouts[b], 0, 1024, cb * 512, 512)
                        gt, gtb = rb(32 + 4 * (b % 2), 4)
                        gtv = gt.rearrange("p (c t) -> p c t", t=512)
                        r0 = b * 2048 + cb * 512
                        P.dma('sp', gtv, gT_d[r0:r0 + 512, qt * 512:(qt + 1) * 512].rearrange("(c p) t -> p c t", p=128), reads=[bg_d[qt]], writes=gtb)
                        for c in range(4):
                            ps, psb = nxt('mm', MM)
                            mm_acc(ps[:, :], psb, [(sl[:, j, c * 128:(c + 1) * 128], xs[b][0][:, j, :]) for j in range(8)], slb + xs[b][1])
                            if b == 0:
                                P.op('dve', lambda e, c=c, ps=ps, accv=accv, gtv=gtv: e.tensor_tensor(out=accv[:, c, :], in0=ps[:, :], in1=gtv[:, c, :], op=ALU.mult),
                                     reads=[psb] + gtb, writes=accb, join=(c > 0))
                            else:
                                tmp, tmpb = rb(40 + 2 * (c % 2), 2, F32)
                                P.op('dve', lambda e, c=c, ps=ps, tmp=tmp, gtv=gtv: e.tensor_tensor(out=tmp[:, 0:512], in0=ps[:, :], in1=gtv[:, c, :], op=ALU.mult),
                                     reads=[psb] + gtb, writes=tmpb)
                                if b == 1:
                                    P.op('pool', lambda e, c=c, accv=accv, tmp=tmp: e.tensor_tensor(out=accv[:, c, :], in0=accv[:, c, :], in1=tmp[:, 0:512], op=ALU.add),
                                         reads=accb + tmpb, writes=accb, join=True)
                                else:
                                    P.op('pool', lambda e, c=c, cb=cb, qt=qt, accv=accv, tmp=tmp: e.tensor_tensor(out=mrgv[:, cb * 4 + c, qt * 512:(qt + 1) * 512], in0=accv[:, c, :],
                                                                                                                 in1=tmp[:, 0:512], op=ALU.add),
                                         reads=accb + tmpb, writes=[RAq[qt]], join=True)
            if mrg_dbg is not None and l == 0:
                bdb = Buf('mrg_dbg')
                for c in range(16):
                    finals.append(store(mrg_dbg[c * 128:(c + 1) * 128, :], mrgv[:, c, :], RAq[0:4], bdb, join=(c > 0)))

            chk('E')
            wo = TW(T['tw_o'][l], 2048, 256)
            for cb in range(8):
                sl, slb = load_slab(wo, 0, 2048, cb * 256, 256)
                for t in range(NT):
                    ps, psb = nxt('mm', MM)
                    mm_acc(ps[:, 0:256], psb, [(mrgv[:, j, t * 128:(t + 1) * 128], sl[:, j, :]) for j in range(16)], slb + [RAq[t // 4]])
                    yt, ytb = rb(44 + (rr.get('yt', 0) % 4), 1, F32)
                    rr['yt'] = rr.get('yt', 0) + 1
                    P.dma('sp', yt[:, 0:256], y_src[t * 128:(t + 1) * 128, cb * 256:(cb + 1) * 256], reads=[ybuf[t]] if l > layers[0] else [], writes=ytb[0:1])
                    P.op('dve', lambda e, yt=yt, ps=ps: e.tensor_tensor(out=yt[:, 0:256], in0=ps[:, 0:256], in1=yt[:, 0:256], op=ALU.add), reads=[psb] + ytb[0:1], writes=ytb[0:1])
                    o = store(OUT[t * 128:(t + 1) * 128, cb * 256:(cb + 1) * 256], yt[:, 0:256], ytb[0:1], ybuf[t], join=(l == layers[0] and cb > 0))
                    finals.append(o)

        def q_epilogue(t, qf, qfb, G_Q):
            stt, stb = nxt('stat', STAT)
            qv = qf[:, 0:1536].rearrange("p (h d) -> p h d", d=192)
            sq, sqb = rb(24, 8, F32)
            sqv = sq[:, 0:1536].rearrange("p (h d) -> p h d", d=192)
            P.op('act', lambda e: e.activation(out=sq[:, 0:1536], in_=qf[:, 0:1536], func=AF.Square), reads=qfb, writes=sqb)
            P.op('dve', lambda e: e.tensor_reduce(out=stt[:, 0:8], in_=sqv, axis=AX.X, op=ALU.add), reads=sqb, writes=[stb])
            rstd_from_ss(stt[:, 0:8], stt[:, 8:16], 192, [stb])
            P.op('dve', lambda e: e.tensor_tensor(out=sqv, in0=qv, in1=stt[:, 8:16].unsqueeze(2).broadcast_to([128, 8, 192]), op=ALU.mult),
                 reads=qfb + [stb], writes=sqb)
            P.op('dve', lambda e: e.tensor_tensor(out=sqv, in0=sqv, in1=G_Q.unsqueeze(1).broadcast_to([128, 8, 192]), op=ALU.mult),
                 reads=sqb + [bgain], writes=sqb)
            qb_, qbb = rb(20, 4)
            qbn = qb_[:, 0:1024].rearrange("p (h d) -> p h d", d=128)
            qbr = qb_[:, 1024:1536].rearrange("p (h d) -> p h d", d=64)
            P.op('act', lambda e: e.activation(out=qbn, in_=sqv[:, :, 0:128], func=AF.Copy), reads=sqb, writes=qbb)
            x1, x2 = sqv[:, :, 128:160], sqv[:, :, 160:192]
            cb_ = COS[:, t, :].unsqueeze(1).broadcast_to([128, 8, 32])
            sb_ = SIN[:, t, :].unsqueeze(1).broadcast_to([128, 8, 32])
            tm, tmb = rb(60, 4, F32)
            t1 = tm[:, 0:256].rearrange("p (h d) -> p h d", d=32)
            t2 = tm[:, 256:512].rearrange("p (h d) -> p h d", d=32)
            P.op('pool', lambda e: e.tensor_tensor(out=t1, in0=x1, in1=cb_, op=ALU.mult), reads=sqb + [bcs], writes=tmb)
            P.op('pool', lambda e: e.tensor_tensor(out=t2, in0=x2, in1=sb_, op=ALU.mult), reads=sqb + [bcs], writes=tmb, join=True)
            P.op('pool', lambda e: e.tensor_tensor(out=qbr[:, :, 0:32], in0=t1, in1=t2, op=ALU.subtract), reads=tmb, writes=qbb, join=True)
            P.op('dve', lambda e: e.tensor_tensor(out=t1, in0=x2, in1=cb_, op=ALU.mult), reads=sqb + [bcs] + qbb, writes=tmb)
            P.op('dve', lambda e: e.tensor_tensor(out=t2, in0=x1, in1=sb_, op=ALU.mult), reads=sqb + [bcs], writes=tmb, join=True)
            P.op('dve', lambda e: e.tensor_tensor(out=qbr[:, :, 32:64], in0=t1, in1=t2, op=ALU.add), reads=tmb, writes=qbb, join=True)
            qs, qsb = rb(1, 2)
            qsn = qs[:, 0:1024].rearrange("p (h t) -> p h t", t=128)
            qsr_, qsrb = rb(5, 2)
            qsr = qsr_[:, 0:1024].rearrange("p (h t) -> p h t", t=128)
            transposes([qbn[:, h, :] for h in range(8)], qbb, lambda q, m: qsn[:, q:q + m, :], qsb)
            transposes([qbr[:, h, :] for h in range(8)], qbb, lambda q, m: qsr[0:64, q:q + m, :], qsrb, width=64)
            store(qnT_d[:, t * 128:(t + 1) * 128].rearrange("(h p) t -> p h t", p=128), qsn, qsb, bq_d, join=True)
            store(qrT_d[:, t * 128:(t + 1) * 128].rearrange("(h p) t -> p h t", p=64), qsr[0:64, :, :], qsrb, bq_d, join=True)

        def kv_epilogue(t, ps, psb, G_KVA, G_KPE, wkvAv, wkvAb, wkvBv, wkvBb):
            stt, stb = nxt('stat', STAT)
            jk, jkb = rb(16, 4)
            P.op('act', lambda e: e.activation(out=jk[:, 0:256], in_=ps[:, 0:256], func=AF.Square, accum_out=stt[:, 0:1]), reads=[psb], writes=jkb + [stb])
            P.op('act', lambda e: e.activation(out=jk[:, 256:320], in_=ps[:, 256:320], func=AF.Square, accum_out=stt[:, 2:3]), reads=[psb], writes=jkb + [stb], join=True)
            rstd_from_ss(stt[:, 0:1], stt[:, 1:2], 256, [stb])
            ckn, cknb = rb(20, 4)
            P.op('dve', lambda e: e.scalar_tensor_tensor(out=ckn[:, 0:256], in0=ps[:, 0:256], scalar=stt[:, 1:2], in1=G_KVA, op0=ALU.mult, op1=ALU.mult),
                 reads=[psb, stb, bgain], writes=cknb)
            chk('k1')
            kp, kpb = rb(60, 4, F32)
            P.op('dve', lambda e: e.tensor_tensor(out=kp[:, 0:64], in0=ps[:, 256:320], in1=G_KPE, op=ALU.mult), reads=[psb, bgain], writes=kpb)
            x1, x2 = kp[:, 0:32], kp[:, 32:64]
            c_, s_ = COS[:, t, :], SIN[:, t, :]
            t1, t2, t3, t4 = kp[:, 64:96], kp[:, 96:128], kp[:, 128:160], kp[:, 160:192]
            kr = ckn[:, 256:320]
            P.op('dve', lambda e: e.tensor_tensor(out=t1, in0=x1, in1=c_, op=ALU.mult), reads=kpb + [bcs], writes=kpb, join=True)
            P.op('dve', lambda e: e.tensor_tensor(out=t2, in0=x2, in1=s_, op=ALU.mult), reads=kpb + [bcs], writes=kpb, join=True)
            P.op('dve', lambda e: e.tensor_tensor(out=t3, in0=x2, in1=c_, op=ALU.mult), reads=kpb + [bcs], writes=kpb, join=True)
            P.op('dve', lambda e: e.tensor_tensor(out=t4, in0=x1, in1=s_, op=ALU.mult), reads=kpb + [bcs], writes=kpb, join=True)
            P.op('dve', lambda e: e.tensor_tensor(out=kr[:, 0:32], in0=t1, in1=t2, op=ALU.subtract), reads=kpb, writes=cknb, join=True)
            P.op('dve', lambda e: e.tensor_tensor(out=kr[:, 32:64], in0=t3, in1=t4, op=ALU.add), reads=kpb, writes=cknb, join=True)
            chk('k2')
            ckT, ckTb = rb(0, 1)
            ckTv = ckT[:, 0:256].rearrange("p (c t) -> p c t", t=128)
            transposes([ckn[:, c * 128:(c + 1) * 128] for c in range(2)], cknb, lambda q, m: ckTv[:, q:q + m, :], ckTb[0:1])
            krs, krsb = rb(1 + (t % 2), 1)
            krv = krs[:, 0:128].rearrange("p (c t) -> p c t", t=128)
            transposes([kr], cknb, lambda q, m: krv[0:64, q:q + m, :], krsb[0:1], width=64)
            store(krT_d[:, t * 128:(t + 1) * 128], krs[0:64, 0:128], krsb[0:1], bkr_d, join=True)
            chk('k3')
            vst, vstb = rb(3 + (t % 2) * 2, 2)
            kns, knsb = rb(8 + (t % 2) * 2, 2)
            sq, sqb = rb(24, 8, F32)
            for nb in range(4):
                wv, wb = (wkvAv, wkvAb) if nb < 2 else (wkvBv, wkvBb)
                ps2, ps2b = nxt('mm', MM)
                mm_acc(ps2[:, :], ps2b, [(ckTv[:, j, :], wv[:, j, (nb % 2) * 512:(nb % 2 + 1) * 512]) for j in range(2)], ckTb[0:1] + wb)
                for hh in range(2):
                    hd = 2 * nb + hh
                    kcol = ps2[:, hh * 256:hh * 256 + 128]
                    vcol = ps2[:, hh * 256 + 128:hh * 256 + 256]
                    fj = not (nb == 0 and hh == 0)
                    P.op('act', lambda e, kcol=kcol, hd=hd: e.activation(out=kns[:, hd * 128:(hd + 1) * 128], in_=kcol, func=AF.Copy),
                         reads=[ps2b], writes=knsb, join=fj)
                    P.op('dve', lambda e, vcol=vcol, hd=hd: e.tensor_copy(out=vst[:, hd * 128:(hd + 1) * 128], in_=vcol),
                         reads=[ps2b], writes=vstb, join=fj)
                    P.op('act', lambda e, kcol=kcol, hd=hd: e.activation(out=sq[:, hd * 128:(hd + 1) * 128], in_=kcol, func=AF.Square),
                         reads=[ps2b], writes=sqb, join=fj)
            chk('k4')
            store(V_d[t * 128:(t + 1) * 128, :], vst[:, 0:1024], vstb, bv_d, join=True)
            P.op('dve', lambda e: e.tensor_reduce(out=stt[:, 8:16], in_=sq[:, 0:1024].rearrange("p (h d) -> p h d", d=128), axis=AX.X, op=ALU.add), reads=sqb, writes=[stb], join=True)
            P.op('dve', lambda e: e.tensor_scalar(out=stt[:, 8:16], in0=stt[:, 8:16], scalar1=stt[:, 2:3], scalar2=None, op0=ALU.add), reads=[stb], writes=[stb], join=True)
            rstd_from_ss(stt[:, 8:16], stt[:, 8:16], 192, [stb], scale_extra=192 ** -0.5)
            P.op('dve', lambda e: e.tensor_copy(out=SCK[:, t, :], in_=stt[:, 8:16]), reads=[stb], writes=[bsck], join=True)
            chk('k5')
            knT, knTb = rb(12 + (t % 2) * 2, 2)
            knTv = knT[:, 0:1024].rearrange("p (h t) -> p h t", t=128)
            transposes([kns[:, h * 128:(h + 1) * 128] for h in range(8)], knsb, lambda q, m: knTv[:, q:q + m, :], knTb)
            store(knT_d[:, t * 128:(t + 1) * 128].rearrange("(h p) t -> p h t", p=128), knTv, knTb, bkn_d, join=True)

        def phase_attention():
            krT, krTb = rb(0, 4)
            P.dma('sp', krT[0:64, :], krT_d[:, :], reads=[bkr_d], writes=krTb)
            for h in range(8):
                o = 4 + (h % 2) * 16
                qn, qnb = rb(o, 4)
                qr, qrb = rb(o + 4, 4)
                kn, knb = rb(o + 8, 4)
                vh, vhb = rb(o + 12, 4)
                vhv = vh.rearrange("p (t d) -> p t d", d=128)
                P.dma('sp', qn, qnT_d[h * 128:(h + 1) * 128, :], reads=[bq_d], writes=qnb)
                P.dma('sp', qr[0:64, :], qrT_d[h * 64:(h + 1) * 64, :], reads=[bq_d], writes=qrb)
                P.dma('sp', kn, knT_d[h * 128:(h + 1) * 128, :], reads=[bkn_d], writes=knb)
                for hf_ in range(2):
                    P.dma('sp', vhv[:, hf_ * 8:(hf_ + 1) * 8, :], V_d[hf_ * 1024:(hf_ + 1) * 1024, h * 128:(h + 1) * 128].rearrange("(t p) d -> p t d", p=128),
                          reads=[bv_d], writes=vhb, join=(hf_ > 0))
                for qt in range(4):
                    nk = 4 * (qt + 1)
                    num, numb = MM[4]
                    den, denb = AUX
                    qs = slice(qt * 512, (qt + 1) * 512)
                    for kt in range(nk):
                        ps, psb = nxt('mmA', MM, 4)
                        ks = slice(kt * 128, (kt + 1) * 128)
                        P.op('pe', lambda e, ps=ps, ks=ks, qs=qs, kn=kn, qn=qn: e.matmul(ps[:, :], lhsT=kn[:, ks], rhs=qn[:, qs], start=True, stop=False),
                             reads=knb + qnb, writes=[psb])
                        P.op('pe', lambda e, ps=ps, ks=ks, qs=qs, qr=qr: e.matmul(ps[:, :], lhsT=krT[0:64, ks], rhs=qr[0:64, qs], start=False, stop=True),
                             reads=krTb + qrb, writes=[psb], join=True)
                        pT, pTb = rb(36 + (rr.get('pT', 0) % 4), 1)
                        rr['pT'] = rr.get('pT', 0) + 1
                        P.op('act', lambda e, ps=ps, pT=pT, kt=kt, h=h: e.activation(out=pT[:, 0:512], in_=ps[:, :], func=AF.Exp, scale=SCK[:, kt, h:h + 1]),
                             reads=[psb, bsck], writes=pTb[0:1])
                        if kt >= 4 * qt:
                            P.op('dve', lambda e, pT=pT, kt=kt, qt=qt: e.tensor_tensor(out=pT[:, 0:512], in0=pT[:, 0:512], in1=maskb[:, kt - 4 * qt, :], op=ALU.mult),
                                 reads=pTb[0:1] + [bconst], writes=pTb[0:1])
                        P.op('pe', lambda e, pT=pT, kt=kt, nk=nk, vhv=vhv, num=num: e.matmul(num[:, :], lhsT=vhv[:, kt, :], rhs=pT[:, 0:512], start=(kt == 0), stop=(kt == nk - 1)),
                             reads=vhb + pTb[0:1], writes=[numb], join=(kt > 0))
                        P.op('pe', lambda e, pT=pT, kt=kt, nk=nk, den=den: e.matmul(den[:, :], lhsT=onesb[:], rhs=pT[:, 0:512], start=(kt == 0), stop=(kt == nk - 1)),
                             reads=pTb[0:1] + [bconst], writes=[denb], join=(kt > 0))
                    rc, rcb = rb(40, 2, F32)
                    P.op('dve', lambda e, rc=rc, den=den: e.reciprocal(out=rc[:, 0:512], in_=den[:, :]), reads=[denb], writes=rcb)
                    ao, aob = rb(42 + (qt % 2), 1)
                    P.op('dve', lambda e, ao=ao, num=num, rc=rc: e.tensor_tensor(out=ao[:, 0:512], in0=num[:, :], in1=rc[:, 0:512], op=ALU.mult), reads=[numb] + rcb, writes=aob[0:1])
                    store(attT_d[h * 128:(h + 1) * 128, qs], ao[:, 0:512], aob[0:1], batt_d[qt])

        def phase_dense_ffn(l):
            h2v = RA[:, 0:16 * S].rearrange("p (c t) -> p c t", t=S)
            phase_norm(OUT, S, T['ffn_norm_g'][l], 'fm', h2v, lambda t: [RAq[t // 4]], src_is_out=True)
            wg, wu, wd = TW(T['tw_dg'][0], 2048, 256), TW(T['tw_du'][0], 2048, 256), TW(T['tw_dd'][0], 512, 512)
            aT, aTb = rb(0, 44)
            aTv = aT.rearrange("p (f t) -> p f t", t=512)
            for qt in range(4):
                for fs in range(22):
                    sg_, sgb = load_slab(wg, 0, 2048, fs * 256, 256)
                    su_, sub = load_slab(wu, 0, 2048, fs * 256, 256)
                    for c in range(2):
                        pg, pgb = nxt('mm6', MM6)
                        pu, pub = nxt('mm6', MM6)
                        mm_acc(pg[:, :], pgb, [(sg_[:, j, c * 128:(c + 1) * 128], h2v[:, j, qt * 512:(qt + 1) * 512]) for j in range(16)], sgb + [RAq[qt]])
                        mm_acc(pu[:, :], pub, [(su_[:, j, c * 128:(c + 1) * 128], h2v[:, j, qt * 512:(qt + 1) * 512]) for j in range(16)], sub + [RAq[qt]])
                        sl_, slb = rb(44 + 2 * (rr.get('silu', 0) % 2), 2, F32)
                        rr['silu'] = rr.get('silu', 0) + 1
                        P.op('act', lambda e, sl_=sl_, pg=pg: e.activation(out=sl_[:, 0:512], in_=pg[:, :], func=AF.Silu), reads=[pgb], writes=slb)
                        P.op('dve', lambda e, sl_=sl_, pu=pu, fs=fs, c=c: e.tensor_tensor(out=aTv[:, fs * 2 + c, :], in0=sl_[:, 0:512], in1=pu[:, :], op=ALU.mult),
                             reads=slb + [pub], writes=aTb, join=True)
                for cb in range(4):
                    accs = [MM[i] for i in range(4)]
                    for fr in range(0, 44, 4):
                        nf = 4
                        sd, sdb = load_slab(wd, fr * 128, nf * 128, cb * 512, 512)
                        for fc in range(nf):
                            f = fr + fc
                            for tt in range(4):
                                ps, psb = accs[tt]
                                P.op('pe', lambda e, ps=ps, f=f, tt=tt, sd=sd, fc=fc: e.matmul(ps[:, :], lhsT=aTv[:, f, tt * 128:(tt + 1) * 128], rhs=sd[:, fc, :], start=(f == 0), stop=(f == 43)),
                                     reads=aTb + sdb, writes=[psb], join=(f > 0))
                    for tt in range(4):
                        t = qt * 4 + tt
                        ps, psb = accs[tt]
                        yt, ytb = rb(48 + 2 * (rr.get('yt2', 0) % 4), 2, F32)
                        rr['yt2'] = rr.get('yt2', 0) + 1
                        P.dma('sp', yt[:, 0:512], OUT[t * 128:(t + 1) * 128, cb * 512:(cb + 1) * 512], reads=[ybuf[t]], writes=ytb)
                        P.op('dve', lambda e, yt=yt, ps=ps: e.tensor_tensor(out=yt[:, 0:512], in0=ps[:, :], in1=yt[:, 0:512], op=ALU.add), reads=[psb] + ytb, writes=ytb)
                        finals.append(store(OUT[t * 128:(t + 1) * 128, cb * 512:(cb + 1) * 512], yt[:, 0:512], ytb, ybuf[t], join=False))

        def phase_moe(l):
            P.dma('sp', RW[:], T['router_w'][0].rearrange("(j p) e -> p j e", p=128), writes=[brw])
            bcast_row(RBI[:], T['router_b'][0], [brw], first=False)
            phase_norm(OUT, S, T['ffn_norm_g'][l], 'tm', src_is_out=True, router=True)
            for m in range(NT):
                ps, psb = AUX
                pairs = [(onesb[:], RSELB[:, k, :]) for k in range(m)] + [(trib[:], RSELB[:, m, :])]
                mm_acc(ps[:, 0:8], psb, pairs, [brt, bconst])
                P.op('dve', lambda e, m=m, ps=ps: e.tensor_copy(out=RPOS[:, m, :], in_=ps[:, 0:8]), reads=[psb], writes=[brt], join=True)
            if rt_dbg is not None:
                for i, src in enumerate((RSEL, RWT, RPOS)):
                    finals.append(store(rt_dbg[:, i * 128:(i + 1) * 128], src[:, :, :].rearrange("p a b -> p (a b)"), [brt], Buf(f'rt_dbg{i}'), join=False))
            aT = RA[:, 0:56 * CAP]
            aTv = aT.rearrange("p (f s) -> p f s", s=CAP)
            aTb = RAq
            GT, GTb = rb(0, 24)
            GTv = GT.rearrange("p (j s) -> p j s", s=CAP)
            OEv = GT.rearrange("p (s f) -> p s f", f=2048)
            PE_, PEb = rb(24, 24)
            PEv = PE_.rearrange("p (k s) -> p k s", s=CAP)
            HALF = CAP // 2
            for e_ in range(8):
                wg, wu, wd = TW(T['tw_mg'][0][e_], 2048, 256), TW(T['tw_mu'][0][e_], 2048, 256), TW(T['tw_md'][0][e_], 1024, 512)
                for k in range(NT):
                    eng = 'dve' if k % 2 == 0 else 'pool'
                    P.op(eng, lambda e, k=k, e_=e_: e.tensor_scalar(out=PEv[:, k, :], in0=iota[:], scalar1=RPOS[:, k, e_:e_ + 1], scalar2=RSEL[:, k, e_:e_ + 1],
                                                                    op0=ALU.is_equal, op1=ALU.mult), reads=[brt, bconst], writes=PEb, join=(k > 0))
                for j in range(16):
                    hs, hsb = rb(48 + 4 * (j % 2), 4)
                    hsv = hs.rearrange("p (k f) -> p k f", f=128)
                    P.dma('sp', hsv, htm_d[:, j * 128:(j + 1) * 128].rearrange("(k p) f -> p k f", p=128), reads=[bhtm], writes=hsb)
                    for half in range(2):
                        ps, psb = nxt('mm', MM)
                        mm_acc(ps[:, 0:HALF], psb, [(hsv[:, k, :], PEv[:, k, half * HALF:(half + 1) * HALF]) for k in range(NT)], hsb + PEb)
                        copy_op(ev_eng(), GTv[:, j, half * HALF:(half + 1) * HALF], ps[:, 0:HALF], [psb], GTb, join=True)
                for fs in range(28):
                    sg_, sgb = load_slab(wg, 0, 2048, fs * 256, 256)
                    su_, sub = load_slab(wu, 0, 2048, fs * 256, 256)
                    for c in range(2):
                        for half in range(2):
                            pg, pgb = nxt('mm6', MM6)
                            pu, pub = nxt('mm6', MM6)
                            hs_ = slice(half * HALF, (half + 1) * HALF)
                            mm_acc(pg[:, 0:HALF], pgb, [(sg_[:, j, c * 128:(c + 1) * 128], GTv[:, j, hs_]) for j in range(16)], sgb + GTb)
                            mm_acc(pu[:, 0:HALF], pub, [(su_[:, j, c * 128:(c + 1) * 128], GTv[:, j, hs_]) for j in range(16)], sub + GTb)
                            sl_, slb = rb(56 + 2 * (rr.get('silu', 0) % 2), 2, F32)
                            rr['silu'] = rr.get('silu', 0) + 1
                            P.op('act', lambda e, sl_=sl_, pg=pg: e.activation(out=sl_[:, 0:HALF], in_=pg[:, 0:HALF], func=AF.Silu), reads=[pgb], writes=slb)
                            P.op('dve', lambda e, sl_=sl_, pu=pu, fs=fs, c=c, hs_=hs_: e.tensor_tensor(out=aTv[:, fs * 2 + c, hs_], in0=sl_[:, 0:HALF], in1=pu[:, 0:HALF], op=ALU.mult),
                                 reads=slb + [pub], writes=aTb, join=True)
                for cb in range(4):
                    accs = [MM[i] for i in range(5)] + [AUX]
                    for fr in range(0, 56, 8):
                        sd, sdb = load_slab(wd, fr * 128, 8 * 128, cb * 512, 512)
                        for fc in range(8):
                            f = fr + fc
                            for s_ in range(NST):
                                ps, psb = accs[s_]
                                P.op('pe', lambda e, ps=ps, f=f, s_=s_, sd=sd, fc=fc: e.matmul(ps[:, :], lhsT=aTv[:, f, s_ * 128:(s_ + 1) * 128], rhs=sd[:, fc, :], start=(f == 0), stop=(f == 55)),
                                     reads=aTb + sdb, writes=[psb], join=(f > 0))
                    for s_ in range(NST):
                        ps, psb = accs[s_]
                        copy_op(ev_eng(), OEv[:, s_, cb * 512:(cb + 1) * 512], ps[:, :], [psb], GTb, join=True)
                for m in range(NT):
                    pwT, pwTb = rb(56 + 2 * (m % 2), 2)
                    pwTv = pwT[:, 0:NST * 128].rearrange("p (s t) -> p s t", t=128)
                    transposes([PEv[:, m, s_ * 128:(s_ + 1) * 128] for s_ in range(NST)], PEb, lambda q, mm_: pwTv[:, q:q + mm_, :], pwTb)
                    for cb in range(4):
                        ps, psb = nxt('mm', MM)
                        mm_acc(ps[:, :], psb, [(pwTv[:, s_, :], OEv[:, s_, cb * 512:(cb + 1) * 512]) for s_ in range(NST)], pwTb + GTb)
                        sc, scb = rb(60 + (rr.get('sc', 0) % 2) * 2, 2, F32)
                        rr['sc'] = rr.get('sc', 0) + 1
                        P.op('dve', lambda e, sc=sc, ps=ps, m=m, e_=e_: e.tensor_scalar(out=sc[:, 0:512], in0=ps[:, :], scalar1=RWT[:, m, e_:e_ + 1], scalar2=None, op0=ALU.mult),
                             reads=[psb, brt], writes=scb)
                        finals.append(P.dma('pool', OUT[m * 128:(m + 1) * 128, cb * 512:(cb + 1) * 512], sc[:, 0:512], reads=scb + [ybuf[m]], writes=[ybuf[m]],
                                            accum_op=ALU.add))

        try:
            if stop_at != 'n4x':
                phase_rope_tables()
            chk('rope')
            for l in layers:
                if 'mixer' in parts:
                    phase_mixer(l)
                if 'ffn' in parts:
                    if l % 2 == 0:
                        phase_dense_ffn(l)
                    else:
                        phase_moe(l)
        except _Stop:
            pass
        if not finals:
            finals.append(store(OUT[0:128, 0:32], RA[:, 0:64].bitcast(F32), RAq[0:1], Buf('dummy_out'), join=False))
        P.emit(final_waits=finals)
    return nc


from concourse.bass_utils import run_bass_kernel_spmd


def kernel(**inputs):
    nc = build_program()
    consts = host_constants()
    B = inputs['x'].shape[0]
    in_maps = []
    tiled_cache = {}
    used = set(nc.used_inputs.keys())
    for b in range(B):
        m = {}
        for k in used:
            if k == 'x':
                m[k] = np.ascontiguousarray(inputs['x'][b])
            elif k == 'mem':
                m[k] = np.ascontiguousarray(inputs['mem'][b])
            elif k == 'pos':
                m[k] = np.ascontiguousarray(inputs['positions'][b].reshape(16, 128).T).astype(np.int32)
            elif k in consts:
                m[k] = consts[k]
            elif k in TILED:
                if k not in tiled_cache:
                    tiled_cache[k] = host_tile(k, np.asarray(inputs[TILED[k][0]]))
                m[k] = tiled_cache[k]
            else:
                m[k] = np.ascontiguousarray(inputs[k])
        in_maps.append(m)
    res = run_bass_kernel_spmd(nc, in_maps, core_ids=list(range(B)))
    return np.stack([np.asarray(r['out']) for r in res.results], axis=0).astype(np.float32)
```

```python
import numpy as np
import concourse.bass as bass
import concourse.mybir as mybir

F32 = mybir.dt.float32
BF16 = mybir.dt.bfloat16
I32 = mybir.dt.int32
ALU = mybir.AluOpType
AF = mybir.ActivationFunctionType
AX = mybir.AxisListType

ENGS = ("pe", "act", "dve", "pool", "sp")
EPOCH = 500
DMA_POOLN = {'sp': 40, 'pool': 24, 'act': 8, 'pe': 1, 'dve': 1}


class Buf:
    __slots__ = ("name", "writers", "readers", "const", "sems", "cnt", "excl")

    def __init__(self, name, const=False, excl=False):
        self.name = name
        self.excl = excl
        self.writers = []
        self.readers = {}
        self.const = const
        self.sems = []
        self.cnt = 0


class Op:
    __slots__ = ("eng", "fn", "deps", "is_dma", "sem", "semval", "signals", "seq", "sigpos", "clock", "dclock")

    def __init__(self, eng, fn, is_dma):
        self.eng = eng
        self.fn = fn
        self.is_dma = is_dma
        self.deps = []
        self.sem = None
        self.semval = 0
        self.signals = False
        self.seq = -1
        self.sigpos = -1
        self.clock = None
        self.dclock = None


class Prog:
    def __init__(self, nc, stack):
        self.nc = nc
        self.stack = stack
        self.ops = {e: [] for e in ENGS}
        self.seen = {e: {f: -1 for f in ENGS} for e in ENGS}
        self.dseen = {e: {} for e in ENGS}
        self.nsem = 0
        self.n_ops = 0
        self.dma_pool = {}
        self.dma_rr = {}

    def sem(self, name):
        self.nsem += 1
        return self.stack.enter_context(self.nc.semaphore(name))

    def sbuf(self, name, shape, dt):
        return self.stack.enter_context(self.nc.sbuf_tensor(name, list(shape), dt))

    def psum(self, name, shape, dt=F32):
        return self.stack.enter_context(self.nc.psum_tensor(name, list(shape), dt))

    def _record(self, eng, fn, reads, writes, is_dma, join):
        op = Op(eng, fn, is_dma)
        deps = {}
        if is_dma:
            pool = self.dma_pool.setdefault(eng, [])
            if len(pool) < DMA_POOLN[eng]:
                pool.append([self.sem(f"d_{eng}{len(pool)}"), 0, None])
                slot = pool[-1]
            else:
                i = self.dma_rr.get(eng, 0)
                self.dma_rr[eng] = (i + 1) % len(pool)
                slot = pool[i]
            if slot[2] is not None:
                deps[id(slot[2])] = slot[2]
            slot[1] += 16
            slot[2] = op
            op.sem = slot[0]
            op.semval = slot[1]
        for b in reads:
            for w in b.writers:
                deps[id(w)] = w
            if b.excl:
                for k, r in b.readers.items():
                    if k != eng:
                        deps[id(r)] = r
        for b in writes:
            for r in b.readers.values():
                deps[id(r)] = r
            if not join:
                for w in b.writers:
                    deps[id(w)] = w
        my_seen = self.seen[eng]
        my_dseen = self.dseen[eng]
        best_c = {}
        best_d = {}
        for d in deps.values():
            if d.is_dma:
                if my_dseen.get(id(d.sem), 0) >= d.semval:
                    continue
                k = id(d.sem)
                if k not in best_d or best_d[k].semval < d.semval:
                    best_d[k] = d
            else:
                if eng == "pe" and d.eng == "pe":
                    continue
                if my_seen[d.eng] >= d.seq:
                    continue
                if d.eng not in best_c or best_c[d.eng].seq < d.seq:
                    best_c[d.eng] = d
        need = list(best_c.values()) + list(best_d.values())
        dropped = [d for d in deps.values() if d not in need]
        for d in need:
            if d.is_dma:
                my_dseen[id(d.sem)] = max(my_dseen.get(id(d.sem), 0), d.semval)
            else:
                d.signals = True
                if my_seen[d.eng] < d.seq:
                    my_seen[d.eng] = d.seq
            for f, s in d.clock.items():
                if my_seen[f] < s:
                    my_seen[f] = s
            for k, v in d.dclock.items():
                if my_dseen.get(k, 0) < v:
                    my_dseen[k] = v
        op.deps = need
        op.seq = len(self.ops[eng])
        op.clock = dict(my_seen)
        op.dclock = dict(my_dseen)
        self.ops[eng].append(op)
        self.n_ops += 1
        for b in reads:
            if not b.const:
                key = (eng, id(op)) if is_dma else eng
                b.readers[key] = op
        for b in writes:
            if join:
                b.writers.append(op)
            else:
                b.writers = [op]
                b.readers = {}
        return op

    def op(self, eng, fn, reads=(), writes=(), join=False):
        return self._record(eng, fn, list(reads), list(writes), False, join)

    def dma(self, queue, out, in_, reads=(), writes=(), join=False, **kw):
        assert len(writes) >= 1
        return self._record(queue, lambda e: e.dma_start(out=out, in_=in_, **kw), list(reads), list(writes), True, join)

    def emit(self, final_waits=()):
        nc = self.nc
        esems = {}
        for e in ENGS:
            pos = 0
            for o in self.ops[e]:
                if (not o.is_dma) and o.signals:
                    o.sigpos = pos
                    pos += 1
            n_ep = (pos + EPOCH - 1) // EPOCH
            esems[e] = [self.sem(f"s_{e}{i}") for i in range(max(n_ep, 1))]
        engobj = {"pe": "tensor", "act": "scalar", "dve": "vector", "pool": "gpsimd", "sp": "sync"}
        with nc.Block() as block:
            def body_for(e):
                def body(eng):
                    for o in self.ops[e]:
                        for d in o.deps:
                            if d.is_dma:
                                eng.wait_ge(d.sem, d.semval)
                            else:
                                ep, idx = divmod(d.sigpos, EPOCH)
                                eng.wait_ge(esems[d.eng][ep], idx + 1)
                        ins = o.fn(eng)
                        if o.is_dma:
                            ins.then_inc(o.sem, 16)
                        elif o.signals:
                            ep, idx = divmod(o.sigpos, EPOCH)
                            ins.then_inc(esems[e][ep], 1)
                    if e == "sp":
                        for d in final_waits:
                            eng.wait_ge(d.sem, d.semval)
                return body
            for e in ENGS:
                if self.ops[e] or e == "sp":
                    getattr(block, engobj[e])(body_for(e))


from contextlib import ExitStack
import math

S = 2048
D = 2048
NT = 16
CAP = 768
NST = CAP // 128
EPS = 1e-6

WEIGHT_SHAPES = {
    'attn_norm_g': (2, 2048), 'w_in': (2, 2048, 9024), 'mla_q_a_norm_g': (2, 512), 'mla_w_uq': (2, 512, 1536),
    'mla_kv_a_norm_g': (2, 256), 'mla_w_ukv': (2, 256, 2048), 'mla_q_norm_g': (2, 192), 'mla_k_norm_g': (2, 192),
    'mla_w_out': (2, 1024, 2048), 'pool_w': (2, 4, 256, 256), 'pool_scale': (2, 1024), 'pool_w_out': (2, 1024, 2048),
    'mem_norm_g': (2, 2048), 'mem_w_kv': (2, 2048, 2048), 'mem_q_norm_g': (2, 256), 'mem_k_norm_g': (2, 256),
    'mem_w_out': (2, 1024, 2048), 'w_o': (2, 2048, 2048), 'ffn_norm_g': (2, 2048),
    'dense_w_gate': (1, 2048, 5632), 'dense_w_up': (1, 2048, 5632), 'dense_w_down': (1, 5632, 2048),
    'router_w': (1, 2048, 8), 'router_b': (1, 8), 'moe_w_gate': (1, 8, 2048, 7168), 'moe_w_up': (1, 8, 2048, 7168),
    'moe_w_down': (1, 8, 7168, 2048),
}
CONST_SHAPES = {'c_ident': (128, 128), 'c_tri': (128, 128), 'c_ones': (128, 128), 'c_pool': (12, 128, 128),
                'c_mask': (4, 128, 512), 'c_iota': (128, CAP), 'c_invf': (128, 32)}


TILED = {
    'tw_in_a': ('w_in', 1, (0, 512), 2048, 256),
    'tw_in_b': ('w_in', 1, (512, 832), 2048, 160),
    'tw_in_c': ('w_in', 1, (832, 9024), 2048, 256),
    'tw_memkv': ('mem_w_kv', 1, None, 2048, 256),
    'tw_o': ('w_o', 1, None, 2048, 256),
    'tw_mlaout': ('mla_w_out', 1, None, 1024, 512),
    'tw_poolout': ('pool_w_out', 1, None, 1024, 512),
    'tw_memout': ('mem_w_out', 1, None, 1024, 512),
    'tw_dg': ('dense_w_gate', 1, None, 2048, 256),
    'tw_du': ('dense_w_up', 1, None, 2048, 256),
    'tw_dd': ('dense_w_down', 1, None, 512, 512),
    'tw_mg': ('moe_w_gate', 2, None, 2048, 256),
    'tw_mu': ('moe_w_up', 2, None, 2048, 256),
    'tw_md': ('moe_w_down', 2, None, 1024, 512),
}


def tiled_shape(name):
    key, nlead, cr, R, C = TILED[name]
    shp = WEIGHT_SHAPES[key]
    lead, (K, F) = shp[:nlead], shp[nlead:]
    if cr is not None:
        F = cr[1] - cr[0]
    return tuple(lead) + (K // R, F // C, 128, R // 128, C)


def host_tile(name, w):
    key, nlead, cr, R, C = TILED[name]
    if cr is not None:
        w = w[..., cr[0]:cr[1]]
    lead, (K, F) = w.shape[:nlead], w.shape[nlead:]
    w = w.reshape(lead + (K // R, R // 128, 128, F // C, C))
    n = len(lead)
    w = w.transpose(tuple(range(n)) + (n, n + 3, n + 2, n + 1, n + 4))
    return np.ascontiguousarray(w)


class TW:
    def __init__(self, ap, R, C, c_base=0):
        self.ap, self.R, self.C, self.c_base = ap, R, C, c_base

    def slab(self, r0, nrows, c0, ncols):
        assert nrows == self.R and ncols == self.C and r0 % self.R == 0 and (c0 - self.c_base) % self.C == 0, (r0, nrows, c0, ncols, self.R, self.C)
        return self.ap[r0 // self.R, (c0 - self.c_base) // self.C]


def host_constants():
    c = {}
    c['c_ident'] = np.eye(128, dtype=np.float32)
    tri = np.zeros((128, 128), np.float32)
    for a in range(128):
        tri[a, a + 1:] = 1.0
    c['c_tri'] = tri
    c['c_ones'] = np.ones((128, 128), np.float32)
    pm = np.zeros((12, 128, 128), np.float32)
    for g, w in enumerate((2, 4, 8, 16)):
        for t in range(128):
            for d in range(w):
                tp = t - d
                if tp >= 0:
                    pm[3 * g + 1, tp, t] += 1.0 / w
                else:
                    pm[3 * g + 2, 128 + tp, t] += 1.0 / w
            pm[3 * g + 1, t, t] -= 1.0
            cnt = min(t + 1, w)
            for d in range(cnt):
                pm[3 * g + 0, t - d, t] += 1.0 / cnt
            pm[3 * g + 0, t, t] -= 1.0
    c['c_pool'] = pm
    mk = np.zeros((4, 128, 512), np.float32)
    for m in range(4):
        for kp in range(128):
            mk[m, kp, :] = (np.arange(512) >= 128 * m + kp)
    c['c_mask'] = mk
    c['c_iota'] = np.broadcast_to(np.arange(CAP, dtype=np.float32)[None, :], (128, CAP)).copy()
    invf = (np.float32(10000.0) ** (-np.arange(0, 64, 2, dtype=np.float32) / np.float32(64))).astype(np.float32)
    c['c_invf'] = np.broadcast_to(invf[None, :], (128, 32)).copy()
    return c


class _Stop(Exception):
    pass


def build_program(layers=(0, 1), parts=('mixer', 'ffn'), dbg_names=(), stop_at=None):
    nc = bass.Bass("TRN2", target_bir_lowering=False)
    class LazyInputs(dict):
        def __missing__(self, k):
            shp = {'x': (S, D), 'mem': (256, D), 'pos': (128, 16)}.get(k) or (tiled_shape(k) if k in TILED else None) or WEIGHT_SHAPES.get(k) or CONST_SHAPES[k]
            v = nc.dram_tensor(k, list(shp), I32 if k == 'pos' else F32, kind="ExternalInput").ap()
            self[k] = v
            return v
    T = LazyInputs()
    nc.used_inputs = T
    OUT = nc.dram_tensor('out', [S, D], F32, kind="ExternalOutput").ap()

    def scratch(name, shape, dt):
        kind = "ExternalOutput" if name in dbg_names else "Internal"
        return nc.dram_tensor(name, list(shape), dt, kind=kind).ap()

    mixT_d = scratch('mixT_d', [1024, S], BF16)
    xattT_d = scratch('xattT_d', [1024, S], BF16)
    attT_d = scratch('attT_d', [1024, S], BF16)
    gT_d = scratch('gT_d', [6144, S], BF16)
    htm_d = scratch('htm_d', [S, D], BF16)
    qnT_d = scratch('qnT_d', [1024, S], BF16)
    qrT_d = scratch('qrT_d', [512, S], BF16)
    knT_d = scratch('knT_d', [1024, S], BF16)
    krT_d = scratch('krT_d', [64, S], BF16)
    V_d = scratch('V_d', [S, 1024], BF16)
    hT_dbg = scratch('hT_dbg', [D, S], BF16) if 'hT_dbg' in dbg_names else None
    mrg_dbg = scratch('mrg_dbg', [D, S], BF16) if 'mrg_dbg' in dbg_names else None
    rt_dbg = scratch('rt_dbg', [128, 3 * 128], F32) if 'rt_dbg' in dbg_names else None

    with ExitStack() as st:
        P = Prog(nc, st)
        finals = []

        def chk(name):
            if stop_at == name:
                raise _Stop()

        RA_E = 56 * CAP
        RA = P.sbuf('RA', [128, RA_E], BF16)
        RAq = [Buf(f'RA_q{i}') for i in range(4)] + [Buf('RA_tail')]
        RB_E = 32768
        RB = P.sbuf('RB', [128, RB_E], BF16)
        RBB = [Buf(f'RB_{i}') for i in range(RB_E // 2048)]

        def rb(off_kb, size_kb, dt=BF16):
            a, b = off_kb * 512, (off_kb + size_kb) * 512
            a, b = int(a), int(b)
            ap = RB[:, a:b]
            if dt is F32:
                ap = ap.bitcast(F32)
            return ap, RBB[a // 2048:(b + 2047) // 2048]

        SLAB_E = 4096
        slabs = [(P.sbuf(f'slab{i}', [128, SLAB_E], BF16), Buf(f'slab{i}')) for i in range(4)]
        GAIN = P.sbuf('GAIN', [128, 1664], F32)
        bgain = Buf('gain')
        PSC = P.sbuf('PSC', [128, 8], F32)
        STAT = [(P.sbuf(f'stat{i}', [128, 64], F32), Buf(f'stat{i}')) for i in range(4)]
        SC = P.sbuf('SC', [128, 16, 64], F32)
        SIN = SC[:, :, 0:32]
        COS = SC[:, :, 32:64]
        bcs = Buf('cossin')
        SCK = P.sbuf('SCK', [128, 16, 8], F32)
        bsck = Buf('scaleK')
        SCM = P.sbuf('SCM', [128, 2, 4], F32)
        bscm = Buf('scaleM')
        ident = P.sbuf('ident', [128, 128], BF16)
        identf = P.sbuf('identf', [128, 128], F32)
        onesb = P.sbuf('onesb', [128, 128], BF16)
        trib = P.sbuf('trib', [128, 128], BF16)
        poolm = P.sbuf('poolm', [128, 12, 128], BF16)
        maskb = P.sbuf('maskb', [128, 4, 512], BF16)
        iota = P.sbuf('iota', [128, CAP], F32)
        invf = P.sbuf('invf', [128, 32], F32)
        pib = P.sbuf('pib', [128, 1], F32)
        bconst = Buf('const', const=True)
        RSEL = P.sbuf('RSEL', [128, 16, 8], F32)
        RWT = P.sbuf('RWT', [128, 16, 8], F32)
        RPOS = P.sbuf('RPOS', [128, 16, 8], F32)
        RSELB = P.sbuf('RSELB', [128, 16, 8], BF16)
        brt = Buf('router')
        RW = P.sbuf('RW', [128, 16, 8], F32)
        RBI = P.sbuf('RBI', [128, 8], F32)
        brw = Buf('rw')

        MM = [(P.psum(f'mm{i}', [128, 512], F32), Buf(f'mm{i}', excl=True)) for i in range(5)]
        TP = [(P.psum(f'tp{i}', [128, 1024], BF16)[:, 0:512], Buf(f'tp{i}', excl=True)) for i in range(2)]
        AUXt = P.psum('aux', [128, 512], F32)
        AUX = (AUXt, Buf('aux', excl=True))
        MM6 = MM + [AUX]
        rr = {}

        def nxt(kind, lst, n=None):
            n = len(lst) if n is None else n
            i = rr.get(kind, 0) % n
            rr[kind] = i + 1
            return lst[i]

        def ev_eng():
            rr['ev'] = rr.get('ev', 0) ^ 1
            return 'act' if rr['ev'] else 'dve'

        def copy_op(eng, out, in_, reads, writes, join=False):
            if eng == 'act':
                return P.op('act', lambda e: e.activation(out=out, in_=in_, func=AF.Copy), reads=reads, writes=writes, join=join)
            return P.op(eng, lambda e: e.tensor_copy(out=out, in_=in_), reads=reads, writes=writes, join=join)

        ybuf = [Buf(f'y{t}') for t in range(NT)]

        def cload(dst, src):
            P.dma('pool', dst, src, writes=[bconst], join=True)
        cload(ident[:], T['c_ident'])
        cload(identf[:], T['c_ident'])
        cload(onesb[:], T['c_ones'])
        cload(trib[:], T['c_tri'])
        cload(poolm[:], T['c_pool'].rearrange("k p f -> p k f"))
        cload(maskb[:], T['c_mask'].rearrange("k p f -> p k f"))
        cload(iota[:], T['c_iota'])
        cload(invf[:], T['c_invf'])

        def load_slab(W2d, r0, nrows, c0, ncols, dst=None):
            if dst is None:
                tile, buf = nxt('slab', slabs)
                bufs = [buf]
                flat = tile[:, :]
            else:
                flat, bufs = dst
            kc = nrows // 128
            assert nrows % 128 == 0 and kc * ncols <= flat.shape[1], (nrows, ncols, flat.shape)
            view = flat[:, 0:kc * ncols].rearrange("p (j f) -> p j f", f=ncols)
            if hasattr(W2d, 'slab'):
                P.dma('pool', view, W2d.slab(r0, nrows, c0, ncols), writes=bufs)
                return view, bufs
            src = W2d[r0:r0 + nrows, c0:c0 + ncols].rearrange("(j p) f -> p j f", p=128)
            step = 8
            for j0 in range(0, kc, step):
                j1 = min(kc, j0 + step)
                P.dma('pool', view[:, j0:j1, :], src[:, j0:j1, :], writes=bufs, join=(j0 > 0))
            return view, bufs

        def bcast_row(dst, row_ap, bufs, first=True):
            P.dma('sp', dst, row_ap.partition_broadcast(128), writes=bufs, join=not first)

        def mm_acc(ps, psb, pairs, reads, first=True, last=True):
            n = len(pairs)
            for i, (l, r) in enumerate(pairs):
                P.op('pe', lambda e, l=l, r=r, i=i: e.matmul(ps, lhsT=l, rhs=r, start=(first and i == 0), stop=(last and i == n - 1)),
                     reads=reads, writes=[psb], join=not (first and i == 0))

        def transposes(srcs, src_reads, dst_fn, dst_writes, width=128):
            for q in range(0, len(srcs), 4):
                m = min(4, len(srcs) - q)
                tp, tpb = nxt('tp', TP)
                tpv = tp.rearrange("p (a b) -> p a b", b=128)
                for i in range(m):
                    P.op('pe', lambda e, i=i, s=srcs[q + i], tpv=tpv: e.transpose(out=tpv[0:width, i, :], in_=s, identity=ident[:]),
                         reads=src_reads + [bconst], writes=[tpb], join=(i > 0))
                copy_op(ev_eng(), dst_fn(q, m), tpv[0:width, 0:m, :], [tpb], dst_writes, join=True)

        def rstd_from_ss(ss_ap, out_ap, n, statb, scale_extra=1.0):
            P.op('dve', lambda e: e.tensor_scalar(out=out_ap, in0=ss_ap, scalar1=1.0 / n, scalar2=EPS, op0=ALU.mult, op1=ALU.add),
                 reads=statb, writes=statb, join=True)
            P.op('act', lambda e: e.activation(out=out_ap, in_=out_ap, func=AF.Sqrt), reads=statb, writes=statb, join=True)
            P.op('dve', lambda e: e.reciprocal(out=out_ap, in_=out_ap), reads=statb, writes=statb, join=True)
            if scale_extra != 1.0:
                P.op('dve', lambda e: e.tensor_scalar(out=out_ap, in0=out_ap, scalar1=float(scale_extra), scalar2=None, op0=ALU.mult),
                     reads=statb, writes=statb, join=True)

        def store(dram_ap, sb_ap, reads, dbuf, join=True, queue='sp'):
            return P.dma(queue, dram_ap, sb_ap, reads=reads, writes=[dbuf], join=join)

        def phase_rope_tables():
            posi = P.sbuf('posi', [128, 16], I32)
            posf = P.sbuf('posf', [128, 16], F32)
            argt_, b1 = rb(0, 4, F32)
            nf_, b2 = rb(4, 4, F32)
            ni_, b3 = rb(8, 4, F32)
            argt = argt_.rearrange("p (t f) -> p t f", f=64)
            nf = nf_.rearrange("p (t f) -> p t f", f=64)
            ni = ni_.bitcast(I32).rearrange("p (t f) -> p t f", f=64)
            bp = Buf('posi')
            J = dict(reads=[bp, bconst] + b1 + b2 + b3, writes=[bp] + b1 + b2 + b3, join=True)
            P.dma('sp', posi[:], T['pos'], writes=[bp])
            P.op('dve', lambda e: e.tensor_copy(out=posf[:], in_=posi[:]), reads=[bp], writes=[bp])
            for t in range(NT):
                P.op('dve', lambda e, t=t: e.tensor_scalar(out=argt[:, t, 0:32], in0=invf[:], scalar1=posf[:, t:t + 1], scalar2=None, op0=ALU.mult), **J)
            P.op('dve', lambda e: e.tensor_scalar(out=argt[:, :, 32:64], in0=argt[:, :, 0:32], scalar1=math.pi / 2, scalar2=None, op0=ALU.add), **J)
            P.op('dve', lambda e: e.tensor_scalar(out=nf[:], in0=argt[:], scalar1=1.0 / (2 * math.pi), scalar2=None, op0=ALU.mult), **J)
            P.op('dve', lambda e: e.tensor_copy(out=ni[:], in_=nf[:]), **J)
            P.op('dve', lambda e: e.tensor_copy(out=nf[:], in_=ni[:]), **J)
            P.op('dve', lambda e: e.scalar_tensor_tensor(out=argt[:], in0=nf[:], scalar=-2 * math.pi, in1=argt[:], op0=ALU.mult, op1=ALU.add), **J)
            P.op('dve', lambda e: e.tensor_scalar(out=nf[:], in0=argt[:], scalar1=math.pi, scalar2=None, op0=ALU.is_gt), **J)
            P.op('dve', lambda e: e.scalar_tensor_tensor(out=argt[:], in0=nf[:], scalar=-2 * math.pi, in1=argt[:], op0=ALU.mult, op1=ALU.add), **J)
            P.op('dve', lambda e: e.tensor_scalar(out=nf[:], in0=argt[:], scalar1=-math.pi, scalar2=None, op0=ALU.is_lt), **J)
            P.op('dve', lambda e: e.scalar_tensor_tensor(out=argt[:], in0=nf[:], scalar=2 * math.pi, in1=argt[:], op0=ALU.mult, op1=ALU.add), **J)
            P.op('act', lambda e: e.activation(out=SC[:], in_=argt[:], func=AF.Sin), reads=[bp] + b1, writes=[bcs])

        bhtm = Buf('htm_d')

        def phase_norm(src2d, ntok, g_row, mode, dst_view=None, dst_bufs_fn=None, src_is_out=False, router=False):
            GBv, GBb = rb(24, 8, F32)
            bcast_row(GBv, g_row, GBb)
            for t in range(ntok // 128):
                xt, xb = rb(8 * (t % 2), 8, F32)
                hb, hbb = rb(16 + 4 * (t % 2), 4)
                stt, stb = nxt('stat', STAT)
                P.dma('sp', xt, src2d[t * 128:(t + 1) * 128, :], reads=[ybuf[t]] if src_is_out else [], writes=xb)
                chk('n0')
                P.op('act', lambda e, xt=xt, hb=hb, stt=stt: e.activation(out=hb, in_=xt, func=AF.Square, accum_out=stt[:, 0:1]),
                     reads=xb, writes=hbb + [stb])
                chk('n1')
                rstd_from_ss(stt[:, 0:1], stt[:, 1:2], 2048, [stb])
                chk('n2')
                if router:
                    hf, hfb = rb(32, 8, F32)
                    P.op('dve', lambda e, xt=xt, hf=hf, stt=stt: e.scalar_tensor_tensor(out=hf, in0=xt, scalar=stt[:, 1:2], in1=GBv,
                                                                                      op0=ALU.mult, op1=ALU.mult), reads=xb + [stb] + GBb, writes=hfb)
                    P.op('act', lambda e, hb=hb, hf=hf: e.activation(out=hb, in_=hf, func=AF.Copy), reads=hfb, writes=hbb)
                    router_tile(t, hf, hfb)
                else:
                    P.op('dve', lambda e, xt=xt, hb=hb, stt=stt: e.scalar_tensor_tensor(out=hb, in0=xt, scalar=stt[:, 1:2], in1=GBv,
                                                                                      op0=ALU.mult, op1=ALU.mult), reads=xb + [stb] + GBb, writes=hbb)
                chk('n3')
                if mode == 'fm':
                    transposes([hb[:, c * 128:(c + 1) * 128] for c in range(16)], hbb,
                               lambda q, m, t=t: dst_view[:, q:q + m, t * 128:(t + 1) * 128], dst_bufs_fn(t))
                else:
                    store(htm_d[t * 128:(t + 1) * 128, :], hb, hbb, bhtm, join=(t > 0))
                chk('n4')
                chk('n4x')

        def router_tile(t, hf, hfb):
            ht32, htb = rb(40, 8, F32)
            htv = ht32.rearrange("p (c t) -> p c t", t=128)
            for q in range(0, 16, 4):
                ps, psb = nxt('mm', MM)
                psv = ps.rearrange("p (a b) -> p a b", b=128)
                for i in range(4):
                    P.op('pe', lambda e, i=i, q=q, psv=psv: e.transpose(out=psv[:, i, :], in_=hf[:, (q + i) * 128:(q + i + 1) * 128], identity=identf[:]),
                         reads=hfb + [bconst], writes=[psb], join=(i > 0))
                copy_op(ev_eng(), htv[:, q:q + 4, :], psv[:, :, :], [psb], htb, join=(q > 0))
            ps, psb = AUX
            mm_acc(ps[:, 0:8], psb, [(htv[:, j, :], RW[:, j, :]) for j in range(16)], htb + [brw])
            stt, stb = nxt('stat', STAT)
            lg = stt[:, 0:8]
            P.op('dve', lambda e: e.tensor_tensor(out=lg, in0=ps[:, 0:8], in1=RBI[:], op=ALU.add), reads=[psb, brw], writes=[stb])
            m1, eq1, l2, m2, eq2, dd = stt[:, 8:9], stt[:, 16:24], stt[:, 24:32], stt[:, 9:10], stt[:, 32:40], stt[:, 10:13]
            J = dict(reads=[stb], writes=[stb], join=True)
            P.op('dve', lambda e: e.reduce_max(out=m1, in_=lg, axis=AX.X), **J)
            P.op('dve', lambda e: e.tensor_scalar(out=eq1, in0=lg, scalar1=m1, scalar2=None, op0=ALU.is_equal), **J)
            P.op('dve', lambda e: e.scalar_tensor_tensor(out=l2, in0=eq1, scalar=-1e30, in1=lg, op0=ALU.mult, op1=ALU.add), **J)
            P.op('dve', lambda e: e.reduce_max(out=m2, in_=l2, axis=AX.X), **J)
            P.op('dve', lambda e: e.tensor_scalar(out=eq2, in0=l2, scalar1=m2, scalar2=None, op0=ALU.is_equal), **J)
            P.op('dve', lambda e: e.tensor_tensor(out=dd[:, 0:1], in0=m2, in1=m1, op=ALU.subtract), **J)
            P.op('act', lambda e: e.activation(out=dd[:, 0:1], in_=dd[:, 0:1], func=AF.Exp), **J)
            P.op('dve', lambda e: e.tensor_scalar(out=dd[:, 1:2], in0=dd[:, 0:1], scalar1=1.0, scalar2=None, op0=ALU.add), **J)
            P.op('dve', lambda e: e.reciprocal(out=dd[:, 1:2], in_=dd[:, 1:2]), **J)
            P.op('dve', lambda e: e.tensor_tensor(out=dd[:, 2:3], in0=dd[:, 0:1], in1=dd[:, 1:2], op=ALU.mult), **J)
            P.op('dve', lambda e: e.tensor_tensor(out=RSEL[:, t, :], in0=eq1, in1=eq2, op=ALU.add), reads=[stb], writes=[brt], join=True)
            P.op('dve', lambda e: e.tensor_scalar(out=RWT[:, t, :], in0=eq1, scalar1=dd[:, 1:2], scalar2=None, op0=ALU.mult), reads=[stb], writes=[brt], join=True)
            P.op('dve', lambda e: e.scalar_tensor_tensor(out=RWT[:, t, :], in0=eq2, scalar=dd[:, 2:3], in1=RWT[:, t, :], op0=ALU.mult, op1=ALU.add),
                 reads=[stb, brt], writes=[brt], join=True)
            P.op('dve', lambda e: e.tensor_copy(out=RSELB[:, t, :], in_=RSEL[:, t, :]), reads=[brt], writes=[brt], join=True)

        bq_d, bkn_d, bkr_d, bv_d = Buf('qT_d'), Buf('knT_d'), Buf('krT_d'), Buf('V_d')
        bmix_d = [Buf(f'mix_d{i}') for i in range(4)]
        bxat_d = [Buf(f'xat_d{i}') for i in range(4)]
        batt_d = [Buf(f'att_d{i}') for i in range(4)]
        bg_d = [Buf(f'g_d{i}') for i in range(4)]

        def phase_mixer(l):
            y_src = T['x'] if l == layers[0] else OUT
            tw_a, tw_b, tw_c = TW(T['tw_in_a'][l], 2048, 256, 0), TW(T['tw_in_b'][l], 2048, 160, 512), TW(T['tw_in_c'][l], 2048, 256, 832)

            class _WIn:
                def slab(self, r0, nrows, c0, ncols):
                    return (tw_a if c0 < 512 else tw_b if c0 < 832 else tw_c).slab(r0, nrows, c0, ncols)
            w_in = _WIn()
            hTv = RA[:, 0:16 * S].rearrange("p (c t) -> p c t", t=S)
            hbuf = lambda t: [RAq[t // 4]]

            phase_norm(y_src, S, T['attn_norm_g'][l], 'fm', hTv, hbuf, src_is_out=(l > layers[0]))
            if hT_dbg is not None and l == 0:
                bdb = Buf('hT_dbg')
                for c in range(16):
                    finals.append(store(hT_dbg[c * 128:(c + 1) * 128, :], hTv[:, c, :], RAq[0:4], bdb, join=(c > 0)))
            memnT, memb = rb(32, 8)
            memv = memnT.rearrange("p (c t) -> p c t", t=256)
            phase_norm(T['mem'], 256, T['mem_norm_g'][l], 'fm', memv, lambda t: memb)
            bcast_row(GAIN[:, 0:512], T['mla_q_a_norm_g'][l], [bgain], first=True)
            bcast_row(GAIN[:, 512:768], T['mla_kv_a_norm_g'][l], [bgain], first=False)
            bcast_row(GAIN[:, 768:960], T['mla_q_norm_g'][l], [bgain], first=False)
            bcast_row(GAIN[:, 960:1152], T['mla_k_norm_g'][l], [bgain], first=False)
            bcast_row(GAIN[:, 1152:1408], T['mem_q_norm_g'][l], [bgain], first=False)
            bcast_row(GAIN[:, 1408:1664], T['mem_k_norm_g'][l], [bgain], first=False)
            P.dma('sp', PSC[:], T['pool_scale'][l].rearrange("(c p) -> p c", p=128), writes=[bgain], join=True, allow_slow_non_contiguous=True)
            P.op('dve', lambda e: e.tensor_tensor(out=GAIN[:, 768:896], in0=GAIN[:, 768:896], in1=GAIN[:, 960:1088], op=ALU.mult),
                 reads=[bgain], writes=[bgain], join=True)
            P.op('dve', lambda e: e.tensor_tensor(out=GAIN[:, 1152:1408], in0=GAIN[:, 1152:1408], in1=GAIN[:, 1408:1664], op=ALU.mult),
                 reads=[bgain], writes=[bgain], join=True)
            G_QA, G_KVA, G_Q, G_KPE, G_QM = GAIN[:, 0:512], GAIN[:, 512:768], GAIN[:, 768:960], GAIN[:, 1088:1152], GAIN[:, 1152:1408]

            chk('A')
            kmT, kmb = rb(40, 4)
            kmv = kmT.rearrange("p (c t) -> p c t", t=256)
            vm, vmb = rb(44, 4)
            vmv = vm.rearrange("p (m f) -> p m f", f=1024)
            wkv = TW(T['tw_memkv'][l], 2048, 256)
            for cb in range(8):
                sl, slb = load_slab(wkv, 0, 2048, cb * 256, 256)
                if cb < 4:
                    for c in range(2):
                        ps, psb = nxt('mm', MM)
                        mm_acc(ps[:, 0:256], psb, [(sl[:, j, c * 128:(c + 1) * 128], memv[:, j, :]) for j in range(16)], slb + memb)
                        copy_op(ev_eng(), kmv[:, cb * 2 + c, :], ps[:, 0:256], [psb], kmb, join=True)
                    for mt in range(2):
                        ps, psb = nxt('mm', MM)
                        mm_acc(ps[:, 0:256], psb, [(memv[:, j, mt * 128:(mt + 1) * 128], sl[:, j, :]) for j in range(16)], slb + memb)
                        jk, jkb = rb(16, 4)
                        P.op('act', lambda e, ps=ps, jk=jk, mt=mt, cb=cb: e.activation(out=jk[:, 0:256], in_=ps[:, 0:256], func=AF.Square,
                                                                                    accum_out=SCM[:, mt, cb:cb + 1]), reads=[psb], writes=jkb + [bscm])
                else:
                    for mt in range(2):
                        ps, psb = nxt('mm', MM)
                        mm_acc(ps[:, 0:256], psb, [(memv[:, j, mt * 128:(mt + 1) * 128], sl[:, j, :]) for j in range(16)], slb + memb)
                        copy_op(ev_eng(), vmv[:, mt, (cb - 4) * 256:(cb - 3) * 256], ps[:, 0:256], [psb], vmb, join=True)
            scm2 = SCM[:, :, :].rearrange("p a b -> p (a b)")
            rstd_from_ss(scm2, scm2, 256, [bscm], scale_extra=256 ** -0.5)

            chk('Bmem')
            wuq, wuqb = rb(48, 12)
            wuqv, _ = load_slab(T['mla_w_uq'][l], 0, 512, 0, 1536, dst=(wuq, wuqb))
            wkvA, wkvAb = rb(32, 4)
            wkvAv, _ = load_slab(T['mla_w_ukv'][l], 0, 256, 0, 1024, dst=(wkvA, wkvAb))
            wkvB, wkvBb = rb(36, 4)
            wkvBv, _ = load_slab(T['mla_w_ukv'][l], 0, 256, 1024, 1024, dst=(wkvB, wkvBb))

            def tm_block(t, sl2, w):
                ps, psb = nxt('mm', MM)
                for half, (sl, slb) in enumerate(sl2):
                    for j in range(16):
                        P.op('pe', lambda e, j=j, sl=sl, ps=ps, t=t, half=half: e.matmul(ps[:, half * w:(half + 1) * w], lhsT=hTv[:, j, t * 128:(t + 1) * 128],
                                                                                        rhs=sl[:, j, :], start=(j == 0), stop=(j == 15)),
                             reads=hbuf(t) + slb, writes=[psb], join=not (half == 0 and j == 0))
                return ps, psb

            sl2 = [load_slab(w_in, 0, 2048, 0, 256), load_slab(w_in, 0, 2048, 256, 256)]
            for t in range(NT):
                ps, psb = tm_block(t, sl2, 256)
                stt, stb = nxt('stat', STAT)
                jk, jkb = rb(16, 4)
                P.op('act', lambda e, ps=ps, jk=jk, stt=stt: e.activation(out=jk[:, 0:512], in_=ps[:, :], func=AF.Square, accum_out=stt[:, 0:1]),
                     reads=[psb], writes=jkb + [stb])
                rstd_from_ss(stt[:, 0:1], stt[:, 1:2], 512, [stb])
                cqn, cqnb = rb(20, 4)
                P.op('dve', lambda e, ps=ps, cqn=cqn, stt=stt: e.scalar_tensor_tensor(out=cqn[:, 0:512], in0=ps[:, :], scalar=stt[:, 1:2], in1=G_QA,
                                                                                    op0=ALU.mult, op1=ALU.mult), reads=[psb, stb, bgain], writes=cqnb)
                cqT, cqTb = rb(0, 1)
                cqTv = cqT.rearrange("p (c t) -> p c t", t=128)
                transposes([cqn[:, c * 128:(c + 1) * 128] for c in range(4)], cqnb, lambda q, m: cqTv[:, q:q + m, :], cqTb[0:1])
                qf, qfb = rb(8, 8, F32)
                for nb in range(3):
                    ps2, ps2b = nxt('mm', MM)
                    mm_acc(ps2[:, :], ps2b, [(cqTv[:, j, :], wuqv[:, j, nb * 512:(nb + 1) * 512]) for j in range(4)], cqTb[0:1] + wuqb)
                    copy_op(ev_eng(), qf[:, nb * 512:(nb + 1) * 512], ps2[:, :], [ps2b], qfb, join=True)
                q_epilogue(t, qf, qfb, G_Q)

            chk('Bcq')
            sl2 = [load_slab(w_in, 0, 2048, 512, 160), load_slab(w_in, 0, 2048, 672, 160)]
            for t in range(NT):
                ps, psb = tm_block(t, sl2, 160)
                chk('k0')
                kv_epilogue(t, ps, psb, G_KVA, G_KPE, wkvAv, wkvAb, wkvBv, wkvBb)

            chk('Bckv')
            pwt, pwtb = rb(48, 4)
            pwv = pwt.rearrange("p (g j f) -> p g j f", g=4, j=2)
            for g in range(4):
                P.dma('pool', pwv[:, g, :, :], T['pool_w'][l][g].rearrange("(j p) f -> p j f", p=128), writes=pwtb, join=(g > 0))
            for ub in range(2):
                sl2 = [load_slab(w_in, 0, 2048, 832 + ub * 512, 256), load_slab(w_in, 0, 2048, 832 + ub * 512 + 256, 256)]
                for t in range(NT):
                    ps, psb = tm_block(t, sl2, 256)
                    ucur, ucb = rb(0 + (t % 2), 1)
                    copy_op(ev_eng(), ucur[:, 0:512], ps[:, :], [psb], ucb[0:1])
                    uprev, upb = rb(0 + ((t + 1) % 2), 1)
                    pp, ppb = nxt('mm', MM)
                    for gg in range(2):
                        g = 2 * ub + gg
                        pairs = [(poolm[:, 3 * g + (1 if t > 0 else 0), :], ucur[:, gg * 256:(gg + 1) * 256])]
                        if t > 0:
                            pairs.append((poolm[:, 3 * g + 2, :], uprev[:, gg * 256:(gg + 1) * 256]))
                        n = len(pairs)
                        for i, (lh, rh) in enumerate(pairs):
                            P.op('pe', lambda e, lh=lh, rh=rh, i=i, n=n, pp=pp, gg=gg: e.matmul(pp[:, gg * 256:(gg + 1) * 256], lhsT=lh, rhs=rh, start=(i == 0), stop=(i == n - 1)),
                                 reads=ucb[0:1] + upb[0:1] + [bconst], writes=[ppb], join=not (gg == 0 and i == 0))
                    pl, plb = rb(2, 1)
                    copy_op(ev_eng(), pl[:, 0:512], pp[:, :], [ppb], plb[0:1])
                    plT, plTb = rb(3, 1)
                    plTv = plT.rearrange("p (c t) -> p c t", t=128)
                    transposes([pl[:, c * 128:(c + 1) * 128] for c in range(4)], plb[0:1], lambda q, m: plTv[:, q:q + m, :], plTb[0:1])
                    pm_, pmb = nxt('mm', MM)
                    for gg in range(2):
                        g = 2 * ub + gg
                        for hc in range(2):
                            for j in range(2):
                                P.op('pe', lambda e, g=g, gg=gg, hc=hc, j=j, pm_=pm_: e.matmul(pm_[:, (gg * 2 + hc) * 128:(gg * 2 + hc + 1) * 128], lhsT=pwv[:, g, j, hc * 128:(hc + 1) * 128],
                                                                                            rhs=plTv[:, gg * 2 + j, :], start=(j == 0), stop=(j == 1)),
                                     reads=plTb[0:1] + pwtb, writes=[pmb], join=not (gg == 0 and hc == 0 and j == 0))
                    mx, mxb = rb(4 + (t % 2), 1)
                    mxv = mx.rearrange("p (c t) -> p c t", t=128)
                    for cc in range(4):
                        P.op('dve', lambda e, cc=cc, mxv=mxv, pm_=pm_, ub=ub: e.tensor_scalar(out=mxv[:, cc, :], in0=pm_[:, cc * 128:(cc + 1) * 128],
                                                                                             scalar1=PSC[:, ub * 4 + cc:ub * 4 + cc + 1], scalar2=None, op0=ALU.mult),
                             reads=[pmb, bgain], writes=mxb[0:1], join=(cc > 0))
                    store(mixT_d[ub * 512:(ub + 1) * 512, t * 128:(t + 1) * 128].rearrange("(c p) t -> p c t", p=128), mxv, mxb[0:1], bmix_d[t // 4])

            chk('Bu')
            for qb in range(2):
                sl2 = [load_slab(w_in, 0, 2048, 1856 + qb * 512, 256), load_slab(w_in, 0, 2048, 1856 + qb * 512 + 256, 256)]
                for t in range(NT):
                    ps, psb = tm_block(t, sl2, 256)
                    stt, stb = nxt('stat', STAT)
                    jk, jkb = rb(16, 4)
                    for hh in range(2):
                        P.op('act', lambda e, ps=ps, jk=jk, stt=stt, hh=hh: e.activation(out=jk[:, hh * 256:(hh + 1) * 256], in_=ps[:, hh * 256:(hh + 1) * 256], func=AF.Square,
                                                                                        accum_out=stt[:, hh:hh + 1]), reads=[psb], writes=jkb + [stb], join=(hh > 0))
                    rstd_from_ss(stt[:, 0:2], stt[:, 2:4], 256, [stb])
                    qmn, qmnb = rb(20, 4)
                    for hh in range(2):
                        P.op('dve', lambda e, ps=ps, qmn=qmn, stt=stt, hh=hh: e.scalar_tensor_tensor(out=qmn[:, hh * 256:(hh + 1) * 256], in0=ps[:, hh * 256:(hh + 1) * 256],
                                                                                                 scalar=stt[:, 2 + hh:3 + hh], in1=G_QM, op0=ALU.mult, op1=ALU.mult),
                             reads=[psb, stb, bgain], writes=qmnb, join=(hh > 0))
                    qmT, qmTb = rb(0, 1)
                    qmTv = qmT.rearrange("p (c t) -> p c t", t=128)
                    transposes([qmn[:, c * 128:(c + 1) * 128] for c in range(4)], qmnb, lambda q, m: qmTv[:, q:q + m, :], qmTb[0:1])
                    xo, xob = rb(4 + (t % 2), 1)
                    xov = xo.rearrange("p (c t) -> p c t", t=128)
                    for hh in range(2):
                        h = 2 * qb + hh
                        pT, pTb = rb(2, 1)
                        pTv = pT[:, 0:256].rearrange("p (m t) -> p m t", t=128)
                        for mt in range(2):
                            ps2, ps2b = nxt('mm', MM)
                            mm_acc(ps2[:, 0:128], ps2b, [(kmv[:, 2 * h + dd, mt * 128:(mt + 1) * 128], qmTv[:, 2 * hh + dd, :]) for dd in range(2)], kmb + qmTb[0:1])
                            P.op('act', lambda e, ps2=ps2, pTv=pTv, mt=mt, h=h: e.activation(out=pTv[:, mt, :], in_=ps2[:, 0:128], func=AF.Exp, scale=SCM[:, mt, h:h + 1]),
                                 reads=[ps2b, bscm], writes=pTb[0:1], join=(mt > 0))
                        ps3, ps3b = nxt('mm', MM)
                        first = True
                        for dv in range(2):
                            for mt in range(2):
                                P.op('pe', lambda e, dv=dv, mt=mt, h=h, ps3=ps3, pTv=pTv: e.matmul(ps3[:, dv * 128:(dv + 1) * 128], lhsT=vmv[:, mt, h * 256 + dv * 128:h * 256 + (dv + 1) * 128],
                                                                                              rhs=pTv[:, mt, :], start=(mt == 0), stop=(mt == 1)),
                                     reads=vmb + pTb[0:1], writes=[ps3b], join=not first)
                                first = False
                        for mt in range(2):
                            P.op('pe', lambda e, mt=mt, ps3=ps3, pTv=pTv: e.matmul(ps3[:, 256:384], lhsT=onesb[:], rhs=pTv[:, mt, :], start=(mt == 0), stop=(mt == 1)),
                                 reads=pTb[0:1] + [bconst], writes=[ps3b], join=True)
                        rc, rcb = rb(3, 1, F32)
                        P.op('dve', lambda e, rc=rc, ps3=ps3: e.reciprocal(out=rc[:, 0:128], in_=ps3[:, 256:384]), reads=[ps3b], writes=rcb[0:1])
                        for dv in range(2):
                            P.op('dve', lambda e, dv=dv, hh=hh, xov=xov, ps3=ps3, rc=rc: e.tensor_tensor(out=xov[:, hh * 2 + dv, :], in0=ps3[:, dv * 128:(dv + 1) * 128], in1=rc[:, 0:128], op=ALU.mult),
                                 reads=[ps3b] + rcb[0:1], writes=xob[0:1], join=not (hh == 0 and dv == 0))
                    store(xattT_d[qb * 512:(qb + 1) * 512, t * 128:(t + 1) * 128].rearrange("(c p) t -> p c t", p=128), xov, xob[0:1], bxat_d[t // 4])

            chk('Bqm')
            for cb in range(24):
                sl, slb = load_slab(w_in, 0, 2048, 2880 + cb * 256, 256)
                for c in range(2):
                    for qt in range(4):
                        ps, psb = nxt('mm', MM)
                        mm_acc(ps[:, :], psb, [(sl[:, j, c * 128:(c + 1) * 128], hTv[:, j, qt * 512:(qt + 1) * 512]) for j in range(16)], slb + [RAq[qt]])
                        sg, sgb = rb(16 + 4 * (rr.get('sg', 0) % 4), 1)
                        rr['sg'] = rr.get('sg', 0) + 1
                        P.op('act', lambda e, sg=sg, ps=ps: e.activation(out=sg[:, 0:512], in_=ps[:, :], func=AF.Sigmoid), reads=[psb], writes=sgb[0:1])
                        r0 = cb * 256 + c * 128
                        store(gT_d[r0:r0 + 128, qt * 512:(qt + 1) * 512], sg[:, 0:512], sgb[0:1], bg_d[qt])

            chk('C')
            phase_attention()

            chk('D')
            mrgv = RA[:, 0:16 * S].rearrange("p (c t) -> p c t", t=S)
            wouts = (TW(T['tw_mlaout'][l], 1024, 512), TW(T['tw_poolout'][l], 1024, 512), TW(T['tw_memout'][l], 1024, 512))
            srcs = (attT_d, mixT_d, xattT_d)
            sbufs = (batt_d, bmix_d, bxat_d)
            for qt in range(4):
                xs = []
                for b in range(3):
                    xt_, xtb = rb(8 * b, 8)
                    xv = xt_.rearrange("p (c t) -> p c t", t=512)
                    P.dma('sp', xv, srcs[b][:, qt * 512:(qt + 1) * 512].rearrange("(c p) t -> p c t", p=128), reads=[sbufs[b][qt]], writes=xtb)
                    xs.append((xv, xtb))
                for cb in range(4):
                    acc, accb = rb(24, 8, F32)
                    accv = acc.rearrange("p (c t) -> p c t", t=512)
                    for b in range(3):
                        sl, slb = load_slab(wouts[b], 0, 1024, cb * 512, 512)
                        gt, gtb = rb(32 + 4 * (b % 2), 4)
                        gtv = gt.rearrange("p (c t) -> p c t", t=512)
                        r0 = b * 2048 + cb * 512
                        P.dma('sp', gtv, gT_d[r0:r0 + 512, qt * 512:(qt + 1) * 512].rearrange("(c p) t -> p c t", p=128), reads=[bg_d[qt]], writes=gtb)
                        for c in range(4):
                            ps, psb = nxt('mm', MM)
                            mm_acc(ps[:, :], psb, [(sl[:, j, c * 128:(c + 1) * 128], xs[b][0][:, j, :]) for j in range(8)], slb + xs[b][1])
                            if b == 0:
                                P.op('dve', lambda e, c=c, ps=ps, accv=accv, gtv=gtv: e.tensor_tensor(out=accv[:, c, :], in0=ps[:, :], in1=gtv[:, c, :], op=ALU.mult),
                                     reads=[psb] + gtb, writes=accb, join=(c > 0))
                            else:
                                tmp, tmpb = rb(40 + 2 * (c % 2), 2, F32)
                                P.op('dve', lambda e, c=c, ps=ps, tmp=tmp, gtv=gtv: e.tensor_tensor(out=tmp[:, 0:512], in0=ps[:, :], in1=gtv[:, c, :], op=ALU.mult),
                                     reads=[psb] + gtb, writes=tmpb)
                                if b == 1:
                                    P.op('pool', lambda e, c=c, accv=accv, tmp=tmp: e.tensor_tensor(out=accv[:, c, :], in0=accv[:, c, :], in1=tmp[:, 0:512], op=ALU.add),
                                         reads=accb + tmpb, writes=accb, join=True)
                                else:
                                    P.op('pool', lambda e, c=c, cb=cb, qt=qt, accv=accv, tmp=tmp: e.tensor_tensor(out=mrgv[:, cb * 4 + c, qt * 512:(qt + 1) * 512], in0=accv[:, c, :],
                                                                                                                 in1=tmp[:, 0:512], op=ALU.add),
                                         reads=accb + tmpb, writes=[RAq[qt]], join=True)
            if mrg_dbg is not None and l == 0:
                bdb = Buf('mrg_dbg')
                for c in range(16):
                    finals.append(store(mrg_dbg[c * 128:(c + 1) * 128, :], mrgv[:, c, :], RAq[0:4], bdb, join=(c > 0)))

            chk('E')
            wo = TW(T['tw_o'][l], 2048, 256)
            for cb in range(8):
                sl, slb = load_slab(wo, 0, 2048, cb * 256, 256)
                for t in range(NT):
                    ps, psb = nxt('mm', MM)
                    mm_acc(ps[:, 0:256], psb, [(mrgv[:, j, t * 128:(t + 1) * 128], sl[:, j, :]) for j in range(16)], slb + [RAq[t // 4]])
                    yt, ytb = rb(44 + 4 * (rr.get('yt', 0) % 4), 1, F32)
                    rr['yt'] = rr.get('yt', 0) + 1
                    P.dma('sp', yt[:, 0:256], y_src[t * 128:(t + 1) * 128, cb * 256:(cb + 1) * 256], reads=[ybuf[t]] if l > layers[0] else [], writes=ytb[0:1])
                    P.op('dve', lambda e, yt=yt, ps=ps: e.tensor_tensor(out=yt[:, 0:256], in0=ps[:, 0:256], in1=yt[:, 0:256], op=ALU.add), reads=[psb] + ytb[0:1], writes=ytb[0:1])
                    o = store(OUT[t * 128:(t + 1) * 128, cb * 256:(cb + 1) * 256], yt[:, 0:256], ytb[0:1], ybuf[t], join=(l == layers[0] and cb > 0))
                    finals.append(o)

        def q_epilogue(t, qf, qfb, G_Q):
            stt, stb = nxt('stat', STAT)
            qv = qf[:, 0:1536].rearrange("p (h d) -> p h d", d=192)
            sq, sqb = rb(24, 8, F32)
            sqv = sq[:, 0:1536].rearrange("p (h d) -> p h d", d=192)
            P.op('act', lambda e: e.activation(out=sq[:, 0:1536], in_=qf[:, 0:1536], func=AF.Square), reads=qfb, writes=sqb)
            P.op('dve', lambda e: e.tensor_reduce(out=stt[:, 0:8], in_=sqv, axis=AX.X, op=ALU.add), reads=sqb, writes=[stb])
            rstd_from_ss(stt[:, 0:8], stt[:, 8:16], 192, [stb])
            P.op('dve', lambda e: e.tensor_tensor(out=sqv, in0=qv, in1=stt[:, 8:16].unsqueeze(2).broadcast_to([128, 8, 192]), op=ALU.mult),
                 reads=qfb + [stb], writes=sqb)
            P.op('dve', lambda e: e.tensor_tensor(out=sqv, in0=sqv, in1=G_Q.unsqueeze(1).broadcast_to([128, 8, 192]), op=ALU.mult),
                 reads=sqb + [bgain], writes=sqb)
            qb_, qbb = rb(20, 4)
            qbn = qb_[:, 0:1024].rearrange("p (h d) -> p h d", d=128)
            qbr = qb_[:, 1024:1536].rearrange("p (h d) -> p h d", d=64)
            P.op('act', lambda e: e.activation(out=qbn, in_=sqv[:, :, 0:128], func=AF.Copy), reads=sqb, writes=qbb)
            x1, x2 = sqv[:, :, 128:160], sqv[:, :, 160:192]
            cb_ = COS[:, t, :].unsqueeze(1).broadcast_to([128, 8, 32])
            sb_ = SIN[:, t, :].unsqueeze(1).broadcast_to([128, 8, 32])
            tm, tmb = rb(60, 4, F32)
            t1 = tm[:, 0:256].rearrange("p (h d) -> p h d", d=32)
            t2 = tm[:, 256:512].rearrange("p (h d) -> p h d", d=32)
            P.op('pool', lambda e: e.tensor_tensor(out=t1, in0=x1, in1=cb_, op=ALU.mult), reads=sqb + [bcs], writes=tmb)
            P.op('pool', lambda e: e.tensor_tensor(out=t2, in0=x2, in1=sb_, op=ALU.mult), reads=sqb + [bcs], writes=tmb, join=True)
            P.op('pool', lambda e: e.tensor_tensor(out=qbr[:, :, 0:32], in0=t1, in1=t2, op=ALU.subtract), reads=tmb, writes=qbb, join=True)
            P.op('dve', lambda e: e.tensor_tensor(out=t1, in0=x2, in1=cb_, op=ALU.mult), reads=sqb + [bcs] + qbb, writes=tmb)
            P.op('dve', lambda e: e.tensor_tensor(out=t2, in0=x1, in1=sb_, op=ALU.mult), reads=sqb + [bcs], writes=tmb, join=True)
            P.op('dve', lambda e: e.tensor_tensor(out=qbr[:, :, 32:64], in0=t1, in1=t2, op=ALU.add), reads=tmb, writes=qbb, join=True)
            qs, qsb = rb(1, 2)
            qsn = qs[:, 0:1024].rearrange("p (h t) -> p h t", t=128)
            qsr_, qsrb = rb(5, 2)
            qsr = qsr_[:, 0:1024].rearrange("p (h t) -> p h t", t=128)
            transposes([qbn[:, h, :] for h in range(8)], qbb, lambda q, m: qsn[:, q:q + m, :], qsb)
            transposes([qbr[:, h, :] for h in range(8)], qbb, lambda q, m: qsr[0:64, q:q + m, :], qsrb, width=64)
            store(qnT_d[:, t * 128:(t + 1) * 128].rearrange("(h p) t -> p h t", p=128), qsn, qsb, bq_d, join=True)
            store(qrT_d[:, t * 128:(t + 1) * 128].rearrange("(h p) t -> p h t", p=64), qsr[0:64, :, :], qsrb, bq_d, join=True)

        def kv_epilogue(t, ps, psb, G_KVA, G_KPE, wkvAv, wkvAb, wkvBv, wkvBb):
            stt, stb = nxt('stat', STAT)
            jk, jkb = rb(16, 4)
            P.op('act', lambda e: e.activation(out=jk[:, 0:256], in_=ps[:, 0:256], func=AF.Square, accum_out=stt[:, 0:1]), reads=[psb], writes=jkb + [stb])
            P.op('act', lambda e: e.activation(out=jk[:, 256:320], in_=ps[:, 256:320], func=AF.Square, accum_out=stt[:, 2:3]), reads=[psb], writes=jkb + [stb], join=True)
            rstd_from_ss(stt[:, 0:1], stt[:, 1:2], 256, [stb])
            ckn, cknb = rb(20, 4)
            P.op('dve', lambda e: e.scalar_tensor_tensor(out=ckn[:, 0:256], in0=ps[:, 0:256], scalar=stt[:, 1:2], in1=G_KVA, op0=ALU.mult, op1=ALU.mult),
                 reads=[psb, stb, bgain], writes=cknb)
            chk('k1')
            kp, kpb = rb(60, 4, F32)
            P.op('dve', lambda e: e.tensor_tensor(out=kp[:, 0:64], in0=ps[:, 256:320], in1=G_KPE, op=ALU.mult), reads=[psb, bgain], writes=kpb)
            x1, x2 = kp[:, 0:32], kp[:, 32:64]
            c_, s_ = COS[:, t, :], SIN[:, t, :]
            t1, t2, t3, t4 = kp[:, 64:96], kp[:, 96:128], kp[:, 128:160], kp[:, 160:192]
            kr = ckn[:, 256:320]
            P.op('dve', lambda e: e.tensor_tensor(out=t1, in0=x1, in1=c_, op=ALU.mult), reads=kpb + [bcs], writes=kpb, join=True)
            P.op('dve', lambda e: e.tensor_tensor(out=t2, in0=x2, in1=s_, op=ALU.mult), reads=kpb + [bcs], writes=kpb, join=True)
            P.op('dve', lambda e: e.tensor_tensor(out=t3, in0=x2, in1=c_, op=ALU.mult), reads=kpb + [bcs], writes=kpb, join=True)
            P.op('dve', lambda e: e.tensor_tensor(out=t4, in0=x1, in1=s_, op=ALU.mult), reads=kpb + [bcs], writes=kpb, join=True)
            P.op('dve', lambda e: e.tensor_tensor(out=kr[:, 0:32], in0=t1, in1=t2, op=ALU.subtract), reads=kpb, writes=cknb, join=True)
            P.op('dve', lambda e: e.tensor_tensor(out=kr[:, 32:64], in0=t3, in1=t4, op=ALU.add), reads=kpb, writes=cknb, join=True)
            chk('k2')
            ckT, ckTb = rb(0, 1)
            ckTv = ckT[:, 0:256].rearrange("p (c t) -> p c t", t=128)
            transposes([ckn[:, c * 128:(c + 1) * 128] for c in range(2)], cknb, lambda q, m: ckTv[:, q:q + m, :], ckTb[0:1])
            krs, krsb = rb(1 + (t % 2), 1)
            krv = krs[:, 0:128].rearrange("p (c t) -> p c t", t=128)
            transposes([kr], cknb, lambda q, m: krv[0:64, q:q + m, :], krsb[0:1], width=64)
            store(krT_d[:, t * 128:(t + 1) * 128], krs[0:64, 0:128], krsb[0:1], bkr_d, join=True)
            chk('k3')
            vst, vstb = rb(3 + (t % 2) * 2, 2)
            kns, knsb = rb(8 + (t % 2) * 2, 2)
            sq, sqb = rb(24, 8, F32)
            for nb in range(4):
                wv, wb = (wkvAv, wkvAb) if nb < 2 else (wkvBv, wkvBb)
                ps2, ps2b = nxt('mm', MM)
                mm_acc(ps2[:, :], ps2b, [(ckTv[:, j, :], wv[:, j, (nb % 2) * 512:(nb % 2 + 1) * 512]) for j in range(2)], ckTb[0:1] + wb)
                for hh in range(2):
                    hd = 2 * nb + hh
                    kcol = ps2[:, hh * 256:hh * 256 + 128]
                    vcol = ps2[:, hh * 256 + 128:hh * 256 + 256]
                    fj = not (nb == 0 and hh == 0)
                    P.op('act', lambda e, kcol=kcol, hd=hd: e.activation(out=kns[:, hd * 128:(hd + 1) * 128], in_=kcol, func=AF.Copy),
                         reads=[ps2b], writes=knsb, join=fj)
                    P.op('dve', lambda e, vcol=vcol, hd=hd: e.tensor_copy(out=vst[:, hd * 128:(hd + 1) * 128], in_=vcol),
                         reads=[ps2b], writes=vstb, join=fj)
                    P.op('act', lambda e, kcol=kcol, hd=hd: e.activation(out=sq[:, hd * 128:(hd + 1) * 128], in_=kcol, func=AF.Square),
                         reads=[ps2b], writes=sqb, join=fj)
            chk('k4')
            store(V_d[t * 128:(t + 1) * 128, :], vst[:, 0:1024], vstb, bv_d, join=True)
            P.op('dve', lambda e: e.tensor_reduce(out=stt[:, 8:16], in_=sq[:, 0:1024].rearrange("p (h d) -> p h d", d=128), axis=AX.X, op=ALU.add), reads=sqb, writes=[stb], join=True)
            P.op('dve', lambda e: e.tensor_scalar(out=stt[:, 8:16], in0=stt[:, 8:16], scalar1=stt[:, 2:3], scalar2=None, op0=ALU.add), reads=[stb], writes=[stb], join=True)
            rstd_from_ss(stt[:, 8:16], stt[:, 8:16], 192, [stb], scale_extra=192 ** -0.5)
            P.op('dve', lambda e: e.tensor_copy(out=SCK[:, t, :], in_=stt[:, 8:16]), reads=[stb], writes=[bsck], join=True)
            chk('k5')
            knT, knTb = rb(12 + (t % 2) * 2, 2)
            knTv = knT[:, 0:1024].rearrange("p (h t) -> p h t", t=128)
            transposes([kns[:, h * 128:(h + 1) * 128] for h in range(8)], knsb, lambda q, m: knTv[:, q:q + m, :], knTb)
            store(knT_d[:, t * 128:(t + 1) * 128].rearrange("(h p) t -> p h t", p=128), knTv, knTb, bkn_d, join=True)

        def phase_attention():
            krT, krTb = rb(0, 4)
            P.dma('sp', krT[0:64, :], krT_d[:, :], reads=[bkr_d], writes=krTb)
            for h in range(8):
                o = 4 + (h % 2) * 16
                qn, qnb = rb(o, 4)
                qr, qrb = rb(o + 4, 4)
                kn, knb = rb(o + 8, 4)
                vh, vhb = rb(o + 12, 4)
                vhv = vh.rearrange("p (t d) -> p t d", d=128)
                P.dma('sp', qn, qnT_d[h * 128:(h + 1) * 128, :], reads=[bq_d], writes=qnb)
                P.dma('sp', qr[0:64, :], qrT_d[h * 64:(h + 1) * 64, :], reads=[bq_d], writes=qrb)
                P.dma('sp', kn, knT_d[h * 128:(h + 1) * 128, :], reads=[bkn_d], writes=knb)
                for hf_ in range(2):
                    P.dma('sp', vhv[:, hf_ * 8:(hf_ + 1) * 8, :], V_d[hf_ * 1024:(hf_ + 1) * 1024, h * 128:(h + 1) * 128].rearrange("(t p) d -> p t d", p=128),
                          reads=[bv_d], writes=vhb, join=(hf_ > 0))
                for qt in range(4):
                    nk = 4 * (qt + 1)
                    num, numb = MM[4]
                    den, denb = AUX
                    qs = slice(qt * 512, (qt + 1) * 512)
                    for kt in range(nk):
                        ps, psb = nxt('mmA', MM, 4)
                        ks = slice(kt * 128, (kt + 1) * 128)
                        P.op('pe', lambda e, ps=ps, ks=ks, qs=qs, kn=kn, qn=qn: e.matmul(ps[:, :], lhsT=kn[:, ks], rhs=qn[:, qs], start=True, stop=False),
                             reads=knb + qnb, writes=[psb])
                        P.op('pe', lambda e, ps=ps, ks=ks, qs=qs, qr=qr: e.matmul(ps[:, :], lhsT=krT[0:64, ks], rhs=qr[0:64, qs], start=False, stop=True),
                             reads=krTb + qrb, writes=[psb], join=True)
                        pT, pTb = rb(44 + 4 * (rr.get('pT', 0) % 4), 1)
                        rr['pT'] = rr.get('pT', 0) + 1
                        P.op('act', lambda e, ps=ps, pT=pT, kt=kt, h=h: e.activation(out=pT[:, 0:512], in_=ps[:, :], func=AF.Exp, scale=SCK[:, kt, h:h + 1]),
                             reads=[psb, bsck], writes=pTb[0:1])
                        if kt >= 4 * qt:
                            P.op('dve', lambda e, pT=pT, kt=kt, qt=qt: e.tensor_tensor(out=pT[:, 0:512], in0=pT[:, 0:512], in1=maskb[:, kt - 4 * qt, :], op=ALU.mult),
                                 reads=pTb[0:1] + [bconst], writes=pTb[0:1])
                        P.op('pe', lambda e, pT=pT, kt=kt, nk=nk, vhv=vhv, num=num: e.matmul(num[:, :], lhsT=vhv[:, kt, :], rhs=pT[:, 0:512], start=(kt == 0), stop=(kt == nk - 1)),
                             reads=vhb + pTb[0:1], writes=[numb], join=(kt > 0))
                        P.op('pe', lambda e, pT=pT, kt=kt, nk=nk, den=den: e.matmul(den[:, :], lhsT=onesb[:], rhs=pT[:, 0:512], start=(kt == 0), stop=(kt == nk - 1)),
                             reads=pTb[0:1] + [bconst], writes=[denb], join=(kt > 0))
                    rc, rcb = rb(40, 2, F32)
                    P.op('dve', lambda e, rc=rc, den=den: e.reciprocal(out=rc[:, 0:512], in_=den[:, :]), reads=[denb], writes=rcb)
                    ao, aob = rb(42 + (qt % 2), 1)
                    P.op('dve', lambda e, ao=ao, num=num, rc=rc: e.tensor_tensor(out=ao[:, 0:512], in0=num[:, :], in1=rc[:, 0:512], op=ALU.mult), reads=[numb] + rcb, writes=aob[0:1])
                    store(attT_d[h * 128:(h + 1) * 128, qs], ao[:, 0:512], aob[0:1], batt_d[qt])

        def phase_dense_ffn(l):
            h2v = RA[:, 0:16 * S].rearrange("p (c t) -> p c t", t=S)
            phase_norm(OUT, S, T['ffn_norm_g'][l], 'fm', h2v, lambda t: [RAq[t // 4]], src_is_out=True)
            wg, wu, wd = TW(T['tw_dg'][0], 2048, 256), TW(T['tw_du'][0], 2048, 256), TW(T['tw_dd'][0], 512, 512)
            aT, aTb = rb(0, 44)
            aTv = aT.rearrange("p (f t) -> p f t", t=512)
            for qt in range(4):
                for fs in range(22):
                    sg_, sgb = load_slab(wg, 0, 2048, fs * 256, 256)
                    su_, sub = load_slab(wu, 0, 2048, fs * 256, 256)
                    for c in range(2):
                        pg, pgb = nxt('mm6', MM6)
                        pu, pub = nxt('mm6', MM6)
                        mm_acc(pg[:, :], pgb, [(sg_[:, j, c * 128:(c + 1) * 128], h2v[:, j, qt * 512:(qt + 1) * 512]) for j in range(16)], sgb + [RAq[qt]])
                        mm_acc(pu[:, :], pub, [(su_[:, j, c * 128:(c + 1) * 128], h2v[:, j, qt * 512:(qt + 1) * 512]) for j in range(16)], sub + [RAq[qt]])
                        sl_, slb = rb(44 + 2 * (rr.get('silu', 0) % 2), 2, F32)
                        rr['silu'] = rr.get('silu', 0) + 1
                        P.op('act', lambda e, sl_=sl_, pg=pg: e.activation(out=sl_[:, 0:512], in_=pg[:, :], func=AF.Silu), reads=[pgb], writes=slb)
                        P.op('dve', lambda e, sl_=sl_, pu=pu, fs=fs, c=c: e.tensor_tensor(out=aTv[:, fs * 2 + c, :], in0=sl_[:, 0:512], in1=pu[:, :], op=ALU.mult),
                             reads=slb + [pub], writes=aTb, join=True)
                for cb in range(4):
                    accs = [MM[i] for i in range(4)]
                    for fr in range(0, 44, 4):
                        nf = 4
                        sd, sdb = load_slab(wd, fr * 128, nf * 128, cb * 512, 512)
                        for fc in range(nf):
                            f = fr + fc
                            for tt in range(4):
                                ps, psb = accs[tt]
                                P.op('pe', lambda e, ps=ps, f=f, tt=tt, sd=sd, fc=fc: e.matmul(ps[:, :], lhsT=aTv[:, f, tt * 128:(tt + 1) * 128], rhs=sd[:, fc, :], start=(f == 0), stop=(f == 43)),
                                     reads=aTb + sdb, writes=[psb], join=(f > 0))
                    for tt in range(4):
                        t = qt * 4 + tt
                        ps, psb = accs[tt]
                        yt, ytb = rb(48 + 4 * (rr.get('yt2', 0) % 4), 2, F32)
                        rr['yt2'] = rr.get('yt2', 0) + 1
                        P.dma('sp', yt[:, 0:512], OUT[t * 128:(t + 1) * 128, cb * 512:(cb + 1) * 512], reads=[ybuf[t]], writes=ytb)
                        P.op('dve', lambda e, yt=yt, ps=ps: e.tensor_tensor(out=yt[:, 0:512], in0=ps[:, :], in1=yt[:, 0:512], op=ALU.add), reads=[psb] + ytb, writes=ytb)
                        finals.append(store(OUT[t * 128:(t + 1) * 128, cb * 512:(cb + 1) * 512], yt[:, 0:512], ytb, ybuf[t], join=False))

        def phase_moe(l):
            P.dma('sp', RW[:], T['router_w'][0].rearrange("(j p) e -> p j e", p=128), writes=[brw])
            bcast_row(RBI[:], T['router_b'][0], [brw], first=False)
            phase_norm(OUT, S, T['ffn_norm_g'][l], 'tm', src_is_out=True, router=True)
            for m in range(NT):
                ps, psb = AUX
                pairs = [(onesb[:], RSELB[:, k, :]) for k in range(m)] + [(trib[:], RSELB[:, m, :])]
                mm_acc(ps[:, 0:8], psb, pairs, [brt, bconst])
                P.op('dve', lambda e, m=m, ps=ps: e.tensor_copy(out=RPOS[:, m, :], in_=ps[:, 0:8]), reads=[psb], writes=[brt], join=True)
            if rt_dbg is not None:
                for i, src in enumerate((RSEL, RWT, RPOS)):
                    finals.append(store(rt_dbg[:, i * 128:(i + 1) * 128], src[:, :, :].rearrange("p a b -> p (a b)"), [brt], Buf(f'rt_dbg{i}'), join=False))
            aT = RA[:, 0:56 * CAP]
            aTv = aT.rearrange("p (f s) -> p f s", s=CAP)
            aTb = RAq
            GT, GTb = rb(0, 24)
            GTv = GT.rearrange("p (j s) -> p j s", s=CAP)
            OEv = GT.rearrange("p (s f) -> p s f", f=2048)
            PE_, PEb = rb(24, 24)
            PEv = PE_.rearrange("p (k s) -> p k s", s=CAP)
            HALF = CAP // 2
            for e_ in range(8):
                wg, wu, wd = TW(T['tw_mg'][0][e_], 2048, 256), TW(T['tw_mu'][0][e_], 2048, 256), TW(T['tw_md'][0][e_], 1024, 512)
                for k in range(NT):
                    eng = 'dve' if k % 2 == 0 else 'pool'
                    P.op(eng, lambda e, k=k, e_=e_: e.tensor_scalar(out=PEv[:, k, :], in0=iota[:], scalar1=RPOS[:, k, e_:e_ + 1], scalar2=RSEL[:, k, e_:e_ + 1],
                                                                    op0=ALU.is_equal, op1=ALU.mult), reads=[brt, bconst], writes=PEb, join=(k > 0))
                for j in range(16):
                    hs, hsb = rb(48 + 4 * (j % 2), 4)
                    hsv = hs.rearrange("p (k f) -> p k f", f=128)
                    P.dma('sp', hsv, htm_d[:, j * 128:(j + 1) * 128].rearrange("(k p) f -> p k f", p=128), reads=[bhtm], writes=hsb)
                    for half in range(2):
                        ps, psb = nxt('mm', MM)
                        mm_acc(ps[:, 0:HALF], psb, [(hsv[:, k, :], PEv[:, k, half * HALF:(half + 1) * HALF]) for k in range(NT)], hsb + PEb)
                        copy_op(ev_eng(), GTv[:, j, half * HALF:(half + 1) * HALF], ps[:, 0:HALF], [psb], GTb, join=True)
                for fs in range(28):
                    sg_, sgb = load_slab(wg, 0, 2048, fs * 256, 256)
                    su_, sub = load_slab(wu, 0, 2048, fs * 256, 256)
                    for c in range(2):
                        for half in range(2):
                            pg, pgb = nxt('mm6', MM6)
                            pu, pub = nxt('mm6', MM6)
                            hs_ = slice(half * HALF, (half + 1) * HALF)
                            mm_acc(pg[:, 0:HALF], pgb, [(sg_[:, j, c * 128:(c + 1) * 128], GTv[:, j, hs_]) for j in range(16)], sgb + GTb)
                            mm_acc(pu[:, 0:HALF], pub, [(su_[:, j, c * 128:(c + 1) * 128], GTv[:, j, hs_]) for j in range(16)], sub + GTb)
                            sl_, slb = rb(56 + 4 * (rr.get('silu', 0) % 2), 2, F32)
                            rr['silu'] = rr.get('silu', 0) + 1
                            P.op('act', lambda e, sl_=sl_, pg=pg: e.activation(out=sl_[:, 0:HALF], in_=pg[:, 0:HALF], func=AF.Silu), reads=[pgb], writes=slb)
                            P.op('dve', lambda e, sl_=sl_, pu=pu, fs=fs, c=c, hs_=hs_: e.tensor_tensor(out=aTv[:, fs * 2 + c, hs_], in0=sl_[:, 0:HALF], in1=pu[:, 0:HALF], op=ALU.mult),
                                 reads=slb + [pub], writes=aTb, join=True)
                for cb in range(4):
                    accs = [MM[i] for i in range(5)] + [AUX]
                    for fr in range(0, 56, 8):
                        sd, sdb = load_slab(wd, fr * 128, 8 * 128, cb * 512, 512)
                        for fc in range(8):
                            f = fr + fc
                            for s_ in range(NST):
                                ps, psb = accs[s_]
                                P.op('pe', lambda e, ps=ps, f=f, s_=s_, sd=sd, fc=fc: e.matmul(ps[:, :], lhsT=aTv[:, f, s_ * 128:(s_ + 1) * 128], rhs=sd[:, fc, :], start=(f == 0), stop=(f == 55)),
                                     reads=aTb + sdb, writes=[psb], join=(f > 0))
                    for s_ in range(NST):
                        ps, psb = accs[s_]
                        copy_op(ev_eng(), OEv[:, s_, cb * 512:(cb + 1) * 512], ps[:, :], [psb], GTb, join=True)
                for m in range(NT):
                    pwT, pwTb = rb(48 + 4 * (m % 2), 2)
                    pwTv = pwT[:, 0:NST * 128].rearrange("p (s t) -> p s t", t=128)
                    transposes([PEv[:, m, s_ * 128:(s_ + 1) * 128] for s_ in range(NST)], PEb, lambda q, mm_: pwTv[:, q:q + mm_, :], pwTb)
                    for cb in range(4):
                        ps, psb = nxt('mm', MM)
                        mm_acc(ps[:, :], psb, [(pwTv[:, s_, :], OEv[:, s_, cb * 512:(cb + 1) * 512]) for s_ in range(NST)], pwTb + GTb)
                        sc, scb = rb(56 + (rr.get('sc', 0) % 2) * 4, 2, F32)
                        rr['sc'] = rr.get('sc', 0) + 1
                        P.op('dve', lambda e, sc=sc, ps=ps, m=m, e_=e_: e.tensor_scalar(out=sc[:, 0:512], in0=ps[:, :], scalar1=RWT[:, m, e_:e_ + 1], scalar2=None, op0=ALU.mult),
                             reads=[psb, brt], writes=scb)
                        finals.append(P.dma('pool', OUT[m * 128:(m + 1) * 128, cb * 512:(cb + 1) * 512], sc[:, 0:512], reads=scb + [ybuf[m]], writes=[ybuf[m]],
                                            accum_op=ALU.add))

        try:
            if stop_at != 'n4x':
                phase_rope_tables()
            chk('rope')
            for l in layers:
                if 'mixer' in parts:
                    phase_mixer(l)
                if 'ffn' in parts:
                    if l % 2 == 0:
                        phase_dense_ffn(l)
                    else:
                        phase_moe(l)
        except _Stop:
            pass
        if not finals:
            finals.append(store(OUT[0:128, 0:32], RA[:, 0:64].bitcast(F32), RAq[0:1], Buf('dummy_out'), join=False))
        P.emit(final_waits=finals)
    return nc


from concourse.bass_utils import run_bass_kernel_spmd


def kernel(**inputs):
    nc = build_program()
    consts = host_constants()
    B = inputs['x'].shape[0]
    in_maps = []
    tiled_cache = {}
    used = set(nc.used_inputs.keys())
    for b in range(B):
        m = {}
        for k in used:
            if k == 'x':
                m[k] = np.ascontiguousarray(inputs['x'][b])
            elif k == 'mem':
                m[k] = np.ascontiguousarray(inputs['mem'][b])
            elif k == 'pos':
                m[k] = np.ascontiguousarray(inputs['positions'][b].reshape(16, 128).T).astype(np.int32)
            elif k in consts:
                m[k] = consts[k]
            elif k in TILED:
                if k not in tiled_cache:
                    tiled_cache[k] = host_tile(k, np.asarray(inputs[TILED[k][0]]))
                m[k] = tiled_cache[k]
            else:
                m[k] = np.ascontiguousarray(inputs[k])
        in_maps.append(m)
    res = run_bass_kernel_spmd(nc, in_maps, core_ids=list(range(B)))
    return np.stack([np.asarray(r['out']) for r in res.results], axis=0).astype(np.float32)
```

```python
import numpy as np
import concourse.bass as bass
import concourse.mybir as mybir

F32 = mybir.dt.float32
BF16 = mybir.dt.bfloat16
I32 = mybir.dt.int32
ALU = mybir.AluOpType
AF = mybir.ActivationFunctionType
AX = mybir.AxisListType

ENGS = ("pe", "act", "dve", "pool", "sp")
EPOCH = 500
DMA_POOLN = {'sp': 40, 'pool': 24, 'act': 8, 'pe': 1, 'dve': 1}


class Buf:
    __slots__ = ("name", "writers", "readers", "const", "sems", "cnt", "excl")

    def __init__(self, name, const=False, excl=False):
        self.name = name
        self.excl = excl
        self.writers = []
        self.readers = {}
        self.const = const
        self.sems = []
        self.cnt = 0


class Op:
    __slots__ = ("eng", "fn", "deps", "is_dma", "sem", "semval", "signals", "seq", "sigpos", "clock", "dclock")

    def __init__(self, eng, fn, is_dma):
        self.eng = eng
        self.fn = fn
        self.is_dma = is_dma
        self.deps = []
        self.sem = None
        self.semval = 0
        self.signals = False
        self.seq = -1
        self.sigpos = -1
        self.clock = None
        self.dclock = None


class Prog:
    def __init__(self, nc, stack):
        self.nc = nc
        self.stack = stack
        self.ops = {e: [] for e in ENGS}
        self.seen = {e: {f: -1 for f in ENGS} for e in ENGS}
        self.dseen = {e: {} for e in ENGS}
        self.nsem = 0
        self.n_ops = 0
        self.dma_pool = {}
        self.dma_rr = {}

    def sem(self, name):
        self.nsem += 1
        return self.stack.enter_context(self.nc.semaphore(name))

    def sbuf(self, name, shape, dt):
        return self.stack.enter_context(self.nc.sbuf_tensor(name, list(shape), dt))

    def psum(self, name, shape, dt=F32):
        return self.stack.enter_context(self.nc.psum_tensor(name, list(shape), dt))

    def _record(self, eng, fn, reads, writes, is_dma, join):
        op = Op(eng, fn, is_dma)
        deps = {}
        if is_dma:
            pool = self.dma_pool.setdefault(eng, [])
            if len(pool) < DMA_POOLN[eng]:
                pool.append([self.sem(f"d_{eng}{len(pool)}"), 0, None])
                slot = pool[-1]
            else:
                i = self.dma_rr.get(eng, 0)
                self.dma_rr[eng] = (i + 1) % len(pool)
                slot = pool[i]
            if slot[2] is not None:
                deps[id(slot[2])] = slot[2]
            slot[1] += 16
            slot[2] = op
            op.sem = slot[0]
            op.semval = slot[1]
        for b in reads:
            for w in b.writers:
                deps[id(w)] = w
            if b.excl:
                for k, r in b.readers.items():
                    if k != eng:
                        deps[id(r)] = r
        for b in writes:
            for r in b.readers.values():
                deps[id(r)] = r
            if not join:
                for w in b.writers:
                    deps[id(w)] = w
        my_seen = self.seen[eng]
        my_dseen = self.dseen[eng]
        best_c = {}
        best_d = {}
        for d in deps.values():
            if d.is_dma:
                if my_dseen.get(id(d.sem), 0) >= d.semval:
                    continue
                k = id(d.sem)
                if k not in best_d or best_d[k].semval < d.semval:
                    best_d[k] = d
            else:
                if eng == "pe" and d.eng == "pe":
                    continue
                if my_seen[d.eng] >= d.seq:
                    continue
                if d.eng not in best_c or best_c[d.eng].seq < d.seq:
                    best_c[d.eng] = d
        need = list(best_c.values()) + list(best_d.values())
        dropped = [d for d in deps.values() if d not in need]
        for d in need:
            if d.is_dma:
                my_dseen[id(d.sem)] = max(my_dseen.get(id(d.sem), 0), d.semval)
            else:
                d.signals = True
                if my_seen[d.eng] < d.seq:
                    my_seen[d.eng] = d.seq
            for f, s in d.clock.items():
                if my_seen[f] < s:
                    my_seen[f] = s
            for k, v in d.dclock.items():
                if my_dseen.get(k, 0) < v:
                    my_dseen[k] = v
        op.deps = need
        op.seq = len(self.ops[eng])
        op.clock = dict(my_seen)
        op.dclock = dict(my_dseen)
        self.ops[eng].append(op)
        self.n_ops += 1
        for b in reads:
            if not b.const:
                key = (eng, id(op)) if is_dma else eng
                b.readers[key] = op
        for b in writes:
            if join:
                b.writers.append(op)
            else:
                b.writers = [op]
                b.readers = {}
        return op

    def op(self, eng, fn, reads=(), writes=(), join=False):
        return self._record(eng, fn, list(reads), list(writes), False, join)

    def dma(self, queue, out, in_, reads=(), writes=(), join=False, **kw):
        assert len(writes) >= 1
        return self._record(queue, lambda e: e.dma_start(out=out, in_=in_, **kw), list(reads), list(writes), True, join)

    def emit(self, final_waits=()):
        nc = self.nc
        esems = {}
        for e in ENGS:
            pos = 0
            for o in self.ops[e]:
                if (not o.is_dma) and o.signals:
                    o.sigpos = pos
                    pos += 1
            n_ep = (pos + EPOCH - 1) // EPOCH
            esems[e] = [self.sem(f"s_{e}{i}") for i in range(max(n_ep, 1))]
        engobj = {"pe": "tensor", "act": "scalar", "dve": "vector", "pool": "gpsimd", "sp": "sync"}
        with nc.Block() as block:
            def body_for(e):
                def body(eng):
                    for o in self.ops[e]:
                        for d in o.deps:
                            if d.is_dma:
                                eng.wait_ge(d.sem, d.semval)
                            else:
                                ep, idx = divmod(d.sigpos, EPOCH)
                                eng.wait_ge(esems[d.eng][ep], idx + 1)
                        ins = o.fn(eng)
                        if o.is_dma:
                            ins.then_inc(o.sem, 16)
                        elif o.signals:
                            ep, idx = divmod(o.sigpos, EPOCH)
                            ins.then_inc(esems[e][ep], 1)
                    if e == "sp":
                        for d in final_waits:
                            eng.wait_ge(d.sem, d.semval)
                return body
            for e in ENGS:
                if self.ops[e] or e == "sp":
                    getattr(block, engobj[e])(body_for(e))


from contextlib import ExitStack
import math

S = 2048
D = 2048
NT = 16
CAP = 768
NST = CAP // 128
EPS = 1e-6

WEIGHT_SHAPES = {
    'attn_norm_g': (2, 2048), 'w_in': (2, 2048, 9024), 'mla_q_a_norm_g': (2, 512), 'mla_w_uq': (2, 512, 1536),
    'mla_kv_a_norm_g': (2, 256), 'mla_w_ukv': (2, 256, 2048), 'mla_q_norm_g': (2, 192), 'mla_k_norm_g': (2, 192),
    'mla_w_out': (2, 1024, 2048), 'pool_w': (2, 4, 256, 256), 'pool_scale': (2, 1024), 'pool_w_out': (2, 1024, 2048),
    'mem_norm_g': (2, 2048), 'mem_w_kv': (2, 2048, 2048), 'mem_q_norm_g': (2, 256), 'mem_k_norm_g': (2, 256),
    'mem_w_out': (2, 1024, 2048), 'w_o': (2, 2048, 2048), 'ffn_norm_g': (2, 2048),
    'dense_w_gate': (1, 2048, 5632), 'dense_w_up': (1, 2048, 5632), 'dense_w_down': (1, 5632, 2048),
    'router_w': (1, 2048, 8), 'router_b': (1, 8), 'moe_w_gate': (1, 8, 2048, 7168), 'moe_w_up': (1, 8, 2048, 7168),
    'moe_w_down': (1, 8, 7168, 2048),
}
CONST_SHAPES = {'c_ident': (128, 128), 'c_tri': (128, 128), 'c_ones': (128, 128), 'c_pool': (12, 128, 128),
                'c_mask': (4, 128, 512), 'c_iota': (128, CAP), 'c_invf': (128, 32)}


TILED = {
    'tw_in_a': ('w_in', 1, (0, 512), 2048, 256),
    'tw_in_b': ('w_in', 1, (512, 832), 2048, 160),
    'tw_in_c': ('w_in', 1, (832, 9024), 2048, 256),
    'tw_memkv': ('mem_w_kv', 1, None, 2048, 256),
    'tw_o': ('w_o', 1, None, 2048, 256),
    'tw_mlaout': ('mla_w_out', 1, None, 1024, 512),
    'tw_poolout': ('pool_w_out', 1, None, 1024, 512),
    'tw_memout': ('mem_w_out', 1, None, 1024, 512),
    'tw_dg': ('dense_w_gate', 1, None, 2048, 256),
    'tw_du': ('dense_w_up', 1, None, 2048, 256),
    'tw_dd': ('dense_w_down', 1, None, 512, 512),
    'tw_mg': ('moe_w_gate', 2, None, 2048, 256),
    'tw_mu': ('moe_w_up', 2, None, 2048, 256),
    'tw_md': ('moe_w_down', 2, None, 1024, 512),
}


def tiled_shape(name):
    key, nlead, cr, R, C = TILED[name]
    shp = WEIGHT_SHAPES[key]
    lead, (K, F) = shp[:nlead], shp[nlead:]
    if cr is not None:
        F = cr[1] - cr[0]
    return tuple(lead) + (K // R, F // C, 128, R // 128, C)


def host_tile(name, w):
    key, nlead, cr, R, C = TILED[name]
    if cr is not None:
        w = w[..., cr[0]:cr[1]]
    lead, (K, F) = w.shape[:nlead], w.shape[nlead:]
    w = w.reshape(lead + (K // R, R // 128, 128, F // C, C))
    n = len(lead)
    w = w.transpose(tuple(range(n)) + (n, n + 3, n + 2, n + 1, n + 4))
    return np.ascontiguousarray(w)


class TW:
    def __init__(self, ap, R, C, c_base=0):
        self.ap, self.R, self.C, self.c_base = ap, R, C, c_base

    def slab(self, r0, nrows, c0, ncols):
        assert nrows == self.R and ncols == self.C and r0 % self.R == 0 and (c0 - self.c_base) % self.C == 0, (r0, nrows, c0, ncols, self.R, self.C)
        return self.ap[r0 // self.R, (c0 - self.c_base) // self.C]


def host_constants():
    c = {}
    c['c_ident'] = np.eye(128, dtype=np.float32)
    tri = np.zeros((128, 128), np.float32)
    for a in range(128):
        tri[a, a + 1:] = 1.0
    c['c_tri'] = tri
    c['c_ones'] = np.ones((128, 128), np.float32)
    pm = np.zeros((12, 128, 128), np.float32)
    for g, w in enumerate((2, 4, 8, 16)):
        for t in range(128):
            for d in range(w):
                tp = t - d
                if tp >= 0:
                    pm[3 * g + 1, tp, t] += 1.0 / w
                else:
                    pm[3 * g + 2, 128 + tp, t] += 1.0 / w
            pm[3 * g + 1, t, t] -= 1.0
            cnt = min(t + 1, w)
            for d in range(cnt):
                pm[3 * g + 0, t - d, t] += 1.0 / cnt
            pm[3 * g + 0, t, t] -= 1.0
    c['c_pool'] = pm
    mk = np.zeros((4, 128, 512), np.float32)
    for m in range(4):
        for kp in range(128):
            mk[m, kp, :] = (np.arange(512) >= 128 * m + kp)
    c['c_mask'] = mk
    c['c_iota'] = np.broadcast_to(np.arange(CAP, dtype=np.float32)[None, :], (128, CAP)).copy()
    invf = (np.float32(10000.0) ** (-np.arange(0, 64, 2, dtype=np.float32) / np.float32(64))).astype(np.float32)
    c['c_invf'] = np.broadcast_to(invf[None, :], (128, 32)).copy()
    return c


class _Stop(Exception):
    pass


def build_program(layers=(0, 1), parts=('mixer', 'ffn'), dbg_names=(), stop_at=None):
    nc = bass.Bass("TRN2", target_bir_lowering=False)
    class LazyInputs(dict):
        def __missing__(self, k):
            shp = {'x': (S, D), 'mem': (256, D), 'pos': (128, 16)}.get(k) or (tiled_shape(k) if k in TILED else None) or WEIGHT_SHAPES.get(k) or CONST_SHAPES[k]
            v = nc.dram_tensor(k, list(shp), I32 if k == 'pos' else F32, kind="ExternalInput").ap()
            self[k] = v
            return v
    T = LazyInputs()
    nc.used_inputs = T
    OUT = nc.dram_tensor('out', [S, D], F32, kind="ExternalOutput").ap()

    def scratch(name, shape, dt):
        kind = "ExternalOutput" if name in dbg_names else "Internal"
        return nc.dram_tensor(name, list(shape), dt, kind=kind).ap()

    mixT_d = scratch('mixT_d', [1024, S], BF16)
    xattT_d = scratch('xattT_d', [1024, S], BF16)
    attT_d = scratch('attT_d', [1024, S], BF16)
    gT_d = scratch('gT_d', [6144, S], BF16)
    htm_d = scratch('htm_d', [S, D], BF16)
    qnT_d = scratch('qnT_d', [1024, S], BF16)
    qrT_d = scratch('qrT_d', [512, S], BF16)
    knT_d = scratch('knT_d', [1024, S], BF16)
    krT_d = scratch('krT_d', [64, S], BF16)
    V_d = scratch('V_d', [S, 1024], BF16)
    hT_dbg = scratch('hT_dbg', [D, S], BF16) if 'hT_dbg' in dbg_names else None
    mrg_dbg = scratch('mrg_dbg', [D, S], BF16) if 'mrg_dbg' in dbg_names else None
    rt_dbg = scratch('rt_dbg', [128, 3 * 128], F32) if 'rt_dbg' in dbg_names else None

    with ExitStack() as st:
        P = Prog(nc, st)
        finals = []

        def chk(name):
            if stop_at == name:
                raise _Stop()

        RA_E = 56 * CAP
        RA = P.sbuf('RA', [128, RA_E], BF16)
        RAq = [Buf(f'RA_q{i}') for i in range(4)] + [Buf('RA_tail')]
        RB_E = 32768
        RB = P.sbuf('RB', [128, RB_E], BF16)
        RBB = [Buf(f'RB_{i}') for i in range(RB_E // 2048)]

        def rb(off_kb, size_kb, dt=BF16):
            a, b = off_kb * 512, (off_kb + size_kb) * 512
            a, b = int(a), int(b)
            ap = RB[:, a:b]
            if dt is F32:
                ap = ap.bitcast(F32)
            return ap, RBB[a // 2048:(b + 2047) // 2048]

        SLAB_E = 4096
        slabs = [(P.sbuf(f'slab{i}', [128, SLAB_E], BF16), Buf(f'slab{i}')) for i in range(4)]
        GAIN = P.sbuf('GAIN', [128, 1664], F32)
        bgain = Buf('gain')
        PSC = P.sbuf('PSC', [128, 8], F32)
        STAT = [(P.sbuf(f'stat{i}', [128, 64], F32), Buf(f'stat{i}')) for i in range(4)]
        SC = P.sbuf('SC', [128, 16, 64], F32)
        SIN = SC[:, :, 0:32]
        COS = SC[:, :, 32:64]
        bcs = Buf('cossin')
        SCK = P.sbuf('SCK', [128, 16, 8], F32)
        bsck = Buf('scaleK')
        SCM = P.sbuf('SCM', [128, 2, 4], F32)
        bscm = Buf('scaleM')
        ident = P.sbuf('ident', [128, 128], BF16)
        identf = P.sbuf('identf', [128, 128], F32)
        onesb = P.sbuf('onesb', [128, 128], BF16)
        trib = P.sbuf('trib', [128, 128], BF16)
        poolm = P.sbuf('poolm', [128, 12, 128], BF16)
        maskb = P.sbuf('maskb', [128, 4, 512], BF16)
        iota = P.sbuf('iota', [128, CAP], F32)
        invf = P.sbuf('invf', [128, 32], F32)
        pib = P.sbuf('pib', [128, 1], F32)
        bconst = Buf('const', const=True)
        RSEL = P.sbuf('RSEL', [128, 16, 8], F32)
        RWT = P.sbuf('RWT', [128, 16, 8], F32)
        RPOS = P.sbuf('RPOS', [128, 16, 8], F32)
        RSELB = P.sbuf('RSELB', [128, 16, 8], BF16)
        brt = Buf('router')
        RW = P.sbuf('RW', [128, 16, 8], F32)
        RBI = P.sbuf('RBI', [128, 8], F32)
        brw = Buf('rw')

        MM = [(P.psum(f'mm{i}', [128, 512], F32), Buf(f'mm{i}', excl=True)) for i in range(5)]
        TP = [(P.psum(f'tp{i}', [128, 1024], BF16)[:, 0:512], Buf(f'tp{i}', excl=True)) for i in range(2)]
        AUXt = P.psum('aux', [128, 512], F32)
        AUX = (AUXt, Buf('aux', excl=True))
        MM6 = MM + [AUX]
        rr = {}

        def nxt(kind, lst, n=None):
            n = len(lst) if n is None else n
            i = rr.get(kind, 0) % n
            rr[kind] = i + 1
            return lst[i]

        def ev_eng():
            rr['ev'] = rr.get('ev', 0) ^ 1
            return 'act' if rr['ev'] else 'dve'

        def copy_op(eng, out, in_, reads, writes, join=False):
            if eng == 'act':
                return P.op('act', lambda e: e.activation(out=out, in_=in_, func=AF.Copy), reads=reads, writes=writes, join=join)
            return P.op(eng, lambda e: e.tensor_copy(out=out, in_=in_), reads=reads, writes=writes, join=join)

        ybuf = [Buf(f'y{t}') for t in range(NT)]

        def cload(dst, src):
            P.dma('pool', dst, src, writes=[bconst], join=True)
        cload(ident[:], T['c_ident'])
        cload(identf[:], T['c_ident'])
        cload(onesb[:], T['c_ones'])
        cload(trib[:], T['c_tri'])
        cload(poolm[:], T['c_pool'].rearrange("k p f -> p k f"))
        cload(maskb[:], T['c_mask'].rearrange("k p f -> p k f"))
        cload(iota[:], T['c_iota'])
        cload(invf[:], T['c_invf'])

        def load_slab(W2d, r0, nrows, c0, ncols, dst=None):
            if dst is None:
                tile, buf = nxt('slab', slabs)
                bufs = [buf]
                flat = tile[:, :]
            else:
                flat, bufs = dst
            kc = nrows // 128
            assert nrows % 128 == 0 and kc * ncols <= flat.shape[1], (nrows, ncols, flat.shape)
            view = flat[:, 0:kc * ncols].rearrange("p (j f) -> p j f", f=ncols)
            if hasattr(W2d, 'slab'):
                P.dma('pool', view, W2d.slab(r0, nrows, c0, ncols), writes=bufs)
                return view, bufs
            src = W2d[r0:r0 + nrows, c0:c0 + ncols].rearrange("(j p) f -> p j f", p=128)
            step = 8
            for j0 in range(0, kc, step):
                j1 = min(kc, j0 + step)
                P.dma('pool', view[:, j0:j1, :], src[:, j0:j1, :], writes=bufs, join=(j0 > 0))
            return view, bufs

        def bcast_row(dst, row_ap, bufs, first=True):
            P.dma('sp', dst, row_ap.partition_broadcast(128), writes=bufs, join=not first)

        def mm_acc(ps, psb, pairs, reads, first=True, last=True):
            n = len(pairs)
            for i, (l, r) in enumerate(pairs):
                P.op('pe', lambda e, l=l, r=r, i=i: e.matmul(ps, lhsT=l, rhs=r, start=(first and i == 0), stop=(last and i == n - 1)),
                     reads=reads, writes=[psb], join=not (first and i == 0))

        def transposes(srcs, src_reads, dst_fn, dst_writes, width=128):
            for q in range(0, len(srcs), 4):
                m = min(4, len(srcs) - q)
                tp, tpb = nxt('tp', TP)
                tpv = tp.rearrange("p (a b) -> p a b", b=128)
                for i in range(m):
                    P.op('pe', lambda e, i=i, s=srcs[q + i], tpv=tpv: e.transpose(out=tpv[0:width, i, :], in_=s, identity=ident[:]),
                         reads=src_reads + [bconst], writes=[tpb], join=(i > 0))
                copy_op(ev_eng(), dst_fn(q, m), tpv[0:width, 0:m, :], [tpb], dst_writes, join=True)

        def rstd_from_ss(ss_ap, out_ap, n, statb, scale_extra=1.0):
            P.op('dve', lambda e: e.tensor_scalar(out=out_ap, in0=ss_ap, scalar1=1.0 / n, scalar2=EPS, op0=ALU.mult, op1=ALU.add),
                 reads=statb, writes=statb, join=True)
            P.op('act', lambda e: e.activation(out=out_ap, in_=out_ap, func=AF.Sqrt), reads=statb, writes=statb, join=True)
            P.op('dve', lambda e: e.reciprocal(out=out_ap, in_=out_ap), reads=statb, writes=statb, join=True)
            if scale_extra != 1.0:
                P.op('dve', lambda e: e.tensor_scalar(out=out_ap, in0=out_ap, scalar1=float(scale_extra), scalar2=None, op0=ALU.mult),
                     reads=statb, writes=statb, join=True)

        def store(dram_ap, sb_ap, reads, dbuf, join=True, queue='sp'):
            return P.dma(queue, dram_ap, sb_ap, reads=reads, writes=[dbuf], join=join)

        def phase_rope_tables():
            posi = P.sbuf('posi', [128, 16], I32)
            posf = P.sbuf('posf', [128, 16], F32)
            argt_, b1 = rb(0, 4, F32)
            nf_, b2 = rb(4, 4, F32)
            ni_, b3 = rb(8, 4, F32)
            argt = argt_.rearrange("p (t f) -> p t f", f=64)
            nf = nf_.rearrange("p (t f) -> p t f", f=64)
            ni = ni_.bitcast(I32).rearrange("p (t f) -> p t f", f=64)
            bp = Buf('posi')
            J = dict(reads=[bp, bconst] + b1 + b2 + b3, writes=[bp] + b1 + b2 + b3, join=True)
            P.dma('sp', posi[:], T['pos'], writes=[bp])
            P.op('dve', lambda e: e.tensor_copy(out=posf[:], in_=posi[:]), reads=[bp], writes=[bp])
            for t in range(NT):
                P.op('dve', lambda e, t=t: e.tensor_scalar(out=argt[:, t, 0:32], in0=invf[:], scalar1=posf[:, t:t + 1], scalar2=None, op0=ALU.mult), **J)
            P.op('dve', lambda e: e.tensor_scalar(out=argt[:, :, 32:64], in0=argt[:, :, 0:32], scalar1=math.pi / 2, scalar2=None, op0=ALU.add), **J)
            P.op('dve', lambda e: e.tensor_scalar(out=nf[:], in0=argt[:], scalar1=1.0 / (2 * math.pi), scalar2=None, op0=ALU.mult), **J)
            P.op('dve', lambda e: e.tensor_copy(out=ni[:], in_=nf[:]), **J)
            P.op('dve', lambda e: e.tensor_copy(out=nf[:], in_=ni[:]), **J)
            P.op('dve', lambda e: e.scalar_tensor_tensor(out=argt[:], in0=nf[:], scalar=-2 * math.pi, in1=argt[:], op0=ALU.mult, op1=ALU.add), **J)
            P.op('dve', lambda e: e.tensor_scalar(out=nf[:], in0=argt[:], scalar1=math.pi, scalar2=None, op0=ALU.is_gt), **J)
            P.op('dve', lambda e: e.scalar_tensor_tensor(out=argt[:], in0=nf[:], scalar=-2 * math.pi, in1=argt[:], op0=ALU.mult, op1=ALU.add), **J)
            P.op('dve', lambda e: e.tensor_scalar(out=nf[:], in0=argt[:], scalar1=-math.pi, scalar2=None, op0=ALU.is_lt), **J)
            P.op('dve', lambda e: e.scalar_tensor_tensor(out=argt[:], in0=nf[:], scalar=2 * math.pi, in1=argt[:], op0=ALU.mult, op1=ALU.add), **J)
            P.op('act', lambda e: e.activation(out=SC[:], in_=argt[:], func=AF.Sin), reads=[bp] + b1, writes=[bcs])

        bhtm = Buf('htm_d')

        def phase_norm(src2d, ntok, g_row, mode, dst_view=None, dst_bufs_fn=None, src_is_out=False, router=False):
            GBv, GBb = rb(24, 8, F32)
            bcast_row(GBv, g_row, GBb)
            for t in range(ntok // 128):
                xt, xb = rb(8 * (t % 2), 8, F32)
                hb, hbb = rb(16 + 4 * (t % 2), 4)
                stt, stb = nxt('stat', STAT)
                P.dma('sp', xt, src2d[t * 128:(t + 1) * 128, :], reads=[ybuf[t]] if src_is_out else [], writes=xb)
                chk('n0')
                P.op('act', lambda e, xt=xt, hb=hb, stt=stt: e.activation(out=hb, in_=xt, func=AF.Square, accum_out=stt[:, 0:1]),
                     reads=xb, writes=hbb + [stb])
                chk('n1')
                rstd_from_ss(stt[:, 0:1], stt[:, 1:2], 2048, [stb])
                chk('n2')
                if router:
                    hf, hfb = rb(32, 8, F32)
                    P.op('dve', lambda e, xt=xt, hf=hf, stt=stt: e.scalar_tensor_tensor(out=hf, in0=xt, scalar=stt[:, 1:2], in1=GBv,
                                                                                      op0=ALU.mult, op1=ALU.mult), reads=xb + [stb] + GBb, writes=hfb)
                    P.op('act', lambda e, hb=hb, hf=hf: e.activation(out=hb, in_=hf, func=AF.Copy), reads=hfb, writes=hbb)
                    router_tile(t, hf, hfb)
                else:
                    P.op('dve', lambda e, xt=xt, hb=hb, stt=stt: e.scalar_tensor_tensor(out=hb, in0=xt, scalar=stt[:, 1:2], in1=GBv,
                                                                                      op0=ALU.mult, op1=ALU.mult), reads=xb + [stb] + GBb, writes=hbb)
                chk('n3')
                if mode == 'fm':
                    transposes([hb[:, c * 128:(c + 1) * 128] for c in range(16)], hbb,
                               lambda q, m, t=t: dst_view[:, q:q + m, t * 128:(t + 1) * 128], dst_bufs_fn(t))
                else:
                    store(htm_d[t * 128:(t + 1) * 128, :], hb, hbb, bhtm, join=(t > 0))
                chk('n4')
                chk('n4x')

        def router_tile(t, hf, hfb):
            ht32, htb = rb(40, 8, F32)
            htv = ht32.rearrange("p (c t) -> p c t", t=128)
            for q in range(0, 16, 4):
                ps, psb = nxt('mm', MM)
                psv = ps.rearrange("p (a b) -> p a b", b=128)
                for i in range(4):
                    P.op('pe', lambda e, i=i, q=q, psv=psv: e.transpose(out=psv[:, i, :], in_=hf[:, (q + i) * 128:(q + i + 1) * 128], identity=identf[:]),
                         reads=hfb + [bconst], writes=[psb], join=(i > 0))
                copy_op(ev_eng(), htv[:, q:q + 4, :], psv[:, :, :], [psb], htb, join=(q > 0))
            ps, psb = AUX
            mm_acc(ps[:, 0:8], psb, [(htv[:, j, :], RW[:, j, :]) for j in range(16)], htb + [brw])
            stt, stb = nxt('stat', STAT)
            lg = stt[:, 0:8]
            P.op('dve', lambda e: e.tensor_tensor(out=lg, in0=ps[:, 0:8], in1=RBI[:], op=ALU.add), reads=[psb, brw], writes=[stb])
            m1, eq1, l2, m2, eq2, dd = stt[:, 8:9], stt[:, 16:24], stt[:, 24:32], stt[:, 9:10], stt[:, 32:40], stt[:, 10:13]
            J = dict(reads=[stb], writes=[stb], join=True)
            P.op('dve', lambda e: e.reduce_max(out=m1, in_=lg, axis=AX.X), **J)
            P.op('dve', lambda e: e.tensor_scalar(out=eq1, in0=lg, scalar1=m1, scalar2=None, op0=ALU.is_equal), **J)
            P.op('dve', lambda e: e.scalar_tensor_tensor(out=l2, in0=eq1, scalar=-1e30, in1=lg, op0=ALU.mult, op1=ALU.add), **J)
            P.op('dve', lambda e: e.reduce_max(out=m2, in_=l2, axis=AX.X), **J)
            P.op('dve', lambda e: e.tensor_scalar(out=eq2, in0=l2, scalar1=m2, scalar2=None, op0=ALU.is_equal), **J)
            P.op('dve', lambda e: e.tensor_tensor(out=dd[:, 0:1], in0=m2, in1=m1, op=ALU.subtract), **J)
            P.op('act', lambda e: e.activation(out=dd[:, 0:1], in_=dd[:, 0:1], func=AF.Exp), **J)
            P.op('dve', lambda e: e.tensor_scalar(out=dd[:, 1:2], in0=dd[:, 0:1], scalar1=1.0, scalar2=None, op0=ALU.add), **J)
            P.op('dve', lambda e: e.reciprocal(out=dd[:, 1:2], in_=dd[:, 1:2]), **J)
            P.op('dve', lambda e: e.tensor_tensor(out=dd[:, 2:3], in0=dd[:, 0:1], in1=dd[:, 1:2], op=ALU.mult), **J)
            P.op('dve', lambda e: e.tensor_tensor(out=RSEL[:, t, :], in0=eq1, in1=eq2, op=ALU.add), reads=[stb], writes=[brt], join=True)
            P.op('dve', lambda e: e.tensor_scalar(out=RWT[:, t, :], in0=eq1, scalar1=dd[:, 1:2], scalar2=None, op0=ALU.mult), reads=[stb], writes=[brt], join=True)
            P.op('dve', lambda e: e.scalar_tensor_tensor(out=RWT[:, t, :], in0=eq2, scalar=dd[:, 2:3], in1=RWT[:, t, :], op0=ALU.mult, op1=ALU.add),
                 reads=[stb, brt], writes=[brt], join=True)
            P.op('dve', lambda e: e.tensor_copy(out=RSELB[:, t, :], in_=RSEL[:, t, :]), reads=[brt], writes=[brt], join=True)

        bq_d, bkn_d, bkr_d, bv_d = Buf('qT_d'), Buf('knT_d'), Buf('krT_d'), Buf('V_d')
        bmix_d = [Buf(f'mix_d{i}') for i in range(4)]
        bxat_d = [Buf(f'xat_d{i}') for i in range(4)]
        batt_d = [Buf(f'att_d{i}') for i in range(4)]
        bg_d = [Buf(f'g_d{i}') for i in range(4)]

        def phase_mixer(l):
            y_src = T['x'] if l == layers[0] else OUT
            tw_a, tw_b, tw_c = TW(T['tw_in_a'][l], 2048, 256, 0), TW(T['tw_in_b'][l], 2048, 160, 512), TW(T['tw_in_c'][l], 2048, 256, 832)

            class _WIn:
                def slab(self, r0, nrows, c0, ncols):
                    return (tw_a if c0 < 512 else tw_b if c0 < 832 else tw_c).slab(r0, nrows, c0, ncols)
            w_in = _WIn()
            hTv = RA[:, 0:16 * S].rearrange("p (c t) -> p c t", t=S)
            hbuf = lambda t: [RAq[t // 4]]

            phase_norm(y_src, S, T['attn_norm_g'][l], 'fm', hTv, hbuf, src_is_out=(l > layers[0]))
            if hT_dbg is not None and l == 0:
                bdb = Buf('hT_dbg')
                for c in range(16):
                    finals.append(store(hT_dbg[c * 128:(c + 1) * 128, :], hTv[:, c, :], RAq[0:4], bdb, join=(c > 0)))
            memnT, memb = rb(32, 8)
            memv = memnT.rearrange("p (c t) -> p c t", t=256)
            phase_norm(T['mem'], 256, T['mem_norm_g'][l], 'fm', memv, lambda t: memb)
            bcast_row(GAIN[:, 0:512], T['mla_q_a_norm_g'][l], [bgain], first=True)
            bcast_row(GAIN[:, 512:768], T['mla_kv_a_norm_g'][l], [bgain], first=False)
            bcast_row(GAIN[:, 768:960], T['mla_q_norm_g'][l], [bgain], first=False)
            bcast_row(GAIN[:, 960:1152], T['mla_k_norm_g'][l], [bgain], first=False)
            bcast_row(GAIN[:, 1152:1408], T['mem_q_norm_g'][l], [bgain], first=False)
            bcast_row(GAIN[:, 1408:1664], T['mem_k_norm_g'][l], [bgain], first=False)
            P.dma('sp', PSC[:], T['pool_scale'][l].rearrange("(c p) -> p c", p=128), writes=[bgain], join=True, allow_slow_non_contiguous=True)
            P.op('dve', lambda e: e.tensor_tensor(out=GAIN[:, 768:896], in0=GAIN[:, 768:896], in1=GAIN[:, 960:1088], op=ALU.mult),
                 reads=[bgain], writes=[bgain], join=True)
            P.op('dve', lambda e: e.tensor_tensor(out=GAIN[:, 1152:1408], in0=GAIN[:, 1152:1408], in1=GAIN[:, 1408:1664], op=ALU.mult),
                 reads=[bgain], writes=[bgain], join=True)
            G_QA, G_KVA, G_Q, G_KPE, G_QM = GAIN[:, 0:512], GAIN[:, 512:768], GAIN[:, 768:960], GAIN[:, 1088:1152], GAIN[:, 1152:1408]

            chk('A')
            kmT, kmb = rb(40, 4)
            kmv = kmT.rearrange("p (c t) -> p c t", t=256)
            vm, vmb = rb(44, 4)
            vmv = vm.rearrange("p (m f) -> p m f", f=1024)
            wkv = TW(T['tw_memkv'][l], 2048, 256)
            for cb in range(8):
                sl, slb = load_slab(wkv, 0, 2048, cb * 256, 256)
                if cb < 4:
                    for c in range(2):
                        ps, psb = nxt('mm', MM)
                        mm_acc(ps[:, 0:256], psb, [(sl[:, j, c * 128:(c + 1) * 128], memv[:, j, :]) for j in range(16)], slb + memb)
                        copy_op(ev_eng(), kmv[:, cb * 2 + c, :], ps[:, 0:256], [psb], kmb, join=True)
                    for mt in range(2):
                        ps, psb = nxt('mm', MM)
                        mm_acc(ps[:, 0:256], psb, [(memv[:, j, mt * 128:(mt + 1) * 128], sl[:, j, :]) for j in range(16)], slb + memb)
                        jk, jkb = rb(16, 4)
                        P.op('act', lambda e, ps=ps, jk=jk, mt=mt, cb=cb: e.activation(out=jk[:, 0:256], in_=ps[:, 0:256], func=AF.Square,
                                                                                    accum_out=SCM[:, mt, cb:cb + 1]), reads=[psb], writes=jkb + [bscm])
                else:
                    for mt in range(2):
                        ps, psb = nxt('mm', MM)
                        mm_acc(ps[:, 0:256], psb, [(memv[:, j, mt * 128:(mt + 1) * 128], sl[:, j, :]) for j in range(16)], slb + memb)
                        copy_op(ev_eng(), vmv[:, mt, (cb - 4) * 256:(cb - 3) * 256], ps[:, 0:256], [psb], vmb, join=True)
            scm2 = SCM[:, :, :].rearrange("p a b -> p (a b)")
            rstd_from_ss(scm2, scm2, 256, [bscm], scale_extra=256 ** -0.5)

            chk('Bmem')
            wuq, wuqb = rb(48, 12)
            wuqv, _ = load_slab(T['mla_w_uq'][l], 0, 512, 0, 1536, dst=(wuq, wuqb))
            wkvA, wkvAb = rb(32, 4)
            wkvAv, _ = load_slab(T['mla_w_ukv'][l], 0, 256, 0, 1024, dst=(wkvA, wkvAb))
            wkvB, wkvBb = rb(36, 4)
            wkvBv, _ = load_slab(T['mla_w_ukv'][l], 0, 256, 1024, 1024, dst=(wkvB, wkvBb))

            def tm_block(t, sl2, w):
                ps, psb = nxt('mm', MM)
                for half, (sl, slb) in enumerate(sl2):
                    for j in range(16):
                        P.op('pe', lambda e, j=j, sl=sl, ps=ps, t=t, half=half: e.matmul(ps[:, half * w:(half + 1) * w], lhsT=hTv[:, j, t * 128:(t + 1) * 128],
                                                                                        rhs=sl[:, j, :], start=(j == 0), stop=(j == 15)),
                             reads=hbuf(t) + slb, writes=[psb], join=not (half == 0 and j == 0))
                return ps, psb

            sl2 = [load_slab(w_in, 0, 2048, 0, 256), load_slab(w_in, 0, 2048, 256, 256)]
            for t in range(NT):
                ps, psb = tm_block(t, sl2, 256)
                stt, stb = nxt('stat', STAT)
                jk, jkb = rb(16, 4)
                P.op('act', lambda e, ps=ps, jk=jk, stt=stt: e.activation(out=jk[:, 0:512], in_=ps[:, :], func=AF.Square, accum_out=stt[:, 0:1]),
                     reads=[psb], writes=jkb + [stb])
                rstd_from_ss(stt[:, 0:1], stt[:, 1:2], 512, [stb])
                cqn, cqnb = rb(20, 4)
                P.op('dve', lambda e, ps=ps, cqn=cqn, stt=stt: e.scalar_tensor_tensor(out=cqn[:, 0:512], in0=ps[:, :], scalar=stt[:, 1:2], in1=G_QA,
                                                                                    op0=ALU.mult, op1=ALU.mult), reads=[psb, stb, bgain], writes=cqnb)
                cqT, cqTb = rb(0, 1)
                cqTv = cqT.rearrange("p (c t) -> p c t", t=128)
                transposes([cqn[:, c * 128:(c + 1) * 128] for c in range(4)], cqnb, lambda q, m: cqTv[:, q:q + m, :], cqTb[0:1])
                qf, qfb = rb(8, 8, F32)
                for nb in range(3):
                    ps2, ps2b = nxt('mm', MM)
                    mm_acc(ps2[:, :], ps2b, [(cqTv[:, j, :], wuqv[:, j, nb * 512:(nb + 1) * 512]) for j in range(4)], cqTb[0:1] + wuqb)
                    copy_op(ev_eng(), qf[:, nb * 512:(nb + 1) * 512], ps2[:, :], [ps2b], qfb, join=True)
                q_epilogue(t, qf, qfb, G_Q)

            chk('Bcq')
            sl2 = [load_slab(w_in, 0, 2048, 512, 160), load_slab(w_in, 0, 2048, 672, 160)]
            for t in range(NT):
                ps, psb = tm_block(t, sl2, 160)
                chk('k0')
                kv_epilogue(t, ps, psb, G_KVA, G_KPE, wkvAv, wkvAb, wkvBv, wkvBb)

            chk('Bckv')
            pwt, pwtb = rb(48, 4)
            pwv = pwt.rearrange("p (g j f) -> p g j f", g=4, j=2)
            for g in range(4):
                P.dma('pool', pwv[:, g, :, :], T['pool_w'][l][g].rearrange("(j p) f -> p j f", p=128), writes=pwtb, join=(g > 0))
            for ub in range(2):
                sl2 = [load_slab(w_in, 0, 2048, 832 + ub * 512, 256), load_slab(w_in, 0, 2048, 832 + ub * 512 + 256, 256)]
                for t in range(NT):
                    ps, psb = tm_block(t, sl2, 256)
                    ucur, ucb = rb(4 * (t % 2), 1)
                    copy_op(ev_eng(), ucur[:, 0:512], ps[:, :], [psb], ucb[0:1])
                    uprev, upb = rb(4 * ((t + 1) % 2), 1)
                    pp, ppb = nxt('mm', MM)
                    for gg in range(2):
                        g = 2 * ub + gg
                        pairs = [(poolm[:, 3 * g + (1 if t > 0 else 0), :], ucur[:, gg * 256:(gg + 1) * 256])]
                        if t > 0:
                            pairs.append((poolm[:, 3 * g + 2, :], uprev[:, gg * 256:(gg + 1) * 256]))
                        n = len(pairs)
                        for i, (lh, rh) in enumerate(pairs):
                            P.op('pe', lambda e, lh=lh, rh=rh, i=i, n=n, pp=pp, gg=gg: e.matmul(pp[:, gg * 256:(gg + 1) * 256], lhsT=lh, rhs=rh, start=(i == 0), stop=(i == n - 1)),
                                 reads=ucb[0:1] + upb[0:1] + [bconst], writes=[ppb], join=not (gg == 0 and i == 0))
                    pl, plb = rb(8 + 4 * (t % 2), 1)
                    copy_op(ev_eng(), pl[:, 0:512], pp[:, :], [ppb], plb[0:1])
                    plT, plTb = rb(24 + 4 * (t % 2), 1)
                    plTv = plT.rearrange("p (c t) -> p c t", t=128)
                    transposes([pl[:, c * 128:(c + 1) * 128] for c in range(4)], plb[0:1], lambda q, m: plTv[:, q:q + m, :], plTb[0:1])
                    pm_, pmb = nxt('mm', MM)
                    for gg in range(2):
                        g = 2 * ub + gg
                        for hc in range(2):
                            for j in range(2):
                                P.op('pe', lambda e, g=g, gg=gg, hc=hc, j=j, pm_=pm_, plTv=plTv: e.matmul(pm_[:, (gg * 2 + hc) * 128:(gg * 2 + hc + 1) * 128], lhsT=pwv[:, g, j, hc * 128:(hc + 1) * 128],
                                                                                            rhs=plTv[:, gg * 2 + j, :], start=(j == 0), stop=(j == 1)),
                                     reads=plTb[0:1] + pwtb, writes=[pmb], join=not (gg == 0 and hc == 0 and j == 0))
                    mx, mxb = rb(52 + 4 * (t % 2), 1)
                    mxv = mx.rearrange("p (c t) -> p c t", t=128)
                    for cc in range(4):
                        P.op('dve', lambda e, cc=cc, mxv=mxv, pm_=pm_, ub=ub: e.tensor_scalar(out=mxv[:, cc, :], in0=pm_[:, cc * 128:(cc + 1) * 128],
                                                                                             scalar1=PSC[:, ub * 4 + cc:ub * 4 + cc + 1], scalar2=None, op0=ALU.mult),
                             reads=[pmb, bgain], writes=mxb[0:1], join=(cc > 0))
                    store(mixT_d[ub * 512:(ub + 1) * 512, t * 128:(t + 1) * 128].rearrange("(c p) t -> p c t", p=128), mxv, mxb[0:1], bmix_d[t // 4])

            chk('Bu')
            for qb in range(2):
                sl2 = [load_slab(w_in, 0, 2048, 1856 + qb * 512, 256), load_slab(w_in, 0, 2048, 1856 + qb * 512 + 256, 256)]
                for t in range(NT):
                    ps, psb = tm_block(t, sl2, 256)
                    stt, stb = nxt('stat', STAT)
                    jk, jkb = rb(16 if t % 2 == 0 else 8, 4)
                    for hh in range(2):
                        P.op('act', lambda e, ps=ps, jk=jk, stt=stt, hh=hh: e.activation(out=jk[:, hh * 256:(hh + 1) * 256], in_=ps[:, hh * 256:(hh + 1) * 256], func=AF.Square,
                                                                                        accum_out=stt[:, hh:hh + 1]), reads=[psb], writes=jkb + [stb], join=(hh > 0))
                    rstd_from_ss(stt[:, 0:2], stt[:, 2:4], 256, [stb])
                    qmn, qmnb = rb(20 if t % 2 == 0 else 12, 4)
                    for hh in range(2):
                        P.op('dve', lambda e, ps=ps, qmn=qmn, stt=stt, hh=hh: e.scalar_tensor_tensor(out=qmn[:, hh * 256:(hh + 1) * 256], in0=ps[:, hh * 256:(hh + 1) * 256],
                                                                                                 scalar=stt[:, 2 + hh:3 + hh], in1=G_QM, op0=ALU.mult, op1=ALU.mult),
                             reads=[psb, stb, bgain], writes=qmnb, join=(hh > 0))
                    qmT, qmTb = rb(4 * (t % 2), 1)
                    qmTv = qmT.rearrange("p (c t) -> p c t", t=128)
                    transposes([qmn[:, c * 128:(c + 1) * 128] for c in range(4)], qmnb, lambda q, m: qmTv[:, q:q + m, :], qmTb[0:1])
                    xo, xob = rb(32 + 4 * (t % 2), 1)
                    xov = xo.rearrange("p (c t) -> p c t", t=128)
                    for hh in range(2):
                        h = 2 * qb + hh
                        pT, pTb = rb(24 + 4 * ((2 * t + hh) % 2), 1)
                        pTv = pT[:, 0:256].rearrange("p (m t) -> p m t", t=128)
                        for mt in range(2):
                            ps2, ps2b = nxt('mm', MM)
                            mm_acc(ps2[:, 0:128], ps2b, [(kmv[:, 2 * h + dd, mt * 128:(mt + 1) * 128], qmTv[:, 2 * hh + dd, :]) for dd in range(2)], kmb + qmTb[0:1])
                            P.op('act', lambda e, ps2=ps2, pTv=pTv, mt=mt, h=h: e.activation(out=pTv[:, mt, :], in_=ps2[:, 0:128], func=AF.Exp, scale=SCM[:, mt, h:h + 1]),
                                 reads=[ps2b, bscm], writes=pTb[0:1], join=(mt > 0))
                        ps3, ps3b = nxt('mm', MM)
                        first = True
                        for dv in range(2):
                            for mt in range(2):
                                P.op('pe', lambda e, dv=dv, mt=mt, h=h, ps3=ps3, pTv=pTv: e.matmul(ps3[:, dv * 128:(dv + 1) * 128], lhsT=vmv[:, mt, h * 256 + dv * 128:h * 256 + (dv + 1) * 128],
                                                                                              rhs=pTv[:, mt, :], start=(mt == 0), stop=(mt == 1)),
                                     reads=vmb + pTb[0:1], writes=[ps3b], join=not first)
                                first = False
                        for mt in range(2):
                            P.op('pe', lambda e, mt=mt, ps3=ps3, pTv=pTv: e.matmul(ps3[:, 256:384], lhsT=onesb[:], rhs=pTv[:, mt, :], start=(mt == 0), stop=(mt == 1)),
                                 reads=pTb[0:1] + [bconst], writes=[ps3b], join=True)
                        rc, rcb = rb(52 + 4 * ((2 * t + hh) % 2), 1, F32)
                        P.op('dve', lambda e, rc=rc, ps3=ps3: e.reciprocal(out=rc[:, 0:128], in_=ps3[:, 256:384]), reads=[ps3b], writes=rcb[0:1])
                        for dv in range(2):
                            P.op('dve', lambda e, dv=dv, hh=hh, xov=xov, ps3=ps3, rc=rc: e.tensor_tensor(out=xov[:, hh * 2 + dv, :], in0=ps3[:, dv * 128:(dv + 1) * 128], in1=rc[:, 0:128], op=ALU.mult),
                                 reads=[ps3b] + rcb[0:1], writes=xob[0:1], join=not (hh == 0 and dv == 0))
                    store(xattT_d[qb * 512:(qb + 1) * 512, t * 128:(t + 1) * 128].rearrange("(c p) t -> p c t", p=128), xov, xob[0:1], bxat_d[t // 4])

            chk('Bqm')
            for cb in range(24):
                sl, slb = load_slab(w_in, 0, 2048, 2880 + cb * 256, 256)
                for c in range(2):
                    for qt in range(4):
                        ps, psb = nxt('mm', MM)
                        mm_acc(ps[:, :], psb, [(sl[:, j, c * 128:(c + 1) * 128], hTv[:, j, qt * 512:(qt + 1) * 512]) for j in range(16)], slb + [RAq[qt]])
                        sg, sgb = rb(16 + 4 * (rr.get('sg', 0) % 4), 1)
                        rr['sg'] = rr.get('sg', 0) + 1
                        P.op('act', lambda e, sg=sg, ps=ps: e.activation(out=sg[:, 0:512], in_=ps[:, :], func=AF.Sigmoid), reads=[psb], writes=sgb[0:1])
                        r0 = cb * 256 + c * 128
                        store(gT_d[r0:r0 + 128, qt * 512:(qt + 1) * 512], sg[:, 0:512], sgb[0:1], bg_d[qt])

            chk('C')
            phase_attention()

            chk('D')
            mrgv = RA[:, 0:16 * S].rearrange("p (c t) -> p c t", t=S)
            wouts = (TW(T['tw_mlaout'][l], 1024, 512), TW(T['tw_poolout'][l], 1024, 512), TW(T['tw_memout'][l], 1024, 512))
            srcs = (attT_d, mixT_d, xattT_d)
            sbufs = (batt_d, bmix_d, bxat_d)
            for qt in range(4):
                xs = []
                for b in range(3):
                    xt_, xtb = rb(8 * b, 8)
                    xv = xt_.rearrange("p (c t) -> p c t", t=512)
                    P.dma('sp', xv, srcs[b][:, qt * 512:(qt + 1) * 512].rearrange("(c p) t -> p c t", p=128), reads=[sbufs[b][qt]], writes=xtb)
                    xs.append((xv, xtb))
                for cb in range(4):
                    acc, accb = rb(24, 8, F32)
                    accv = acc.rearrange("p (c t) -> p c t", t=512)
                    for b in range(3):
                        sl, slb = load_slab(wouts[b], 0, 1024, cb * 512, 512)
                        gt, gtb = rb(32 + 4 * (b % 2), 4)
                        gtv = gt.rearrange("p (c t) -> p c t", t=512)
                        r0 = b * 2048 + cb * 512
                        P.dma('sp', gtv, gT_d[r0:r0 + 512, qt * 512:(qt + 1) * 512].rearrange("(c p) t -> p c t", p=128), reads=[bg_d[qt]], writes=gtb)
                        for c in range(4):
                            ps, psb = nxt('mm', MM)
                            mm_acc(ps[:, :], psb, [(sl[:, j, c * 128:(c + 1) * 128], xs[b][0][:, j, :]) for j in range(8)], slb + xs[b][1])
                            if b == 0:
                                P.op('dve', lambda e, c=c, ps=ps, accv=accv, gtv=gtv: e.tensor_tensor(out=accv[:, c, :], in0=ps[:, :], in1=gtv[:, c, :], op=ALU.mult),
                                     reads=[psb] + gtb, writes=accb, join=(c > 0))
                            else:
                                tmp, tmpb = rb(40 + 2 * (c % 2), 2, F32)
                                P.op('dve', lambda e, c=c, ps=ps, tmp=tmp, gtv=gtv: e.tensor_tensor(out=tmp[:, 0:512], in0=ps[:, :], in1=gtv[:, c, :], op=ALU.mult),
                                     reads=[psb] + gtb, writes=tmpb)
                                if b == 1:
                                    P.op('pool', lambda e, c=c, accv=accv, tmp=tmp: e.tensor_tensor(out=accv[:, c, :], in0=accv[:, c, :], in1=tmp[:, 0:512], op=ALU.add),
                                         reads=accb + tmpb, writes=accb, join=True)
                                else:
                                    P.op('pool', lambda e, c=c, cb=cb, qt=qt, accv=accv, tmp=tmp: e.tensor_tensor(out=mrgv[:, cb * 4 + c, qt * 512:(qt + 1) * 512], in0=accv[:, c, :],
                                                                                                                 in1=tmp[:, 0:512], op=ALU.add),
                                         reads=accb + tmpb, writes=[RAq[qt]], join=True)
            if mrg_dbg is not None and l == 0:
                bdb = Buf('mrg_dbg')
                for c in range(16):
                    finals.append(store(mrg_dbg[c * 128:(c + 1) * 128, :], mrgv[:, c, :], RAq[0:4], bdb, join=(c > 0)))

            chk('E')
            wo = TW(T['tw_o'][l], 2048, 256)
            for cb in range(8):
                sl, slb = load_slab(wo, 0, 2048, cb * 256, 256)
                for t in range(NT):
                    ps, psb = nxt('mm', MM)
                    mm_acc(ps[:, 0:256], psb, [(mrgv[:, j, t * 128:(t + 1) * 128], sl[:, j, :]) for j in range(16)], slb + [RAq[t // 4]])
                    yt, ytb = rb(44 + 4 * (rr.get('yt', 0) % 4), 1, F32)
                    rr['yt'] = rr.get('yt', 0) + 1
                    P.dma('sp', yt[:, 0:256], y_src[t * 128:(t + 1) * 128, cb * 256:(cb + 1) * 256], reads=[ybuf[t]] if l > layers[0] else [], writes=ytb[0:1])
                    P.op('dve', lambda e, yt=yt, ps=ps: e.tensor_tensor(out=yt[:, 0:256], in0=ps[:, 0:256], in1=yt[:, 0:256], op=ALU.add), reads=[psb] + ytb[0:1], writes=ytb[0:1])
                    o = store(OUT[t * 128:(t + 1) * 128, cb * 256:(cb + 1) * 256], yt[:, 0:256], ytb[0:1], ybuf[t], join=(l == layers[0] and cb > 0))
                    finals.append(o)

        def q_epilogue(t, qf, qfb, G_Q):
            stt, stb = nxt('stat', STAT)
            qv = qf[:, 0:1536].rearrange("p (h d) -> p h d", d=192)
            sq, sqb = rb(24, 8, F32)
            sqv = sq[:, 0:1536].rearrange("p (h d) -> p h d", d=192)
            P.op('act', lambda e: e.activation(out=sq[:, 0:1536], in_=qf[:, 0:1536], func=AF.Square), reads=qfb, writes=sqb)
            P.op('dve', lambda e: e.tensor_reduce(out=stt[:, 0:8], in_=sqv, axis=AX.X, op=ALU.add), reads=sqb, writes=[stb])
            rstd_from_ss(stt[:, 0:8], stt[:, 8:16], 192, [stb])
            P.op('dve', lambda e: e.tensor_tensor(out=sqv, in0=qv, in1=stt[:, 8:16].unsqueeze(2).broadcast_to([128, 8, 192]), op=ALU.mult),
                 reads=qfb + [stb], writes=sqb)
            P.op('dve', lambda e: e.tensor_tensor(out=sqv, in0=sqv, in1=G_Q.unsqueeze(1).broadcast_to([128, 8, 192]), op=ALU.mult),
                 reads=sqb + [bgain], writes=sqb)
            qb_, qbb = rb(20, 4)
            qbn = qb_[:, 0:1024].rearrange("p (h d) -> p h d", d=128)
            qbr = qb_[:, 1024:1536].rearrange("p (h d) -> p h d", d=64)
            P.op('act', lambda e: e.activation(out=qbn, in_=sqv[:, :, 0:128], func=AF.Copy), reads=sqb, writes=qbb)
            x1, x2 = sqv[:, :, 128:160], sqv[:, :, 160:192]
            cb_ = COS[:, t, :].unsqueeze(1).broadcast_to([128, 8, 32])
            sb_ = SIN[:, t, :].unsqueeze(1).broadcast_to([128, 8, 32])
            tm, tmb = rb(60, 4, F32)
            t1 = tm[:, 0:256].rearrange("p (h d) -> p h d", d=32)
            t2 = tm[:, 256:512].rearrange("p (h d) -> p h d", d=32)
            P.op('pool', lambda e: e.tensor_tensor(out=t1, in0=x1, in1=cb_, op=ALU.mult), reads=sqb + [bcs], writes=tmb)
            P.op('pool', lambda e: e.tensor_tensor(out=t2, in0=x2, in1=sb_, op=ALU.mult), reads=sqb + [bcs], writes=tmb, join=True)
            P.op('pool', lambda e: e.tensor_tensor(out=qbr[:, :, 0:32], in0=t1, in1=t2, op=ALU.subtract), reads=tmb, writes=qbb, join=True)
            P.op('dve', lambda e: e.tensor_tensor(out=t1, in0=x2, in1=cb_, op=ALU.mult), reads=sqb + [bcs] + qbb, writes=tmb)
            P.op('dve', lambda e: e.tensor_tensor(out=t2, in0=x1, in1=sb_, op=ALU.mult), reads=sqb + [bcs], writes=tmb, join=True)
            P.op('dve', lambda e: e.tensor_tensor(out=qbr[:, :, 32:64], in0=t1, in1=t2, op=ALU.add), reads=tmb, writes=qbb, join=True)
            qs, qsb = rb(1, 2)
            qsn = qs[:, 0:1024].rearrange("p (h t) -> p h t", t=128)
            qsr_, qsrb = rb(5, 2)
            qsr = qsr_[:, 0:1024].rearrange("p (h t) -> p h t", t=128)
            transposes([qbn[:, h, :] for h in range(8)], qbb, lambda q, m: qsn[:, q:q + m, :], qsb)
            transposes([qbr[:, h, :] for h in range(8)], qbb, lambda q, m: qsr[0:64, q:q + m, :], qsrb, width=64)
            store(qnT_d[:, t * 128:(t + 1) * 128].rearrange("(h p) t -> p h t", p=128), qsn, qsb, bq_d, join=True)
            store(qrT_d[:, t * 128:(t + 1) * 128].rearrange("(h p) t -> p h t", p=64), qsr[0:64, :, :], qsrb, bq_d, join=True)

        def kv_epilogue(t, ps, psb, G_KVA, G_KPE, wkvAv, wkvAb, wkvBv, wkvBb):
            stt, stb = nxt('stat', STAT)
            jk, jkb = rb(16, 4)
            P.op('act', lambda e: e.activation(out=jk[:, 0:256], in_=ps[:, 0:256], func=AF.Square, accum_out=stt[:, 0:1]), reads=[psb], writes=jkb + [stb])
            P.op('act', lambda e: e.activation(out=jk[:, 256:320], in_=ps[:, 256:320], func=AF.Square, accum_out=stt[:, 2:3]), reads=[psb], writes=jkb + [stb], join=True)
            rstd_from_ss(stt[:, 0:1], stt[:, 1:2], 256, [stb])
            ckn, cknb = rb(20, 4)
            P.op('dve', lambda e: e.scalar_tensor_tensor(out=ckn[:, 0:256], in0=ps[:, 0:256], scalar=stt[:, 1:2], in1=G_KVA, op0=ALU.mult, op1=ALU.mult),
                 reads=[psb, stb, bgain], writes=cknb)
            chk('k1')
            kp, kpb = rb(60, 4, F32)
            P.op('dve', lambda e: e.tensor_tensor(out=kp[:, 0:64], in0=ps[:, 256:320], in1=G_KPE, op=ALU.mult), reads=[psb, bgain], writes=kpb)
            x1, x2 = kp[:, 0:32], kp[:, 32:64]
            c_, s_ = COS[:, t, :], SIN[:, t, :]
            t1, t2, t3, t4 = kp[:, 64:96], kp[:, 96:128], kp[:, 128:160], kp[:, 160:192]
            kr = ckn[:, 256:320]
            P.op('dve', lambda e: e.tensor_tensor(out=t1, in0=x1, in1=c_, op=ALU.mult), reads=kpb + [bcs], writes=kpb, join=True)
            P.op('dve', lambda e: e.tensor_tensor(out=t2, in0=x2, in1=s_, op=ALU.mult), reads=kpb + [bcs], writes=kpb, join=True)
            P.op('dve', lambda e: e.tensor_tensor(out=t3, in0=x2, in1=c_, op=ALU.mult), reads=kpb + [bcs], writes=kpb, join=True)
            P.op('dve', lambda e: e.tensor_tensor(out=t4, in0=x1, in1=s_, op=ALU.mult), reads=kpb + [bcs], writes=kpb, join=True)
            P.op('dve', lambda e: e.tensor_tensor(out=kr[:, 0:32], in0=t1, in1=t2, op=ALU.subtract), reads=kpb, writes=cknb, join=True)
            P.op('dve', lambda e: e.tensor_tensor(out=kr[:, 32:64], in0=t3, in1=t4, op=ALU.add), reads=kpb, writes=cknb, join=True)
            chk('k2')
            ckT, ckTb = rb(0, 1)
            ckTv = ckT[:, 0:256].rearrange("p (c t) -> p c t", t=128)
            transposes([ckn[:, c * 128:(c + 1) * 128] for c in range(2)], cknb, lambda q, m: ckTv[:, q:q + m, :], ckTb[0:1])
            krs, krsb = rb(1 + (t % 2), 1)
            krv = krs[:, 0:128].rearrange("p (c t) -> p c t", t=128)
            transposes([kr], cknb, lambda q, m: krv[0:64, q:q + m, :], krsb[0:1], width=64)
            store(krT_d[:, t * 128:(t + 1) * 128], krs[0:64, 0:128], krsb[0:1], bkr_d, join=True)
            chk('k3')
            vst, vstb = rb(3 + (t % 2) * 2, 2)
            kns, knsb = rb(8 + (t % 2) * 2, 2)
            sq, sqb = rb(24, 8, F32)
            for nb in range(4):
                wv, wb = (wkvAv, wkvAb) if nb < 2 else (wkvBv, wkvBb)
                ps2, ps2b = nxt('mm', MM)
                mm_acc(ps2[:, :], ps2b, [(ckTv[:, j, :], wv[:, j, (nb % 2) * 512:(nb % 2 + 1) * 512]) for j in range(2)], ckTb[0:1] + wb)
                for hh in range(2):
                    hd = 2 * nb + hh
                    kcol = ps2[:, hh * 256:hh * 256 + 128]
                    vcol = ps2[:, hh * 256 + 128:hh * 256 + 256]
                    fj = not (nb == 0 and hh == 0)
                    P.op('act', lambda e, kcol=kcol, hd=hd: e.activation(out=kns[:, hd * 128:(hd + 1) * 128], in_=kcol, func=AF.Copy),
                         reads=[ps2b], writes=knsb, join=fj)
                    P.op('dve', lambda e, vcol=vcol, hd=hd: e.tensor_copy(out=vst[:, hd * 128:(hd + 1) * 128], in_=vcol),
                         reads=[ps2b], writes=vstb, join=fj)
                    P.op('act', lambda e, kcol=kcol, hd=hd: e.activation(out=sq[:, hd * 128:(hd + 1) * 128], in_=kcol, func=AF.Square),
                         reads=[ps2b], writes=sqb, join=fj)
            chk('k4')
            store(V_d[t * 128:(t + 1) * 128, :], vst[:, 0:1024], vstb, bv_d, join=True)
            P.op('dve', lambda e: e.tensor_reduce(out=stt[:, 8:16], in_=sq[:, 0:1024].rearrange("p (h d) -> p h d", d=128), axis=AX.X, op=ALU.add), reads=sqb, writes=[stb], join=True)
            P.op('dve', lambda e: e.tensor_scalar(out=stt[:, 8:16], in0=stt[:, 8:16], scalar1=stt[:, 2:3], scalar2=None, op0=ALU.add), reads=[stb], writes=[stb], join=True)
            rstd_from_ss(stt[:, 8:16], stt[:, 8:16], 192, [stb], scale_extra=192 ** -0.5)
            P.op('dve', lambda e: e.tensor_copy(out=SCK[:, t, :], in_=stt[:, 8:16]), reads=[stb], writes=[bsck], join=True)
            chk('k5')
            knT, knTb = rb(12 + (t % 2) * 2, 2)
            knTv = knT[:, 0:1024].rearrange("p (h t) -> p h t", t=128)
            transposes([kns[:, h * 128:(h + 1) * 128] for h in range(8)], knsb, lambda q, m: knTv[:, q:q + m, :], knTb)
            store(knT_d[:, t * 128:(t + 1) * 128].rearrange("(h p) t -> p h t", p=128), knTv, knTb, bkn_d, join=True)

        def phase_attention():
            krT, krTb = rb(0, 4)
            P.dma('sp', krT[0:64, :], krT_d[:, :], reads=[bkr_d], writes=krTb)
            for h in range(8):
                o = 4 + (h % 2) * 16
                qn, qnb = rb(o, 4)
                qr, qrb = rb(o + 4, 4)
                kn, knb = rb(o + 8, 4)
                vh, vhb = rb(o + 12, 4)
                vhv = vh.rearrange("p (t d) -> p t d", d=128)
                P.dma('sp', qn, qnT_d[h * 128:(h + 1) * 128, :], reads=[bq_d], writes=qnb)
                P.dma('sp', qr[0:64, :], qrT_d[h * 64:(h + 1) * 64, :], reads=[bq_d], writes=qrb)
                P.dma('sp', kn, knT_d[h * 128:(h + 1) * 128, :], reads=[bkn_d], writes=knb)
                for hf_ in range(2):
                    P.dma('sp', vhv[:, hf_ * 8:(hf_ + 1) * 8, :], V_d[hf_ * 1024:(hf_ + 1) * 1024, h * 128:(h + 1) * 128].rearrange("(t p) d -> p t d", p=128),
                          reads=[bv_d], writes=vhb, join=(hf_ > 0))
                for qt in range(4):
                    nk = 4 * (qt + 1)
                    num, numb = MM[4]
                    den, denb = AUX
                    qs = slice(qt * 512, (qt + 1) * 512)
                    for kt in range(nk):
                        ps, psb = nxt('mmA', MM, 4)
                        ks = slice(kt * 128, (kt + 1) * 128)
                        P.op('pe', lambda e, ps=ps, ks=ks, qs=qs, kn=kn, qn=qn: e.matmul(ps[:, :], lhsT=kn[:, ks], rhs=qn[:, qs], start=True, stop=False),
                             reads=knb + qnb, writes=[psb])
                        P.op('pe', lambda e, ps=ps, ks=ks, qs=qs, qr=qr: e.matmul(ps[:, :], lhsT=krT[0:64, ks], rhs=qr[0:64, qs], start=False, stop=True),
                             reads=krTb + qrb, writes=[psb], join=True)
                        pT, pTb = rb(44 + 4 * (rr.get('pT', 0) % 4), 1)
                        rr['pT'] = rr.get('pT', 0) + 1
                        P.op('act', lambda e, ps=ps, pT=pT, kt=kt, h=h: e.activation(out=pT[:, 0:512], in_=ps[:, :], func=AF.Exp, scale=SCK[:, kt, h:h + 1]),
                             reads=[psb, bsck], writes=pTb[0:1])
                        if kt >= 4 * qt:
                            P.op('dve', lambda e, pT=pT, kt=kt, qt=qt: e.tensor_tensor(out=pT[:, 0:512], in0=pT[:, 0:512], in1=maskb[:, kt - 4 * qt, :], op=ALU.mult),
                                 reads=pTb[0:1] + [bconst], writes=pTb[0:1])
                        P.op('pe', lambda e, pT=pT, kt=kt, nk=nk, vhv=vhv, num=num: e.matmul(num[:, :], lhsT=vhv[:, kt, :], rhs=pT[:, 0:512], start=(kt == 0), stop=(kt == nk - 1)),
                             reads=vhb + pTb[0:1], writes=[numb], join=(kt > 0))
                        P.op('pe', lambda e, pT=pT, kt=kt, nk=nk, den=den: e.matmul(den[:, :], lhsT=onesb[:], rhs=pT[:, 0:512], start=(kt == 0), stop=(kt == nk - 1)),
                             reads=pTb[0:1] + [bconst], writes=[denb], join=(kt > 0))
                    rc, rcb = rb(40, 2, F32)
                    P.op('dve', lambda e, rc=rc, den=den: e.reciprocal(out=rc[:, 0:512], in_=den[:, :]), reads=[denb], writes=rcb)
                    ao, aob = rb(42 + (qt % 2), 1)
                    P.op('dve', lambda e, ao=ao, num=num, rc=rc: e.tensor_tensor(out=ao[:, 0:512], in0=num[:, :], in1=rc[:, 0:512], op=ALU.mult), reads=[numb] + rcb, writes=aob[0:1])
                    store(attT_d[h * 128:(h + 1) * 128, qs], ao[:, 0:512], aob[0:1], batt_d[qt])

        def phase_dense_ffn(l):
            h2v = RA[:, 0:16 * S].rearrange("p (c t) -> p c t", t=S)
            phase_norm(OUT, S, T['ffn_norm_g'][l], 'fm', h2v, lambda t: [RAq[t // 4]], src_is_out=True)
            wg, wu, wd = TW(T['tw_dg'][0], 2048, 256), TW(T['tw_du'][0], 2048, 256), TW(T['tw_dd'][0], 512, 512)
            aT, aTb = rb(0, 44)
            aTv = aT.rearrange("p (f t) -> p f t", t=512)
            for qt in range(4):
                for fs in range(22):
                    sg_, sgb = load_slab(wg, 0, 2048, fs * 256, 256)
                    su_, sub = load_slab(wu, 0, 2048, fs * 256, 256)
                    for c in range(2):
                        pg, pgb = nxt('mm6', MM6)
                        pu, pub = nxt('mm6', MM6)
                        mm_acc(pg[:, :], pgb, [(sg_[:, j, c * 128:(c + 1) * 128], h2v[:, j, qt * 512:(qt + 1) * 512]) for j in range(16)], sgb + [RAq[qt]])
                        mm_acc(pu[:, :], pub, [(su_[:, j, c * 128:(c + 1) * 128], h2v[:, j, qt * 512:(qt + 1) * 512]) for j in range(16)], sub + [RAq[qt]])
                        sl_, slb = rb(44 + 2 * (rr.get('silu', 0) % 2), 2, F32)
                        rr['silu'] = rr.get('silu', 0) + 1
                        P.op('act', lambda e, sl_=sl_, pg=pg: e.activation(out=sl_[:, 0:512], in_=pg[:, :], func=AF.Silu), reads=[pgb], writes=slb)
                        P.op('dve', lambda e, sl_=sl_, pu=pu, fs=fs, c=c: e.tensor_tensor(out=aTv[:, fs * 2 + c, :], in0=sl_[:, 0:512], in1=pu[:, :], op=ALU.mult),
                             reads=slb + [pub], writes=aTb, join=True)
                for cb in range(4):
                    accs = [MM[i] for i in range(4)]
                    for fr in range(0, 44, 4):
                        nf = 4
                        sd, sdb = load_slab(wd, fr * 128, nf * 128, cb * 512, 512)
                        for fc in range(nf):
                            f = fr + fc
                            for tt in range(4):
                                ps, psb = accs[tt]
                                P.op('pe', lambda e, ps=ps, f=f, tt=tt, sd=sd, fc=fc: e.matmul(ps[:, :], lhsT=aTv[:, f, tt * 128:(tt + 1) * 128], rhs=sd[:, fc, :], start=(f == 0), stop=(f == 43)),
                                     reads=aTb + sdb, writes=[psb], join=(f > 0))
                    for tt in range(4):
                        t = qt * 4 + tt
                        ps, psb = accs[tt]
                        yt, ytb = rb(48 + 4 * (rr.get('yt2', 0) % 4), 2, F32)
                        rr['yt2'] = rr.get('yt2', 0) + 1
                        P.dma('sp', yt[:, 0:512], OUT[t * 128:(t + 1) * 128, cb * 512:(cb + 1) * 512], reads=[ybuf[t]], writes=ytb)
                        P.op('dve', lambda e, yt=yt, ps=ps: e.tensor_tensor(out=yt[:, 0:512], in0=ps[:, :], in1=yt[:, 0:512], op=ALU.add), reads=[psb] + ytb, writes=ytb)
                        finals.append(store(OUT[t * 128:(t + 1) * 128, cb * 512:(cb + 1) * 512], yt[:, 0:512], ytb, ybuf[t], join=False))

        def phase_moe(l):
            P.dma('sp', RW[:], T['router_w'][0].rearrange("(j p) e -> p j e", p=128), writes=[brw])
            bcast_row(RBI[:], T['router_b'][0], [brw], first=False)
            phase_norm(OUT, S, T['ffn_norm_g'][l], 'tm', src_is_out=True, router=True)
            for m in range(NT):
                ps, psb = AUX
                pairs = [(onesb[:], RSELB[:, k, :]) for k in range(m)] + [(trib[:], RSELB[:, m, :])]
                mm_acc(ps[:, 0:8], psb, pairs, [brt, bconst])
                P.op('dve', lambda e, m=m, ps=ps: e.tensor_copy(out=RPOS[:, m, :], in_=ps[:, 0:8]), reads=[psb], writes=[brt], join=True)
            if rt_dbg is not None:
                for i, src in enumerate((RSEL, RWT, RPOS)):
                    finals.append(store(rt_dbg[:, i * 128:(i + 1) * 128], src[:, :, :].rearrange("p a b -> p (a b)"), [brt], Buf(f'rt_dbg{i}'), join=False))
            aT = RA[:, 0:56 * CAP]
            aTv = aT.rearrange("p (f s) -> p f s", s=CAP)
            aTb = RAq
            GT, GTb = rb(0, 24)
            GTv = GT.rearrange("p (j s) -> p j s", s=CAP)
            OEv = GT.rearrange("p (s f) -> p s f", f=2048)
            PE_, PEb = rb(24, 24)
            PEv = PE_.rearrange("p (k s) -> p k s", s=CAP)
            HALF = CAP // 2
            for e_ in range(8):
                wg, wu, wd = TW(T['tw_mg'][0][e_], 2048, 256), TW(T['tw_mu'][0][e_], 2048, 256), TW(T['tw_md'][0][e_], 1024, 512)
                for k in range(NT):
                    eng = 'dve' if k % 2 == 0 else 'pool'
                    P.op(eng, lambda e, k=k, e_=e_: e.tensor_scalar(out=PEv[:, k, :], in0=iota[:], scalar1=RPOS[:, k, e_:e_ + 1], scalar2=RSEL[:, k, e_:e_ + 1],
                                                                    op0=ALU.is_equal, op1=ALU.mult), reads=[brt, bconst], writes=PEb, join=(k > 0))
                for j in range(16):
                    hs, hsb = rb(48 + 4 * (j % 2), 4)
                    hsv = hs.rearrange("p (k f) -> p k f", f=128)
                    P.dma('sp', hsv, htm_d[:, j * 128:(j + 1) * 128].rearrange("(k p) f -> p k f", p=128), reads=[bhtm], writes=hsb)
                    for half in range(2):
                        ps, psb = nxt('mm', MM)
                        mm_acc(ps[:, 0:HALF], psb, [(hsv[:, k, :], PEv[:, k, half * HALF:(half + 1) * HALF]) for k in range(NT)], hsb + PEb)
                        copy_op(ev_eng(), GTv[:, j, half * HALF:(half + 1) * HALF], ps[:, 0:HALF], [psb], GTb, join=True)
                for fs in range(28):
                    sg_, sgb = load_slab(wg, 0, 2048, fs * 256, 256)
                    su_, sub = load_slab(wu, 0, 2048, fs * 256, 256)
                    for c in range(2):
                        for half in range(2):
                            pg, pgb = nxt('mm6', MM6)
                            pu, pub = nxt('mm6', MM6)
                            hs_ = slice(half * HALF, (half + 1) * HALF)
                            mm_acc(pg[:, 0:HALF], pgb, [(sg_[:, j, c * 128:(c + 1) * 128], GTv[:, j, hs_]) for j in range(16)], sgb + GTb)
                            mm_acc(pu[:, 0:HALF], pub, [(su_[:, j, c * 128:(c + 1) * 128], GTv[:, j, hs_]) for j in range(16)], sub + GTb)
                            sl_, slb = rb(56 + 4 * (rr.get('silu', 0) % 2), 2, F32)
                            rr['silu'] = rr.get('silu', 0) + 1
                            P.op('act', lambda e, sl_=sl_, pg=pg: e.activation(out=sl_[:, 0:HALF], in_=pg[:, 0:HALF], func=AF.Silu), reads=[pgb], writes=slb)
                            P.op('dve', lambda e, sl_=sl_, pu=pu, fs=fs, c=c, hs_=hs_: e.tensor_tensor(out=aTv[:, fs * 2 + c, hs_], in0=sl_[:, 0:HALF], in1=pu[:, 0:HALF], op=ALU.mult),
                                 reads=slb + [pub], writes=aTb, join=True)
                for cb in range(4):
                    accs = [MM[i] for i in range(5)] + [AUX]
                    for fr in range(0, 56, 8):
                        sd, sdb = load_slab(wd, fr * 128, 8 * 128, cb * 512, 512)
                        for fc in range(8):
                            f = fr + fc
                            for s_ in range(NST):
                                ps, psb = accs[s_]
                                P.op('pe', lambda e, ps=ps, f=f, s_=s_, sd=sd, fc=fc: e.matmul(ps[:, :], lhsT=aTv[:, f, s_ * 128:(s_ + 1) * 128], rhs=sd[:, fc, :], start=(f == 0), stop=(f == 55)),
                                     reads=aTb + sdb, writes=[psb], join=(f > 0))
                    for s_ in range(NST):
                        ps, psb = accs[s_]
                        copy_op(ev_eng(), OEv[:, s_, cb * 512:(cb + 1) * 512], ps[:, :], [psb], GTb, join=True)
                for m in range(NT):
                    pwT, pwTb = rb(48 + 4 * (m % 2), 2)
                    pwTv = pwT[:, 0:NST * 128].rearrange("p (s t) -> p s t", t=128)
                    transposes([PEv[:, m, s_ * 128:(s_ + 1) * 128] for s_ in range(NST)], PEb, lambda q, mm_: pwTv[:, q:q + mm_, :], pwTb)
                    for cb in range(4):
                        ps, psb = nxt('mm', MM)
                        mm_acc(ps[:, :], psb, [(pwTv[:, s_, :], OEv[:, s_, cb * 512:(cb + 1) * 512]) for s_ in range(NST)], pwTb + GTb)
                        sc, scb = rb(56 + (rr.get('sc', 0) % 2) * 4, 2, F32)
                        rr['sc'] = rr.get('sc', 0) + 1
                        P.op('dve', lambda e, sc=sc, ps=ps, m=m, e_=e_: e.tensor_scalar(out=sc[:, 0:512], in0=ps[:, :], scalar1=RWT[:, m, e_:e_ + 1], scalar2=None, op0=ALU.mult),
                             reads=[psb, brt], writes=scb)
                        finals.append(P.dma('pool', OUT[m * 128:(m + 1) * 128, cb * 512:(cb + 1) * 512], sc[:, 0:512], reads=scb + [ybuf[m]], writes=[ybuf[m]],
                                            accum_op=ALU.add))

        try:
            if stop_at != 'n4x':
                phase_rope_tables()
            chk('rope')
            for l in layers:
                if 'mixer' in parts:
                    phase_mixer(l)
                if 'ffn' in parts:
                    if l % 2 == 0:
                        phase_dense_ffn(l)
                    else:
                        phase_moe(l)
        except _Stop:
            pass
        if not finals:
            finals.append(store(OUT[0:128, 0:32], RA[:, 0:64].bitcast(F32), RAq[0:1], Buf('dummy_out'), join=False))
        P.emit(final_waits=finals)
    return nc


from concourse.bass_utils import run_bass_kernel_spmd


def kernel(**inputs):
    nc = build_program()
    consts = host_constants()
    B = inputs['x'].shape[0]
    in_maps = []
    tiled_cache = {}
    used = set(nc.used_inputs.keys())
    for b in range(B):
        m = {}
        for k in used:
            if k == 'x':
                m[k] = np.ascontiguousarray(inputs['x'][b])
            elif k == 'mem':
                m[k] = np.ascontiguousarray(inputs['mem'][b])
            elif k == 'pos':
                m[k] = np.ascontiguousarray(inputs['positions'][b].reshape(16, 128).T).astype(np.int32)
            elif k in consts:
                m[k] = consts[k]
            elif k in TILED:
                if k not in tiled_cache:
                    tiled_cache[k] = host_tile(k, np.asarray(inputs[TILED[k][0]]))
                m[k] = tiled_cache[k]
            else:
                m[k] = np.ascontiguousarray(inputs[k])
        in_maps.append(m)
    res = run_bass_kernel_spmd(nc, in_maps, core_ids=list(range(B)))
    return np.stack([np.asarray(r['out']) for r in res.results], axis=0).astype(np.float32)
```
